# Optimizing a Trainium2 kernel written in Bass

```python
import math
import jax, jax.numpy as jnp
from jax import lax
import numpy as np

D_MODEL = 2048
BATCH = 4
SEQ = 2048
DEPTH = 1

MEM_LEN = 256
MIX_WIDTH = D_MODEL
MLA_HEADS = 8
QK_NOPE_DIM = 128
QK_ROPE_DIM = 64
V_HEAD_DIM = 128
Q_LORA_RANK = 384
KV_LORA_RANK = 256
ROPE_THETA = 10000.0
Q_BLOCK = 128
SSM_WIDTH = MIX_WIDTH - MLA_HEADS * V_HEAD_DIM
SSM_GROUP = 16
SSM_GROUPS = SSM_WIDTH // SSM_GROUP
SSM_STATE = 64
DT_MIN = 0.001
DT_MAX = 0.1
XATTN_HEADS = 4
XATTN_HEAD_DIM = D_MODEL // XATTN_HEADS
N_EXPERTS = 32
TOP_K = 4
D_FF = D_MODEL
SWIGLU_LIMIT = 7.0
SWIGLU_ALPHA = 1.702
EXPERT_BLOCK = 256
EPS = 1e-6
COL_Q = Q_LORA_RANK
COL_KV = COL_Q + KV_LORA_RANK
COL_KPE = COL_KV + QK_ROPE_DIM
IN_COLS = COL_KPE + SSM_WIDTH

kernel_name = 'hymba_mla_s5_xattn_moe_block'


def rms_norm(x, g):
    xf = x.astype(jnp.float32)
    xf = xf * lax.rsqrt(jnp.mean(xf * xf, axis=-1, keepdims=True) + EPS)
    return (xf * g.astype(jnp.float32)).astype(x.dtype)


def rope(x, cos, sin):
    x1, x2 = jnp.split(x.astype(jnp.float32), 2, axis=-1)
    return jnp.concatenate([x1 * cos - x2 * sin, x2 * cos + x1 * sin], axis=-1).astype(x.dtype)


def mla_group(c_q, c_kv, k_pe, positions, q_a_norm, w_q_b, kv_a_norm, w_kv_b):
    B, S, _ = c_q.shape
    H = MLA_HEADS
    q = (rms_norm(c_q, q_a_norm) @ w_q_b).reshape(B, S, H, QK_NOPE_DIM + QK_ROPE_DIM)
    q_nope, q_pe = q[..., :QK_NOPE_DIM], q[..., QK_NOPE_DIM:]
    kv = (rms_norm(c_kv, kv_a_norm) @ w_kv_b).reshape(B, S, H, QK_NOPE_DIM + V_HEAD_DIM)
    k_nope, v = kv[..., :QK_NOPE_DIM], kv[..., QK_NOPE_DIM:]
    inv_freq = 1.0 / (ROPE_THETA ** (jnp.arange(0, QK_ROPE_DIM, 2, dtype=jnp.float32) / QK_ROPE_DIM))
    ang = positions.astype(jnp.float32)[..., None] * inv_freq
    cos, sin = jnp.cos(ang), jnp.sin(ang)
    q_pe = rope(q_pe, cos[:, :, None, :], sin[:, :, None, :])
    k_pe = rope(k_pe, cos, sin)
    scale = (QK_NOPE_DIM + QK_ROPE_DIM) ** -0.5
    nb = S // Q_BLOCK

    def to_blocks(t):
        return t.reshape(B, nb, Q_BLOCK, H, t.shape[-1]).transpose(1, 0, 3, 2, 4)

    k_idx = jnp.arange(S)

    def block_attn(args):
        qn, qp, blk = args
        s = (jnp.einsum('bhqd,bkhd->bhqk', qn, k_nope)
             + jnp.einsum('bhqd,bkd->bhqk', qp, k_pe)).astype(jnp.float32) * scale
        q_idx = blk * Q_BLOCK + jnp.arange(Q_BLOCK)
        s = jnp.where(q_idx[:, None] >= k_idx[None, :], s, -jnp.inf)
        p = jax.nn.softmax(s, axis=-1).astype(v.dtype)
        return jnp.einsum('bhqk,bkhd->bhqd', p, v)

    o = lax.map(block_attn, (to_blocks(q_nope), to_blocks(q_pe), jnp.arange(nb)))
    return o.transpose(1, 0, 3, 2, 4).reshape(B, S, H * V_HEAD_DIM)


def _cscan_op(e1, e2):
    a1r, a1i, b1r, b1i = e1
    a2r, a2i, b2r, b2i = e2
    ar = a2r * a1r - a2i * a1i
    ai = a2r * a1i + a2i * a1r
    br = a2r * b1r - a2i * b1i + b2r
    bi = a2r * b1i + a2i * b1r + b2i
    return (ar, ai, br, bi)


def s5_group(u, lam_re, lam_im, log_dt, b_re, b_im, c_re, c_im, d_skip, w_glu, b_glu):
    B, S, _ = u.shape
    uf = u.astype(jnp.float32)
    lr = lam_re.astype(jnp.float32)
    li = lam_im.astype(jnp.float32)
    dt = jnp.exp(log_dt.astype(jnp.float32))[:, None]
    mag = jnp.exp(lr * dt)
    ab_re = mag * jnp.cos(li * dt)
    ab_im = mag * jnp.sin(li * dt)
    nr, ni = ab_re - 1.0, ab_im
    den = lr * lr + li * li
    coef_re = (nr * lr + ni * li) / den
    coef_im = (ni * lr - nr * li) / den
    br = b_re.astype(jnp.float32)
    bim = b_im.astype(jnp.float32)
    bb_re = coef_re[..., None] * br - coef_im[..., None] * bim
    bb_im = coef_re[..., None] * bim + coef_im[..., None] * br
    ug = uf.reshape(B, S, SSM_GROUPS, SSM_GROUP)
    bu_re = jnp.einsum('bsgc,gpc->bsgp', ug, bb_re)
    bu_im = jnp.einsum('bsgc,gpc->bsgp', ug, bb_im)
    a_re = jnp.broadcast_to(ab_re, bu_re.shape)
    a_im = jnp.broadcast_to(ab_im, bu_re.shape)
    _, _, x_re, x_im = lax.associative_scan(_cscan_op, (a_re, a_im, bu_re, bu_im), axis=1)
    y = (jnp.einsum('bsgp,gcp->bsgc', x_re, c_re.astype(jnp.float32))
         - jnp.einsum('bsgp,gcp->bsgc', x_im, c_im.astype(jnp.float32)))
    y = y.reshape(B, S, SSM_WIDTH) + d_skip.astype(jnp.float32) * uf
    y = jax.nn.gelu(y).astype(u.dtype)
    gate = jax.nn.sigmoid((y @ w_glu + b_glu).astype(jnp.float32)).astype(u.dtype)
    return y * gate


def cross_attn(h, mem_n, w_xq, w_xk, w_xv, w_xo):
    B, S, D = h.shape
    M = mem_n.shape[1]
    q = (h @ w_xq).reshape(B, S, XATTN_HEADS, XATTN_HEAD_DIM)
    k = (mem_n @ w_xk).reshape(B, M, XATTN_HEADS, XATTN_HEAD_DIM)
    v = (mem_n @ w_xv).reshape(B, M, XATTN_HEADS, XATTN_HEAD_DIM)
    s = jnp.einsum('bqhd,bkhd->bhqk', q, k).astype(jnp.float32) * XATTN_HEAD_DIM ** -0.5
    p = jax.nn.softmax(s, axis=-1).astype(v.dtype)
    o = jnp.einsum('bhqk,bkhd->bqhd', p, v).reshape(B, S, D)
    return o @ w_xo


def moe(xn, w_router, b_router, w_up, b_up, w_down, b_down):
    B, S, D = xn.shape
    T = B * S
    TK = T * TOP_K
    xt = xn.reshape(T, D)
    logits = (xt @ w_router + b_router).astype(jnp.float32)
    top_val, top_idx = lax.top_k(logits, TOP_K)
    gates = jax.nn.softmax(top_val, axis=-1)
    flat_e = top_idx.reshape(TK)
    flat_g = gates.reshape(TK)
    flat_tok = jnp.arange(TK, dtype=jnp.int32) // TOP_K
    order = jnp.argsort(flat_e)
    sorted_e = flat_e[order]
    counts = jnp.bincount(flat_e, length=N_EXPERTS)
    padded = (counts + EXPERT_BLOCK - 1) // EXPERT_BLOCK * EXPERT_BLOCK
    pad_end = jnp.cumsum(padded)
    pad_start = pad_end - padded
    start = jnp.cumsum(counts) - counts
    dest = pad_start[sorted_e] + jnp.arange(TK, dtype=jnp.int32) - start[sorted_e]
    n_blocks = -(-TK // EXPERT_BLOCK) + N_EXPERTS
    n_slots = n_blocks * EXPERT_BLOCK
    slot_tok = jnp.full((n_slots,), T, jnp.int32).at[dest].set(flat_tok[order])
    slot_gate = jnp.zeros((n_slots,), jnp.float32).at[dest].set(flat_g[order])
    block_e = jnp.minimum(
        jnp.searchsorted(pad_end, jnp.arange(n_blocks) * EXPERT_BLOCK, side='right'),
        N_EXPERTS - 1)
    x_pad = jnp.concatenate([xt, jnp.zeros((1, D), xt.dtype)], axis=0)

    def expert_block(args):
        tok, e = args
        xb = x_pad[tok]
        hu = (xb @ w_up[e] + b_up[e]).astype(jnp.float32)
        glu = jnp.minimum(hu[:, :D_FF], SWIGLU_LIMIT)
        lin = jnp.clip(hu[:, D_FF:], -SWIGLU_LIMIT, SWIGLU_LIMIT)
        act = (glu * jax.nn.sigmoid(SWIGLU_ALPHA * glu) * (lin + 1.0)).astype(xb.dtype)
        return act @ w_down[e] + b_down[e]

    out = lax.map(expert_block, (slot_tok.reshape(n_blocks, EXPERT_BLOCK), block_e))
    weighted = out.reshape(n_slots, D).astype(jnp.float32) * slot_gate[:, None]
    y = jnp.zeros((T + 1, D), jnp.float32).at[slot_tok].add(weighted)
    return y[:T].reshape(B, S, D).astype(xn.dtype)


def setup_inputs(seed: int = 0) -> dict:
    key = jax.random.key(seed)
    ks = jax.random.split(key, 40)
    f32 = jnp.float32
    L, D, G, P, C = DEPTH, D_MODEL, SSM_GROUPS, SSM_STATE, SSM_GROUP

    def w(k, shape, fan_in):
        return jax.random.normal(k, shape, f32) * fan_in ** -0.5

    def gain(k, shape):
        return 1.0 + 0.02 * jax.random.normal(k, shape, f32)

    def small(k, shape):
        return 0.01 * jax.random.normal(k, shape, f32)

    x = jax.random.normal(ks[0], (BATCH, SEQ, D), f32)
    mem = jax.random.normal(ks[1], (BATCH, MEM_LEN, D), f32)
    offsets = jax.random.randint(ks[2], (BATCH, 1), 0, 1024, dtype=jnp.int32)
    positions = (offsets + jnp.arange(SEQ, dtype=jnp.int32)[None, :]).astype(jnp.int32)
    lam_re = -0.5 + 0.01 * jax.random.normal(ks[8], (L, G, P), f32)
    lam_im = math.pi * jnp.arange(P, dtype=f32)[None, None, :] + 0.01 * jax.random.normal(ks[9], (L, G, P), f32)
    log_dt = jax.random.uniform(ks[10], (L, G), f32, math.log(DT_MIN), math.log(DT_MAX))
    return {
        'x': x,
        'mem': mem,
        'positions': positions,
        'attn_norm': gain(ks[3], (L, D)),
        'w_in': w(ks[4], (L, D, IN_COLS), D),
        'q_a_norm': gain(ks[5], (L, Q_LORA_RANK)),
        'w_q_b': w(ks[6], (L, Q_LORA_RANK, MLA_HEADS * (QK_NOPE_DIM + QK_ROPE_DIM)), Q_LORA_RANK),
        'kv_a_norm': gain(ks[7], (L, KV_LORA_RANK)),
        'w_kv_b': w(ks[11], (L, KV_LORA_RANK, MLA_HEADS * (QK_NOPE_DIM + V_HEAD_DIM)), KV_LORA_RANK),
        'ssm_lambda_re': lam_re,
        'ssm_lambda_im': lam_im,
        'ssm_log_dt': log_dt,
        'ssm_b_re': w(ks[12], (L, G, P, C), 2 * C),
        'ssm_b_im': w(ks[13], (L, G, P, C), 2 * C),
        'ssm_c_re': w(ks[14], (L, G, C, P), 2 * P),
        'ssm_c_im': w(ks[15], (L, G, C, P), 2 * P),
        'ssm_d': 0.1 * jax.random.normal(ks[16], (L, SSM_WIDTH), f32),
        'w_glu': w(ks[17], (L, SSM_WIDTH, SSM_WIDTH), SSM_WIDTH),
        'b_glu': small(ks[18], (L, SSM_WIDTH)),
        'mix_norm_attn': gain(ks[19], (L, MLA_HEADS * V_HEAD_DIM)),
        'mix_norm_ssm': gain(ks[20], (L, SSM_WIDTH)),
        'w_out': w(ks[21], (L, MIX_WIDTH, D), MIX_WIDTH),
        'xattn_norm': gain(ks[22], (L, D)),
        'mem_norm': gain(ks[23], (L, D)),
        'w_xq': w(ks[24], (L, D, D), D),
        'w_xk': w(ks[25], (L, D, D), D),
        'w_xv': w(ks[26], (L, D, D), D),
        'w_xo': w(ks[27], (L, D, D), D),
        'ffn_norm': gain(ks[28], (L, D)),
        'w_router': w(ks[29], (L, D, N_EXPERTS), D),
        'b_router': small(ks[30], (L, N_EXPERTS)),
        'w_up': w(ks[31], (L, N_EXPERTS, D, 2 * D_FF), D),
        'b_up': small(ks[32], (L, N_EXPERTS, 2 * D_FF)),
        'w_down': w(ks[33], (L, N_EXPERTS, D_FF, D), D_FF),
        'b_down': small(ks[34], (L, N_EXPERTS, D)),
        'final_norm': gain(ks[35], (D,)),
    }


def reference(x, mem, positions, attn_norm, w_in, q_a_norm, w_q_b, kv_a_norm, w_kv_b,
              ssm_lambda_re, ssm_lambda_im, ssm_log_dt, ssm_b_re, ssm_b_im, ssm_c_re, ssm_c_im,
              ssm_d, w_glu, b_glu, mix_norm_attn, mix_norm_ssm, w_out,
              xattn_norm, mem_norm, w_xq, w_xk, w_xv, w_xo,
              ffn_norm, w_router, b_router, w_up, b_up, w_down, b_down, final_norm):
    for l in range(DEPTH):
        h = rms_norm(x, attn_norm[l])
        proj = h @ w_in[l]
        c_q = proj[..., :COL_Q]
        c_kv = proj[..., COL_Q:COL_KV]
        k_pe = proj[..., COL_KV:COL_KPE]
        u = proj[..., COL_KPE:]
        attn_o = mla_group(c_q, c_kv, k_pe, positions, q_a_norm[l], w_q_b[l], kv_a_norm[l], w_kv_b[l])
        ssm_o = s5_group(u, ssm_lambda_re[l], ssm_lambda_im[l], ssm_log_dt[l], ssm_b_re[l], ssm_b_im[l],
                         ssm_c_re[l], ssm_c_im[l], ssm_d[l], w_glu[l], b_glu[l])
        mixed = jnp.concatenate([rms_norm(attn_o, mix_norm_attn[l]), rms_norm(ssm_o, mix_norm_ssm[l])], axis=-1)
        x = x + mixed @ w_out[l]
        x = x + cross_attn(rms_norm(x, xattn_norm[l]), rms_norm(mem, mem_norm[l]),
                           w_xq[l], w_xk[l], w_xv[l], w_xo[l])
        x = x + moe(rms_norm(x, ffn_norm[l]), w_router[l], b_router[l], w_up[l], b_up[l], w_down[l], b_down[l])
    return rms_norm(x, final_norm)
```

```python
import numpy as np
from contextlib import ExitStack
import concourse.bass as bass
import concourse.mybir as mybir
from concourse.bass_utils import run_bass_kernel_spmd

F32 = mybir.dt.float32
BF16 = mybir.dt.bfloat16
I32 = mybir.dt.int32
ALU = mybir.AluOpType
AF = mybir.ActivationFunctionType

D = 2048
SEQ = 2048
NOWN = 1024
NALL = 2048
MEM = 256
NEXP = 32
DFF = 2048
EPS = 1e-6
NEG = -30000.0
EPOCH = 20000


class Reg:
    __slots__ = ("w", "r", "dsem", "name")

    def __init__(self, name=""):
        self.w = []
        self.r = []
        self.dsem = None
        self.name = name


class Prog:
    def __init__(self, nc):
        self.nc = nc
        self.es = ExitStack()
        self.eng = {"pe": nc.tensor, "dve": nc.vector, "act": nc.scalar,
                    "pool": nc.gpsimd, "sp": nc.sync}
        self.cnt = {k: 0 for k in self.eng}
        self.epoch = {k: 0 for k in self.eng}
        self.sems = {}
        self.seen = {k: {} for k in self.eng}
        self.nsem = 0
        self.dma_events = []
        self.enabled = True
        for k in self.eng:
            self._newsem((k, 0))

    def _newsem(self, key):
        s = self.es.enter_context(self.nc.semaphore("s%d" % self.nsem))
        self.nsem += 1
        self.sems[key] = s
        return s

    def _wait(self, e, ev):
        key, val = ev
        if self.seen[e].get(key, 0) >= val:
            return
        self.eng[e].wait_ge(self.sems[key], val)
        self.seen[e][key] = val

    def _deps(self, e, R, W):
        evs = []
        for r in R:
            evs += r.w
        for w in W:
            for ev in w.w + w.r:
                if ev[0][0] == e:
                    continue
                evs.append(ev)
        for ev in evs:
            self._wait(e, ev)

    def op(self, e, fn, R=(), W=()):
        if not self.enabled:
            return None
        self._deps(e, R, W)
        inst = fn(self.eng[e])
        if self.cnt[e] >= EPOCH:
            self.epoch[e] += 1
            self.cnt[e] = 0
            self._newsem((e, self.epoch[e]))
        key = (e, self.epoch[e])
        inst.then_inc(self.sems[key], 1)
        self.cnt[e] += 1
        ev = (key, self.cnt[e])
        for w in W:
            w.w = [x for x in w.w if x[0][0] != e] + [ev]
            w.r = []
        for r in R:
            r.r = [x for x in r.r if x[0][0] != e] + [ev]
        return inst

    def dma(self, q, out, in_, R=(), W=(), sreg=None):
        if not self.enabled:
            return None
        self._deps(q, R, W)
        own = sreg if sreg is not None else (W[0] if len(W) else R[0])
        if own.dsem is None:
            key = ("d", self.nsem)
            self._newsem(key)
            own.dsem = [key, 0]
        inst = self.eng[q].dma_start(out=out, in_=in_)
        own.dsem[1] += 16
        inst.then_inc(self.sems[own.dsem[0]], 16)
        ev = (own.dsem[0], own.dsem[1])
        for w in W:
            w.w = [x for x in w.w if x[0] != ev[0]] + [ev]
            w.r = []
        for r in R:
            r.r = [x for x in r.r if x[0] != ev[0]] + [ev]
        self.dma_events.append(ev)
        return inst

    def barrier(self):
        if not self.enabled:
            return
        evs = [((k, self.epoch[k]), self.cnt[k]) for k in self.eng if self.cnt[k] > 0]
        last = {}
        for ev in self.dma_events:
            last[ev[0]] = max(last.get(ev[0], 0), ev[1])
        evs += list(last.items())
        for e in self.eng:
            for ev in evs:
                if ev[0][0] == e:
                    continue
                self._wait(e, ev)
        self.dma_events = []

    def final_wait(self, e="sp"):
        last = {}
        for ev in self.dma_events:
            last[ev[0]] = max(last.get(ev[0], 0), ev[1])
        for ev in last.items():
            self._wait(e, ev)
        for k in self.eng:
            if k != e and self.cnt[k] > 0:
                self._wait(e, ((k, self.epoch[k]), self.cnt[k]))


PV_ROWS = dict(attn=0, xattn=16, memn=32, ffn=48, qa=64, kva=67, mixa=69, mixs=77,
               ssmd=85, bglu=93)
TWO_PI = 6.283185307179586
C1 = 6.28125
C2 = TWO_PI - C1


def build(taps=(), stop=None):
    nc = bass.Bass("TRN2", target_bir_lowering=False)
    P = Prog(nc)
    NEXP_D = NEXP if stop is None else 1

    def stop_if(tag):
        if stop == tag:
            P.final_wait("sp")
            P.enabled = False

    def din(name, shape, dt=F32):
        return nc.dram_tensor(name, list(shape), dt, kind="ExternalInput").ap()

    x_own = din("x_own", [NOWN, D])
    x_pre = din("x_pre", [NOWN, D])
    pos_d = din("pos", [1, NALL], I32)
    pbias_d = din("pbias", [128, 1])
    mem_d = din("mem", [MEM, D])
    pv_d = din("pv", [128, 128])
    w_in = din("w_in", [D, 1728])
    w_q_b = din("w_q_b", [384, 1536])
    w_kv_b = din("w_kv_b", [256, 2048])
    lam_re = din("lam_re", [64, 64])
    lam_im = din("lam_im", [64, 64])
    log_dt = din("log_dt", [64, 1])
    b_re = din("b_re", [64, 64, 16])
    b_im = din("b_im", [64, 64, 16])
    c_re = din("c_re", [64, 16, 64])
    c_im = din("c_im", [64, 16, 64])
    w_glu = din("w_glu", [1024, 1024])
    w_out = din("w_out", [D, D])
    w_xq = din("w_xq", [D, D])
    w_xk = din("w_xk", [D, D])
    w_xv = din("w_xv", [D, D])
    w_xo = din("w_xo", [D, D])
    w_router = din("w_router", [D, NEXP])
    b_router = din("b_router", [1, NEXP])
    w_up = din("w_up", [NEXP_D, D, 2 * DFF])
    b_up = din("b_up", [NEXP * 32, 128])
    w_down = din("w_down", [NEXP_D, DFF, D])
    b_down = din("b_down", [NEXP, D])
    final_norm = din("final_norm", [1, D])
    y_d = nc.dram_tensor("y", [NOWN, D], F32, kind="ExternalOutput").ap()
    tap_d = {}
    for (tn, tshape, tdt) in taps:
        tap_d[tn] = nc.dram_tensor("tap_" + tn, list(tshape), tdt, kind="ExternalOutput").ap()

    uid = [0]

    def alloc(es, name, shape, dt):
        uid[0] += 1
        return es.enter_context(nc.sbuf_tensor("%s_%d" % (name, uid[0]), list(shape), dt))

    def palloc(es, name, shape, dt):
        uid[0] += 1
        return es.enter_context(nc.psum_tensor("%s_%d" % (name, uid[0]), list(shape), dt))

    def TT(e, out, a, b, op, R, W):
        return P.op(e, lambda g: g.tensor_tensor(out=out, in0=a, in1=b, op=op), R, W)

    def TS(e, out, a, s1, s2, op0, op1, R, W):
        if op1 is None:
            return P.op(e, lambda g: g.tensor_scalar(out=out, in0=a, scalar1=s1, scalar2=None, op0=op0), R, W)
        return P.op(e, lambda g: g.tensor_scalar(out=out, in0=a, scalar1=s1, scalar2=s2, op0=op0, op1=op1), R, W)

    def STT(out, a, s, b, op0, op1, R, W, **kw):
        return P.op("dve", lambda g: g.scalar_tensor_tensor(out=out, in0=a, scalar=s, in1=b, op0=op0, op1=op1, **kw), R, W)

    def ACT(out, in_, func, R, W, **kw):
        return P.op("act", lambda g: g.activation(out=out, in_=in_, func=func, **kw), R, W)

    def MM(out, lhsT, rhs, start, stop, R, W):
        return P.op("pe", lambda g: g.matmul(out, lhsT, rhs, start=start, stop=stop), R, W)

    def TR(out, in_, ident, R, W):
        return P.op("pe", lambda g: g.transpose(out, in_, ident), R, W)

    def CP(e, out, in_, R, W):
        if e == "act":
            return P.op(e, lambda g: g.activation(out=out, in_=in_, func=AF.Copy), R, W)
        return P.op(e, lambda g: g.tensor_copy(out=out, in_=in_), R, W)

    def MS(e, ap, val, W):
        return P.op(e, lambda g: g.memset(ap, val), (), W)

    def tap(name, sb_ap, R):
        if name in tap_d:
            P.dma("sp", tap_d[name], sb_ap, R=R, sreg=Reg())

    G = ExitStack()
    with G:
        ident_f = alloc(G, "ident_f", [128, 128], F32)
        ident_b = alloc(G, "ident_b", [128, 128], BF16)
        ones_b = alloc(G, "ones_b", [128, 128], BF16)
        ones_f = alloc(G, "ones_f", [128, 128], F32)
        maskW = alloc(G, "maskW", [128, 896], BF16)
        pvT = alloc(G, "pvT", [128, 128], F32)
        pbias = alloc(G, "pbias_sb", [128, 1], F32)
        wslot = [alloc(G, "wslot%d" % i, [128, 16, 512], BF16) for i in range(3)]
        wreg = [Reg("w%d" % i) for i in range(3)]
        wctr = [0]
        CONST = Reg("const")

        def wload(dram2d, nk, c0, ncols, r0=0):
            s = wctr[0] % 3
            wctr[0] += 1
            src = dram2d[r0:r0 + nk * 128, c0:c0 + ncols].rearrange("(kc p) n -> p kc n", p=128)
            P.dma("pool", wslot[s][:, 0:nk, 0:ncols], src, W=[wreg[s]])
            return wslot[s], wreg[s]

        with ExitStack() as C0:
            it = alloc(C0, "iota_t", [128, 896], I32)
            pvr = alloc(C0, "pv_raw", [128, 128], F32)
            ps0 = palloc(C0, "ps_c0", [128, 512], F32)
            rt = Reg()
            P.op("pool", lambda g: g.iota(it[:, 0:128], pattern=[[1, 128]], base=0, channel_multiplier=-1), (), [rt])
            TS("dve", ident_f[:], it[:, 0:128], 0, None, ALU.is_equal, None, [rt], [CONST])
            TS("dve", ident_b[:], it[:, 0:128], 0, None, ALU.is_equal, None, [rt], [CONST])
            MS("dve", ones_b[:], 1.0, [CONST])
            MS("dve", ones_f[:], 1.0, [CONST])
            P.op("pool", lambda g: g.iota(it[:, :], pattern=[[1, 896]], base=-384, channel_multiplier=-1), [rt], [rt])
            TS("dve", maskW[:], it[:, :], 0, None, ALU.is_ge, None, [rt], [CONST])
            rp = Reg()
            P.dma("sp", pvr[:], pv_d, W=[rp])
            P.dma("sp", pbias[:], pbias_d, W=[CONST])
            rps = Reg()
            TR(ps0[:, 0:128], pvr[:], ident_f[:], [rp, CONST], [rps])
            CP("dve", pvT[:], ps0[:, 0:128], [rps], [CONST])
            P.barrier()

        def pcol(name, i):
            c = PV_ROWS[name] + i
            return pvT[:, c:c + 1]

        def neg_sincos(es, ang, shape, nsin, ncos, rin, rout):
            n_part = shape[0]
            kt = alloc(es, "sc_k", shape, I32)
            kf = alloc(es, "sc_kf", shape, F32)
            r1 = alloc(es, "sc_r1", shape, F32)
            r2 = alloc(es, "sc_r2", shape, F32)
            rr = Reg()
            TS("dve", r1[:], ang, 1.0 / TWO_PI, None, ALU.mult, None, [rin], [rr])
            CP("dve", kt[:], r1[:], [rr], [rr])
            CP("dve", kf[:], kt[:], [rr], [rr])
            STT(r1[:], kf[:], -C1, ang, ALU.mult, ALU.add, [rr, rin], [rr])
            STT(r2[:], kf[:], -C2, r1[:], ALU.mult, ALU.add, [rr], [rr])
            TS("dve", r1[:], r2[:], 0.0, TWO_PI, ALU.is_lt, ALU.mult, [rr], [rr])
            TT("dve", r2[:], r2[:], r1[:], ALU.add, [rr], [rr])
            TS("dve", r1[:], r2[:], TWO_PI, -TWO_PI, ALU.is_ge, ALU.mult, [rr], [rr])
            TT("dve", r2[:], r2[:], r1[:], ALU.add, [rr], [rr])
            ACT(nsin, r2[:], AF.Sin, [rr], [rout], bias=negpi[0:n_part, :])
            TS("dve", r1[:], r2[:], np.pi / 2, None, ALU.add, None, [rr], [rr])
            TS("dve", kf[:], r1[:], TWO_PI, -TWO_PI, ALU.is_ge, ALU.mult, [rr], [rr])
            TT("dve", r1[:], r1[:], kf[:], ALU.add, [rr], [rr])
            ACT(ncos, r1[:], AF.Sin, [rr], [rout], bias=negpi[0:n_part, :])

        negpi = alloc(G, "negpi", [128, 1], F32)
        MS("dve", negpi[:], -np.pi, [CONST])
        epsc = alloc(G, "epsc", [128, 1], F32)
        MS("dve", epsc[:], EPS, [CONST])

        def rstd_from(ssq_ap, out_ap, n, rin, rout, tmp_ap):
            ACT(tmp_ap, ssq_ap, AF.Sqrt, [rin], [rout], scale=1.0 / n, bias=epsc[0:ssq_ap.shape[0], :])
            P.op("dve", lambda g: g.reciprocal(out=out_ap, in_=tmp_ap), [rout], [rout])

        zeroc = alloc(G, "zeroc", [128, 1], F32)
        MS("dve", zeroc[:], 0.0, [CONST])
        mix_d = nc.dram_tensor("mix_scr", [D, NOWN], BF16, kind="Internal").ap()
        r_mixd = Reg("mixd")
        psb = [palloc(G, "psb%d" % i, [128, 512], F32) for i in range(7)]
        r_ps = [Reg("ps%d" % i) for i in range(7)]
        pbt = palloc(G, "pbt", [128, 1024], BF16)
        r_pbt = Reg("pbt")

        UT = ExitStack()
        with UT:
            uT = alloc(UT, "uT", [128, 8, NALL], BF16)
            r_uT = Reg("uT")
            ATT = ExitStack()
            with ATT:
                cqT = alloc(ATT, "cqT", [128, 3, NOWN], BF16)
                ckvT = alloc(ATT, "ckvT", [128, 2, NALL], BF16)
                kpeT = alloc(ATT, "kpeT", [64, NALL], BF16)
                cosT = alloc(ATT, "cosT", [64, NALL], F32)
                sinS = alloc(ATT, "sinS", [64, NALL], F32)
                r_cq, r_ckv, r_kpe, r_cs = Reg("cq"), Reg("ckv"), Reg("kpe"), Reg("cs")
                with ExitStack() as A0:
                    posi = alloc(A0, "posi", [64, NALL], I32)
                    ang = alloc(A0, "ang", [64, NALL], F32)
                    nsn = alloc(A0, "nsn", [64, NALL], F32)
                    idx = alloc(A0, "idx", [64, 1], I32)
                    invf = alloc(A0, "invf", [64, 1], F32)
                    ra = Reg()
                    P.dma("sp", posi[:], pos_d.broadcast_to([64, NALL]), W=[ra])
                    P.op("pool", lambda g: g.iota(idx[0:32, :], pattern=[[0, 1]], base=0, channel_multiplier=1), (), [ra])
                    P.op("pool", lambda g: g.iota(idx[32:64, :], pattern=[[0, 1]], base=0, channel_multiplier=1), (), [ra])
                    ACT(invf[:], idx[:], AF.Exp, [ra], [ra], scale=-float(np.log(10000.0)) / 32.0)
                    CP("dve", ang[:], posi[:], [ra], [ra])
                    TS("dve", ang[:], ang[:], invf[:, 0:1], None, ALU.mult, None, [ra], [ra])
                    neg_sincos(A0, ang[:], [64, NALL], nsn[:], cosT[:], ra, r_cs)
                    TS("dve", cosT[:], cosT[:], -1.0, None, ALU.mult, None, [r_cs], [r_cs])
                    CP("dve", sinS[0:32, :], nsn[0:32, :], [r_cs], [r_cs])
                    TS("dve", sinS[32:64, :], nsn[32:64, :], -1.0, None, ALU.mult, None, [r_cs], [r_cs])
                    P.barrier()
                tap("cosT", cosT[:, :], [r_cs])
                with ExitStack() as A:
                    xT = alloc(A, "xT", [128, 16, NOWN], BF16)
                    r_xT = [Reg("xT%d" % c) for c in range(8)]
                    xs = [alloc(A, "xs%d" % i, [128, D], F32) for i in range(2)]
                    xnb = [alloc(A, "xnb%d" % i, [128, D], BF16) for i in range(2)]
                    r_xs = [Reg(), Reg()]
                    r_xn = [Reg(), Reg()]
                    st = alloc(A, "statA", [128, 64], F32)
                    r_st = Reg()
                    csb = alloc(A, "csb", [128, 384], F32)
                    cjunk = alloc(A, "cjunk", [128, 384], F32)
                    cnb = alloc(A, "cnb", [128, 384], BF16)
                    r_csb, r_cnb = Reg(), Reg()
                    wrot = alloc(A, "wrotA", [128, 16, 64], BF16)
                    r_wrot = Reg()
                    ta = alloc(A, "ropeA", [64, 512], F32)
                    tb = alloc(A, "ropeB", [64, 512], F32)
                    r_ta = Reg()
                    win3 = w_in.rearrange("(kc p) n -> p kc n", p=128)
                    P.dma("pool", wrot[:, :, 0:32], win3[:, :, 672:704], W=[r_wrot])
                    P.dma("pool", wrot[:, :, 32:64], win3[:, :, 640:672], W=[r_wrot])

                    def norm_T(ncols, nchunk, stcol, gain_name, dstT, dcol0, r_dst, ps_src, r_psrc):
                        CP("act", csb[:, 0:ncols], ps_src, [r_psrc], [r_csb])
                        ACT(cjunk[:, 0:ncols], csb[:, 0:ncols], AF.Square, [r_csb], [r_st], accum_out=st[:, stcol:stcol + 1])
                        rstd_from(st[:, stcol:stcol + 1], st[:, stcol + 1:stcol + 2], ncols, r_st, r_st, st[:, stcol + 2:stcol + 3])
                        TS("dve", cnb[:, 0:ncols], csb[:, 0:ncols], st[:, stcol + 1:stcol + 2], None, ALU.mult, None,
                           [r_csb, r_st], [r_cnb])
                        for j in range(nchunk):
                            TR(pbt[:, j * 128:(j + 1) * 128], cnb[:, j * 128:(j + 1) * 128], ident_b[:], [r_cnb, CONST], [r_pbt])
                        for j in range(nchunk):
                            TS("dve", dstT[:, j, dcol0:dcol0 + 128], pbt[:, j * 128:(j + 1) * 128], pcol(gain_name, j), None,
                               ALU.mult, None, [r_pbt, CONST], [r_dst])

                    for hf in range(2):
                        for c in range(8):
                            s = c % 2
                            src = (x_pre if hf == 0 else x_own)[c * 128:(c + 1) * 128, :]
                            P.dma("sp", xs[s][:], src, W=[r_xs[s]])
                            sc = hf * 8 + c
                            ACT(xnb[s][:], xs[s][:], AF.Square, [r_xs[s]], [r_xn[s], r_st], accum_out=st[:, sc:sc + 1])
                            rstd_from(st[:, sc:sc + 1], st[:, 16 + sc:17 + sc], D, r_st, r_st, st[:, 32 + sc:33 + sc])
                            ACT(xnb[s][:], xs[s][:], AF.Copy, [r_xs[s], r_st], [r_xn[s]], scale=st[:, 16 + sc:17 + sc])
                            for q in range(2):
                                for j in range(8):
                                    fc = q * 8 + j
                                    TR(pbt[:, j * 128:(j + 1) * 128], xnb[s][:, fc * 128:(fc + 1) * 128], ident_b[:],
                                       [r_xn[s], CONST], [r_pbt])
                                for j in range(8):
                                    fc = q * 8 + j
                                    if j % 2 == 0:
                                        TS("dve", xT[:, fc, c * 128:(c + 1) * 128], pbt[:, j * 128:(j + 1) * 128],
                                           pcol("attn", fc), None, ALU.mult, None, [r_pbt, CONST], [r_xT[c]])
                                    else:
                                        ACT(xT[:, fc, c * 128:(c + 1) * 128], pbt[:, j * 128:(j + 1) * 128], AF.Copy,
                                            [r_pbt, CONST], [r_xT[c]], scale=pcol("attn", fc))
                        if hf == 1:
                            tap("xT", xT[:, :, :], r_xT)
                            wt, wr = wload(w_in, 16, 0, 384)
                            for c in range(8):
                                q = c % 2
                                for kc in range(16):
                                    MM(psb[q][:, 0:384], xT[:, kc, c * 128:(c + 1) * 128], wt[:, kc, 0:384], kc == 0, kc == 15,
                                       [r_xT[c], wr], [r_ps[q]])
                                norm_T(384, 3, 48, "qa", cqT, c * 128, r_cq, psb[q][:, 0:384], r_ps[q])
                        wt, wr = wload(w_in, 16, 384, 320)
                        for c in range(8):
                            q = c % 2
                            for kc in range(16):
                                MM(psb[q][:, 0:256], xT[:, kc, c * 128:(c + 1) * 128], wt[:, kc, 0:256], kc == 0, kc == 15,
                                   [r_xT[c], wr], [r_ps[q]])
                            norm_T(256, 2, 52, "kva", ckvT, hf * NOWN + c * 128, r_ckv, psb[q][:, 0:256], r_ps[q])
                        for t in range(2):
                            cols = slice(t * 512, (t + 1) * 512)
                            gcols = slice(hf * NOWN + t * 512, hf * NOWN + (t + 1) * 512)
                            for kc in range(16):
                                MM(psb[2][0:64, :], wt[:, kc, 256:320], xT[:, kc, cols], kc == 0, kc == 15,
                                   r_xT[4 * t:4 * t + 4] + [wr], [r_ps[2]])
                            for kc in range(16):
                                MM(psb[3][0:64, :], wrot[:, kc, :], xT[:, kc, cols], kc == 0, kc == 15,
                                   r_xT[4 * t:4 * t + 4] + [r_wrot], [r_ps[3]])
                            TT("dve", ta[:], psb[2][0:64, :], cosT[:, gcols], ALU.mult, [r_ps[2], r_cs], [r_ta])
                            TT("dve", tb[:], psb[3][0:64, :], sinS[:, gcols], ALU.mult, [r_ps[3], r_cs], [r_ta])
                            TT("dve", kpeT[:, gcols], ta[:], tb[:], ALU.add, [r_ta], [r_kpe])
                        for pc in range(2):
                            wt, wr = wload(w_in, 16, 704 + 512 * pc, 512)
                            for sc in range(4):
                                cc = pc * 4 + sc
                                for t in range(2):
                                    q = 4 + (sc * 2 + t) % 2
                                    cols = slice(t * 512, (t + 1) * 512)
                                    gcols = slice(hf * NOWN + t * 512, hf * NOWN + (t + 1) * 512)
                                    for kc in range(16):
                                        MM(psb[q][:, :], wt[:, kc, sc * 128:(sc + 1) * 128], xT[:, kc, cols], kc == 0, kc == 15,
                                           r_xT[4 * t:4 * t + 4] + [wr], [r_ps[q]])
                                    if t % 2 == 0:
                                        CP("dve", uT[:, cc, gcols], psb[q][:, :], [r_ps[q]], [r_uT])
                                    else:
                                        CP("act", uT[:, cc, gcols], psb[q][:, :], [r_ps[q]], [r_uT])
                    tap("cqT", cqT[:, :, :], [r_cq])
                    tap("ckvT", ckvT[:, :, :], [r_ckv])
                    tap("kpeT", kpeT[:, :], [r_kpe])
                    tap("uT", uT[:, :, :], [r_uT])
                    P.barrier()
                    stop_if("A")
                with ExitStack() as BC:
                    attnT = alloc(BC, "attnT", [128, 8, NOWN], BF16)
                    r_attn = Reg("attn")
                    qnT = alloc(BC, "qnT", [128, 4, NOWN], BF16)
                    qpeT = alloc(BC, "qpeT", [64, 4, NOWN], BF16)
                    knT = alloc(BC, "knT", [128, 4, NALL], BF16)
                    Vt = alloc(BC, "Vt", [128, 16, 512], BF16)
                    r_qn, r_qpe, r_kn, r_V = Reg("qn"), Reg("qpe"), Reg("kn"), Reg("V")
                    wqrot = alloc(BC, "wqrot", [128, 3, 8, 64], BF16)
                    r_wqrot = Reg()
                    ta = alloc(BC, "ropeA2", [64, 512], F32)
                    tb = alloc(BC, "ropeB2", [64, 512], F32)
                    r_ta = Reg()
                    Et = [alloc(BC, "Et%d" % i, [128, 512], BF16) for i in range(2)]
                    r_E = [Reg(), Reg()]
                    rec = alloc(BC, "rec", [128, 512], F32)
                    r_rec = Reg()
                    wq4 = w_q_b.rearrange("(kc p) (h c) -> p kc h c", p=128, c=192)
                    for kc in range(3):
                        P.dma("pool", wqrot[:, kc, :, 0:32], wq4[:, kc, :, 160:192], W=[r_wqrot])
                        P.dma("pool", wqrot[:, kc, :, 32:64], wq4[:, kc, :, 128:160], W=[r_wqrot])
                    SCALE = 192.0 ** -0.5
                    for hg in range(2):
                        for hp in range(2):
                            wt, wr = wload(w_q_b, 3, (hg * 2 + hp) * 384, 384)
                            for hh in range(2):
                                hl = hp * 2 + hh
                                h = hg * 4 + hl
                                for t in range(2):
                                    cols = slice(t * 512, (t + 1) * 512)
                                    for kc in range(3):
                                        MM(psb[0][:, :], wt[:, kc, hh * 192:hh * 192 + 128], cqT[:, kc, cols], kc == 0, kc == 2,
                                           [r_cq, wr], [r_ps[0]])
                                    CP("act", qnT[:, hl, cols], psb[0][:, :], [r_ps[0]], [r_qn])
                                    for kc in range(3):
                                        MM(psb[1][0:64, :], wt[:, kc, hh * 192 + 128:hh * 192 + 192], cqT[:, kc, cols], kc == 0, kc == 2,
                                           [r_cq, wr], [r_ps[1]])
                                    for kc in range(3):
                                        MM(psb[2][0:64, :], wqrot[:, kc, h, :], cqT[:, kc, cols], kc == 0, kc == 2,
                                           [r_cq, r_wqrot], [r_ps[2]])
                                    gcols = slice(NOWN + t * 512, NOWN + (t + 1) * 512)
                                    TT("dve", ta[:], psb[1][0:64, :], cosT[:, gcols], ALU.mult, [r_ps[1], r_cs], [r_ta])
                                    TT("dve", tb[:], psb[2][0:64, :], sinS[:, gcols], ALU.mult, [r_ps[2], r_cs], [r_ta])
                                    TT("dve", qpeT[:, hl, cols], ta[:], tb[:], ALU.add, [r_ta], [r_qpe])
                        for hp in range(2):
                            wt, wr = wload(w_kv_b, 2, (hg * 2 + hp) * 512, 512)
                            for hh in range(2):
                                hl = hp * 2 + hh
                                for t in range(4):
                                    cols = slice(t * 512, (t + 1) * 512)
                                    q = 3 + t % 2
                                    for kc in range(2):
                                        MM(psb[q][:, :], wt[:, kc, hh * 256:hh * 256 + 128], ckvT[:, kc, cols], kc == 0, kc == 1,
                                           [r_ckv, wr], [r_ps[q]])
                                    if t % 2 == 0:
                                        CP("act", knT[:, hl, cols], psb[q][:, :], [r_ps[q]], [r_kn])
                                    else:
                                        CP("dve", knT[:, hl, cols], psb[q][:, :], [r_ps[q]], [r_kn])
                                for c in range(16):
                                    q = 5 + c % 2
                                    for kc in range(2):
                                        MM(psb[q][:, 0:128], ckvT[:, kc, c * 128:(c + 1) * 128], wt[:, kc, hh * 256 + 128:hh * 256 + 256],
                                           kc == 0, kc == 1, [r_ckv, wr], [r_ps[q]])
                                    if c % 2 == 0:
                                        CP("dve", Vt[:, c, hl * 128:(hl + 1) * 128], psb[q][:, 0:128], [r_ps[q]], [r_V])
                                    else:
                                        CP("act", Vt[:, c, hl * 128:(hl + 1) * 128], psb[q][:, 0:128], [r_ps[q]], [r_V])
                        if hg == 0:
                            tap("qnT", qnT[:, :, :], [r_qn])
                            tap("qpeT", qpeT[:, :, :], [r_qpe])
                            tap("knT", knT[:, :, :], [r_kn])
                            tap("Vt", Vt[:, :, :], [r_V])
                            stop_if("B0")
                        for hl in range(4):
                            h = hg * 4 + hl
                            for j in range(2):
                                nkc = 8 + 4 * j + 4
                                po, pm = psb[2 + j], psb[4 + j]
                                r_po, r_pm = r_ps[2 + j], r_ps[4 + j]

                                def s_mm(kc):
                                    r = kc - (8 + 4 * j)
                                    q0 = max(r, 0) * 128
                                    sb = kc % 2
                                    qs = slice(j * 512 + q0, (j + 1) * 512)
                                    MM(psb[sb][:, q0:512], knT[:, hl, kc * 128:(kc + 1) * 128], qnT[:, hl, qs], True, False,
                                       [r_kn, r_qn], [r_ps[sb]])
                                    MM(psb[sb][:, q0:512], kpeT[:, kc * 128:(kc + 1) * 128], qpeT[:, hl, qs], False, True,
                                       [r_kpe, r_qpe], [r_ps[sb]])

                                s_mm(0)
                                for kc in range(nkc):
                                    if kc + 1 < nkc:
                                        s_mm(kc + 1)
                                    r = kc - (8 + 4 * j)
                                    q0 = max(r, 0) * 128
                                    sb = kc % 2
                                    bias = pbias[:, 0:1] if kc < 8 else zeroc[:, 0:1]
                                    ACT(Et[sb][:, q0:512], psb[sb][:, q0:512], AF.Exp, [r_ps[sb], CONST], [r_E[sb]],
                                        scale=SCALE, bias=bias)
                                    if r >= 0:
                                        m0 = 384 - 128 * r + q0
                                        TT("dve", Et[sb][:, q0:512], Et[sb][:, q0:512], maskW[:, m0:m0 + 512 - q0], ALU.mult,
                                           [r_E[sb], CONST], [r_E[sb]])
                                    MM(po[:, q0:512], Vt[:, kc, hl * 128:(hl + 1) * 128], Et[sb][:, q0:512], kc == 0, kc == nkc - 1,
                                       [r_V, r_E[sb]], [r_po])
                                    MM(pm[:, q0:512], ones_b[:, :], Et[sb][:, q0:512], kc == 0, kc == nkc - 1,
                                       [CONST, r_E[sb]], [r_pm])
                                P.op("dve", lambda g: g.reciprocal(out=rec[:], in_=pm[:, :]), [r_pm], [r_rec])
                                TT("dve", attnT[:, h, j * 512:(j + 1) * 512], po[:, :], rec[:], ALU.mult, [r_po, r_rec], [r_attn])
                    tap("attnT", attnT[:, :, :], [r_attn])
                    stop_if("C1")
                    sq = alloc(BC, "sqA", [128, 512], BF16)
                    r_sq = Reg()
                    rsb = alloc(BC, "rsbA", [128, 512], F32)
                    r_rsb = Reg()
                    mo = alloc(BC, "moA", [128, 8, 512], BF16)
                    r_mo = Reg()
                    for j in range(2):
                        cols = slice(j * 512, (j + 1) * 512)
                        for h in range(8):
                            ACT(sq[:], attnT[:, h, cols], AF.Square, [r_attn], [r_sq])
                            MM(psb[0][:, :], ones_b[:, :], sq[:], h == 0, h == 7, [CONST, r_sq], [r_ps[0]])
                        ACT(rsb[:], psb[0][:, :], AF.Sqrt, [r_ps[0], CONST], [r_rsb], scale=1.0 / 1024, bias=epsc[:, 0:1])
                        P.op("dve", lambda g: g.reciprocal(out=rsb[:], in_=rsb[:]), [r_rsb], [r_rsb])
                        for h in range(8):
                            STT(mo[:, h, :], attnT[:, h, cols], pcol("mixa", h), rsb[:], ALU.mult, ALU.mult,
                                [r_attn, r_rsb, CONST], [r_mo])
                        P.dma("sp", mix_d[0:1024, cols].rearrange("(h p) n -> p h n", p=128), mo[:, :, :], R=[r_mo], W=[r_mixd])
                    P.barrier()
                    stop_if("C")
            with ExitStack() as Dp:
                def tab(name, n=32):
                    return alloc(Dp, name, [128, n], F32)
                r_tb = Reg("ssmtab")
                yact = alloc(Dp, "yact", [128, 8, NOWN], BF16)
                r_ya = Reg("yact")
                Bpad = alloc(Dp, "Bpad", [128, 2, 32, 128], BF16)
                Cpad = alloc(Dp, "Cpad", [128, 2, 32, 128], BF16)
                EC = alloc(Dp, "EC", [128, 11, 32], F32)
                ES = alloc(Dp, "ES", [128, 11, 32], F32)
                LR, LI, LDT = tab("LR"), tab("LI"), tab("LDT")
                DT, MAG, TH = tab("DT"), tab("MAG"), tab("TH")
                CS, SN = tab("CS"), tab("SN")
                NR, NI, DEN, CR, CI, T1 = tab("NR"), tab("NI"), tab("DEN"), tab("CR"), tab("CI"), tab("T1")
                SETUP = ExitStack()
                SETUP.__enter__()
                lamw = alloc(SETUP, "lamw", [64, 3, 128], F32)
                P.dma("sp", lamw[:, 0, 0:64], lam_re, W=[r_tb])
                P.dma("sp", lamw[:, 0, 64:128], lam_re, W=[r_tb])
                P.dma("sp", lamw[:, 1, 0:64], lam_im, W=[r_tb])
                P.dma("sp", lamw[:, 1, 64:128], lam_im, W=[r_tb])
                ldc = alloc(SETUP, "ldc", [64, 1], F32)
                r_ldc = Reg()
                P.dma("sp", ldc[:, :], log_dt, W=[r_ldc])
                CP("dve", lamw[:, 2, :], ldc[:, 0:1].broadcast_to([64, 128]), [r_ldc], [r_tb])
                for i, dst in enumerate((LR, LI, LDT)):
                    TR(psb[0][:, 0:64], lamw[:, i, :], ident_f[0:64, 0:64], [r_tb, CONST], [r_ps[0]])
                    CP("dve", dst[0:64, :], psb[0][0:64, 0:64:2], [r_ps[0]], [r_tb])
                    CP("dve", dst[64:128, :], psb[0][64:128, 1:64:2], [r_ps[0]], [r_tb])
                ACT(DT[:], LDT[:], AF.Exp, [r_tb], [r_tb])
                TT("dve", TH[:], LR[:], DT[:], ALU.mult, [r_tb], [r_tb])
                ACT(MAG[:], TH[:], AF.Exp, [r_tb], [r_tb])
                TT("dve", TH[:], LI[:], DT[:], ALU.mult, [r_tb], [r_tb])
                neg_sincos(SETUP, TH[:], [128, 32], SN[:], CS[:], r_tb, r_tb)
                TS("dve", CS[:], CS[:], -1.0, None, ALU.mult, None, [r_tb], [r_tb])
                TS("dve", SN[:], SN[:], -1.0, None, ALU.mult, None, [r_tb], [r_tb])
                TT("dve", NR[:], MAG[:], CS[:], ALU.mult, [r_tb], [r_tb])
                TS("dve", NR[:], NR[:], -1.0, None, ALU.add, None, [r_tb], [r_tb])
                TT("dve", NI[:], MAG[:], SN[:], ALU.mult, [r_tb], [r_tb])
                TT("dve", DEN[:], LR[:], LR[:], ALU.mult, [r_tb], [r_tb])
                TT("dve", T1[:], LI[:], LI[:], ALU.mult, [r_tb], [r_tb])
                TT("dve", DEN[:], DEN[:], T1[:], ALU.add, [r_tb], [r_tb])
                P.op("dve", lambda g: g.reciprocal(out=DEN[:], in_=DEN[:]), [r_tb], [r_tb])
                TT("dve", CR[:], NR[:], LR[:], ALU.mult, [r_tb], [r_tb])
                TT("dve", T1[:], NI[:], LI[:], ALU.mult, [r_tb], [r_tb])
                TT("dve", CR[:], CR[:], T1[:], ALU.add, [r_tb], [r_tb])
                TT("dve", CR[:], CR[:], DEN[:], ALU.mult, [r_tb], [r_tb])
                TT("dve", CI[:], NI[:], LR[:], ALU.mult, [r_tb], [r_tb])
                TT("dve", T1[:], NR[:], LI[:], ALU.mult, [r_tb], [r_tb])
                TT("dve", CI[:], CI[:], T1[:], ALU.subtract, [r_tb], [r_tb])
                TT("dve", CI[:], CI[:], DEN[:], ALU.mult, [r_tb], [r_tb])
                CP("dve", EC[:, 0, :], CS[:], [r_tb], [r_tb])
                CP("dve", ES[:, 0, :], SN[:], [r_tb], [r_tb])
                for k in range(10):
                    TT("dve", T1[:], EC[:, k, :], EC[:, k, :], ALU.mult, [r_tb], [r_tb])
                    TT("dve", NR[:], ES[:, k, :], ES[:, k, :], ALU.mult, [r_tb], [r_tb])
                    TT("dve", EC[:, k + 1, :], T1[:], NR[:], ALU.subtract, [r_tb], [r_tb])
                    TT("dve", T1[:], EC[:, k, :], ES[:, k, :], ALU.mult, [r_tb], [r_tb])
                    TS("dve", ES[:, k + 1, :], T1[:], 2.0, None, ALU.mult, None, [r_tb], [r_tb])
                Ball = alloc(SETUP, "Ball", [128, 2, 32, 16], F32)
                for ri, bsrc in enumerate((b_re, b_im)):
                    b3 = bsrc.rearrange("(j g) p c -> (g p) j c", g=2)
                    for jb in range(8):
                        P.dma("sp", Ball[:, ri, jb * 4:(jb + 1) * 4, :], b3[:, jb * 4:(jb + 1) * 4, :], W=[r_tb])
                BB = alloc(SETUP, "BB", [128, 2, 32, 16], F32)
                T2 = alloc(SETUP, "T2", [128, 32, 16], F32)
                crb = CR[:, :].unsqueeze(2).broadcast_to([128, 32, 16])
                cib = CI[:, :].unsqueeze(2).broadcast_to([128, 32, 16])
                TT("dve", BB[:, 0, :, :], Ball[:, 0, :, :], crb, ALU.mult, [r_tb], [r_tb])
                TT("dve", T2[:], Ball[:, 1, :, :], cib, ALU.mult, [r_tb], [r_tb])
                TT("dve", BB[:, 0, :, :], BB[:, 0, :, :], T2[:], ALU.subtract, [r_tb], [r_tb])
                TT("dve", BB[:, 1, :, :], Ball[:, 1, :, :], crb, ALU.mult, [r_tb], [r_tb])
                TT("dve", T2[:], Ball[:, 0, :, :], cib, ALU.mult, [r_tb], [r_tb])
                TT("dve", BB[:, 1, :, :], BB[:, 1, :, :], T2[:], ALU.add, [r_tb], [r_tb])
                Zp = alloc(SETUP, "Zp", [128, 2, 4, 128], F32)
                MS("dve", Zp[:], 0.0, [r_tb])
                MS("dve", Cpad[:], 0.0, [r_tb])
                Wc = alloc(SETUP, "Wc", [32, 2, 32, 128], F32)
                MS("dve", Wc[:], 0.0, [r_tb])
                for ri, csrc in enumerate((c_re, c_im)):
                    c4 = csrc.rearrange("(j g) c p -> g c j p", g=2)
                    for g2 in range(2):
                        P.dma("sp", Wc[g2 * 16:(g2 + 1) * 16, ri, :, g2 * 64:(g2 + 1) * 64], c4[g2], W=[r_tb])
                r_bp = Reg("bpad")
                for j in range(32):
                    base = 32 * (j % 4)
                    for ri in range(2):
                        q = (2 * j + ri) % 2
                        CP("dve", Zp[0:64, ri, j % 4, base:base + 16], BB[0:64, ri, j, :], [r_tb], [r_tb])
                        CP("dve", Zp[64:128, ri, j % 4, base + 16:base + 32], BB[64:128, ri, j, :], [r_tb], [r_tb])
                        TR(psb[q][:, 0:128], Zp[:, ri, j % 4, :], ident_f[:], [r_tb, CONST], [r_ps[q]])
                        CP("act", Bpad[:, ri, j, :], psb[q][:, 0:128], [r_ps[q]], [r_bp])
                        TR(psb[2 + q][:, 0:32], Wc[:, ri, j, :], ident_f[0:32, 0:32], [r_tb, CONST], [r_ps[2 + q]])
                        if ri == 0:
                            CP("act", Cpad[:, 0, j, base:base + 32], psb[2 + q][:, 0:32], [r_ps[2 + q]], [r_bp])
                        else:
                            ACT(Cpad[:, 1, j, base:base + 32], psb[2 + q][:, 0:32], AF.Copy, [r_ps[2 + q]], [r_bp], scale=-1.0)
                P.barrier()
                stop_if("D0")
                SETUP.close()
                MAINS = ExitStack()
                MAINS.__enter__()
                Rc = alloc(MAINS, "Rc", [128, 1024], F32)
                Rs = alloc(MAINS, "Rs", [128, 1024], F32)
                r_R = Reg("R")
                TA = alloc(MAINS, "TA", [128, 1024], F32)
                TB = alloc(MAINS, "TB", [128, 1024], F32)
                r_T = Reg("T")
                WR = alloc(MAINS, "WR", [128, 1024], F32)
                WI = alloc(MAINS, "WI", [128, 1024], F32)
                r_W = Reg("W")
                VR = alloc(MAINS, "VR", [128, 1024], F32)
                VI = alloc(MAINS, "VI", [128, 1024], F32)
                r_V2 = Reg("V2")
                XR = alloc(MAINS, "XR", [128, 1024], BF16)
                XI = alloc(MAINS, "XI", [128, 1024], BF16)
                r_X = Reg("X")
                ini = alloc(MAINS, "ini", [128, 4], F32)
                r_ini = Reg("ini")
                yv = alloc(MAINS, "yv", [128, 1024], F32)
                yw = alloc(MAINS, "yw", [128, 1024], F32)
                r_yv = Reg("yv")
                for j in range(32):
                    cc = j // 4
                    MS("dve", Rc[:, 0:1], 1.0, [r_R])
                    MS("dve", Rs[:, 0:1], 0.0, [r_R])
                    for k in range(10):
                        n = 1 << k
                        ck, sk = EC[:, k, j:j + 1], ES[:, k, j:j + 1]
                        TS("dve", TA[:, 0:n], Rs[:, 0:n], sk, None, ALU.mult, None, [r_R, r_tb], [r_T])
                        STT(Rc[:, n:2 * n], Rc[:, 0:n], ck, TA[:, 0:n], ALU.mult, ALU.subtract, [r_R, r_T, r_tb], [r_R])
                        TS("dve", TA[:, 0:n], Rc[:, 0:n], sk, None, ALU.mult, None, [r_R, r_tb], [r_T])
                        STT(Rs[:, n:2 * n], Rs[:, 0:n], ck, TA[:, 0:n], ALU.mult, ALU.add, [r_R, r_T, r_tb], [r_R])
                    magb = MAG[:, j:j + 1].broadcast_to([128, 1024])
                    for hf in range(2):
                        for t in range(2):
                            cols = slice(hf * NOWN + t * 512, hf * NOWN + (t + 1) * 512)
                            lc = slice(t * 512, (t + 1) * 512)
                            MM(psb[0][:, :], Bpad[:, 0, j, :], uT[:, cc, cols], True, True, [r_bp, r_uT], [r_ps[0]])
                            MM(psb[1][:, :], Bpad[:, 1, j, :], uT[:, cc, cols], True, True, [r_bp, r_uT], [r_ps[1]])
                            TT("dve", TA[:, lc], psb[0][:, :], Rc[:, lc], ALU.mult, [r_ps[0], r_R], [r_T])
                            TT("dve", TB[:, lc], psb[1][:, :], Rs[:, lc], ALU.mult, [r_ps[1], r_R], [r_T])
                            TT("dve", WR[:, lc], TA[:, lc], TB[:, lc], ALU.add, [r_T], [r_W])
                            TT("dve", TA[:, lc], psb[1][:, :], Rc[:, lc], ALU.mult, [r_ps[1], r_R], [r_T])
                            TT("dve", TB[:, lc], psb[0][:, :], Rs[:, lc], ALU.mult, [r_ps[0], r_R], [r_T])
                            TT("dve", WI[:, lc], TA[:, lc], TB[:, lc], ALU.subtract, [r_T], [r_W])
                        if hf == 0:
                            i_r, i_i = 0.0, 0.0
                            rdeps = [r_W, r_tb]
                        else:
                            cL, sL = EC[:, 10, j:j + 1], ES[:, 10, j:j + 1]
                            TS("dve", ini[:, 2:3], VI[:, 1023:1024], sL, None, ALU.mult, None, [r_V2, r_tb], [r_ini])
                            STT(ini[:, 0:1], VR[:, 1023:1024], cL, ini[:, 2:3], ALU.mult, ALU.subtract, [r_V2, r_ini, r_tb], [r_ini])
                            TS("dve", ini[:, 3:4], VR[:, 1023:1024], sL, None, ALU.mult, None, [r_V2, r_tb], [r_ini])
                            STT(ini[:, 1:2], VI[:, 1023:1024], cL, ini[:, 3:4], ALU.mult, ALU.add, [r_V2, r_ini, r_tb], [r_ini])
                            i_r, i_i = ini[:, 0:1], ini[:, 1:2]
                            rdeps = [r_W, r_tb, r_ini]
                        P.op("dve", lambda g: g.tensor_tensor_scan(out=VR[:, :], data0=magb, data1=WR[:, :], initial=i_r,
                                                                   op0=ALU.mult, op1=ALU.add), rdeps, [r_V2])
                        P.op("dve", lambda g: g.tensor_tensor_scan(out=VI[:, :], data0=magb, data1=WI[:, :], initial=i_i,
                                                                   op0=ALU.mult, op1=ALU.add), rdeps, [r_V2])
                    TT("dve", TA[:, :], VR[:, :], Rc[:, :], ALU.mult, [r_V2, r_R], [r_T])
                    TT("dve", TB[:, :], VI[:, :], Rs[:, :], ALU.mult, [r_V2, r_R], [r_T])
                    TT("dve", XR[:, :], TA[:, :], TB[:, :], ALU.subtract, [r_T], [r_X])
                    TT("dve", TA[:, :], VR[:, :], Rs[:, :], ALU.mult, [r_V2, r_R], [r_T])
                    TT("dve", TB[:, :], VI[:, :], Rc[:, :], ALU.mult, [r_V2, r_R], [r_T])
                    TT("dve", XI[:, :], TA[:, :], TB[:, :], ALU.add, [r_T], [r_X])
                    for t in range(2):
                        lc = slice(t * 512, (t + 1) * 512)
                        MM(psb[4 + t][:, :], Cpad[:, 0, j, :], XR[:, lc], j % 4 == 0, False, [r_bp, r_X], [r_ps[4 + t]])
                        MM(psb[4 + t][:, :], Cpad[:, 1, j, :], XI[:, lc], False, j % 4 == 3, [r_bp, r_X], [r_ps[4 + t]])
                    if j % 4 == 3:
                        for t in range(2):
                            lc = slice(t * 512, (t + 1) * 512)
                            gc = slice(NOWN + t * 512, NOWN + (t + 1) * 512)
                            STT(yv[:, lc], uT[:, cc, gc], pcol("ssmd", cc), psb[4 + t][:, :], ALU.mult, ALU.add,
                                [r_uT, CONST, r_ps[4 + t]], [r_yv])
                        ACT(yw[:, :], yv[:, :], AF.Square, [r_yv], [r_yv])
                        TS("dve", yw[:, :], yw[:, :], 0.044715, 1.0, ALU.mult, ALU.add, [r_yv], [r_yv])
                        TT("dve", yw[:, :], yw[:, :], yv[:, :], ALU.mult, [r_yv], [r_yv])
                        ACT(yw[:, :], yw[:, :], AF.Sigmoid, [r_yv], [r_yv], scale=1.5957691216057308)
                        TT("dve", yact[:, cc, :], yw[:, :], yv[:, :], ALU.mult, [r_yv], [r_ya])
                P.barrier()
                stop_if("D1")
                MAINS.close()
                so = alloc(Dp, "so", [128, 8, 512], F32)
                r_so = Reg("so")
                sgt = alloc(Dp, "sgt", [128, 512], F32)
                sqb = alloc(Dp, "sqb", [128, 512], BF16)
                r_sg = Reg("sg")
                mo = alloc(Dp, "moS", [128, 8, 512], BF16)
                r_mo = Reg()
                wts = [wload(w_glu, 8, 0, 512), wload(w_glu, 8, 512, 512)]
                for t in range(2):
                    lc = slice(t * 512, (t + 1) * 512)
                    for co in range(8):
                        wt, wr = wts[co // 4]
                        q = co % 2
                        for kc in range(8):
                            MM(psb[q][:, :], wt[:, kc, (co % 4) * 128:(co % 4 + 1) * 128], yact[:, kc, lc], kc == 0, kc == 7,
                               [wr, r_ya], [r_ps[q]])
                        ACT(sgt[:, :], psb[q][:, :], AF.Sigmoid, [r_ps[q], CONST], [r_sg], bias=pcol("bglu", co))
                        TT("dve", so[:, co, :], sgt[:, :], yact[:, co, lc], ALU.mult, [r_sg, r_ya], [r_so])
                    tap("ssmT", so[:, :, :], [r_so]) if t == 0 else None
                    for co in range(8):
                        ACT(sqb[:, :], so[:, co, :], AF.Square, [r_so], [r_sg])
                        MM(psb[2][:, :], ones_b[:, :], sqb[:, :], co == 0, co == 7, [CONST, r_sg], [r_ps[2]])
                    ACT(sgt[:, :], psb[2][:, :], AF.Sqrt, [r_ps[2], CONST], [r_sg], scale=1.0 / 1024, bias=epsc[:, 0:1])
                    P.op("dve", lambda g: g.reciprocal(out=sgt[:, :], in_=sgt[:, :]), [r_sg], [r_sg])
                    for co in range(8):
                        STT(mo[:, co, :], so[:, co, :], pcol("mixs", co), sgt[:, :], ALU.mult, ALU.mult, [r_so, r_sg, CONST], [r_mo])
                    P.dma("sp", mix_d[1024:2048, lc].rearrange("(h p) n -> p h n", p=128), mo[:, :, :], R=[r_mo], W=[r_mixd])
                P.barrier()
                stop_if("D")
        XRs = ExitStack()
        with XRs:
            x_res = alloc(XRs, "x_res", [128, 8, D], F32)
            GtH = alloc(XRs, "GtH", [128, 8, NEXP], F32)
            r_GH = Reg("G")
            r_x = [Reg("x%d" % c) for c in range(8)]
            for c in range(8):
                P.dma("sp", x_res[:, c, :], x_own[c * 128:(c + 1) * 128, :], W=[r_x[c]])

            def proj_add(actT, r_act, wdram):
                for qd in range(4):
                    wt, wr = wload(wdram, 16, qd * 512, 512)
                    for c in range(8):
                        q = c % 2
                        for kc in range(16):
                            MM(psb[q][:, :], actT[:, kc, c * 128:(c + 1) * 128], wt[:, kc, :], kc == 0, kc == 15,
                               [r_act, wr], [r_ps[q]])
                        TT("dve", x_res[:, c, qd * 512:(qd + 1) * 512], psb[q][:, :], x_res[:, c, qd * 512:(qd + 1) * 512], ALU.add,
                           [r_ps[q], r_x[c]], [r_x[c]])

            def norm_to_T(es, gain_name, dstT, r_dst, stt, r_stt, also_f32=None):
                junk = alloc(es, "nT_junk", [128, D], BF16)
                xb = alloc(es, "nT_xb", [128, D], BF16)
                r_j, r_xb = Reg(), Reg()
                for c in range(8):
                    ACT(junk[:], x_res[:, c, :], AF.Square, [r_x[c]], [r_j, r_stt], accum_out=stt[:, c:c + 1])
                    rstd_from(stt[:, c:c + 1], stt[:, 8 + c:9 + c], D, r_stt, r_stt, stt[:, 16 + c:17 + c])
                    ACT(xb[:], x_res[:, c, :], AF.Copy, [r_x[c], r_stt], [r_xb], scale=stt[:, 8 + c:9 + c])
                    for qq in range(2):
                        for j in range(8):
                            fc = qq * 8 + j
                            TR(pbt[:, j * 128:(j + 1) * 128], xb[:, fc * 128:(fc + 1) * 128], ident_b[:], [r_xb, CONST], [r_pbt])
                        for j in range(8):
                            fc = qq * 8 + j
                            if j % 2 == 0:
                                TS("dve", dstT[:, fc, c * 128:(c + 1) * 128], pbt[:, j * 128:(j + 1) * 128], pcol(gain_name, fc), None,
                                   ALU.mult, None, [r_pbt, CONST], [r_dst])
                            else:
                                ACT(dstT[:, fc, c * 128:(c + 1) * 128], pbt[:, j * 128:(j + 1) * 128], AF.Copy, [r_pbt, CONST], [r_dst],
                                    scale=pcol(gain_name, fc))

            with ExitStack() as E:
                mixT = alloc(E, "mixT", [128, 16, NOWN], BF16)
                r_mix = Reg("mix")
                P.dma("sp", mixT[:, :, :], mix_d.rearrange("(kc p) n -> p kc n", p=128), R=[r_mixd], W=[r_mix])
                proj_add(mixT, r_mix, w_out)
                tap("x1", x_res[:, :, :], r_x)
                P.barrier()
                stop_if("E")
            with ExitStack() as Fp:
                stt = alloc(Fp, "sttF", [128, 32], F32)
                r_stt = Reg()
                hxT = alloc(Fp, "hxT", [128, 16, NOWN], BF16)
                r_hx = Reg("hx")
                with ExitStack() as F1:
                    norm_to_T(F1, "xattn", hxT, r_hx, stt, r_stt)
                    P.barrier()
                memT = alloc(Fp, "memT", [128, 16, MEM], BF16)
                r_mem = Reg("memT")
                with ExitStack() as F2:
                    ms = alloc(F2, "ms", [128, D], F32)
                    mjunk = alloc(F2, "mjunk", [128, D], BF16)
                    mb = alloc(F2, "mb", [128, D], BF16)
                    r_ms, r_mb = Reg(), Reg()
                    for c in range(2):
                        P.dma("sp", ms[:], mem_d[c * 128:(c + 1) * 128, :], W=[r_ms])
                        ACT(mjunk[:], ms[:], AF.Square, [r_ms], [r_mb, r_stt], accum_out=stt[:, 24 + c:25 + c])
                        rstd_from(stt[:, 24 + c:25 + c], stt[:, 26 + c:27 + c], D, r_stt, r_stt, stt[:, 28 + c:29 + c])
                        ACT(mb[:], ms[:], AF.Copy, [r_ms, r_stt], [r_mb], scale=stt[:, 26 + c:27 + c])
                        for qq in range(2):
                            for j in range(8):
                                fc = qq * 8 + j
                                TR(pbt[:, j * 128:(j + 1) * 128], mb[:, fc * 128:(fc + 1) * 128], ident_b[:], [r_mb, CONST], [r_pbt])
                            for j in range(8):
                                fc = qq * 8 + j
                                TS("dve", memT[:, fc, c * 128:(c + 1) * 128], pbt[:, j * 128:(j + 1) * 128], pcol("memn", fc), None,
                                   ALU.mult, None, [r_pbt, CONST], [r_mem])
                    P.barrier()
                kT = alloc(Fp, "kT", [128, 16, MEM], BF16)
                r_kT = Reg("kT")
                vM = alloc(Fp, "vM", [128, 2, D], BF16)
                r_vM = Reg("vM")
                qT = alloc(Fp, "qT", [128, 4, NOWN], BF16)
                r_qT = Reg("qT")
                oT = alloc(Fp, "oT", [128, 4, NOWN], BF16)
                r_oT = Reg("oT")
                for qd in range(4):
                    wt, wr = wload(w_xk, 16, qd * 512, 512)
                    for s4 in range(4):
                        q = s4 % 2
                        for kc in range(16):
                            MM(psb[q][:, 0:MEM], wt[:, kc, s4 * 128:(s4 + 1) * 128], memT[:, kc, :], kc == 0, kc == 15, [wr, r_mem], [r_ps[q]])
                        CP("act", kT[:, qd * 4 + s4, :], psb[q][:, 0:MEM], [r_ps[q]], [r_kT])
                for qd in range(4):
                    wt, wr = wload(w_xv, 16, qd * 512, 512)
                    for c in range(2):
                        q = c % 2
                        for kc in range(16):
                            MM(psb[q][:, :], memT[:, kc, c * 128:(c + 1) * 128], wt[:, kc, :], kc == 0, kc == 15, [wr, r_mem], [r_ps[q]])
                        CP("act", vM[:, c, qd * 512:(qd + 1) * 512], psb[q][:, :], [r_ps[q]], [r_vM])
                Ex = [alloc(Fp, "Ex%d" % i, [128, 512], BF16) for i in range(2)]
                r_Ex = [Reg(), Reg()]
                recx = alloc(Fp, "recx", [128, 512], F32)
                r_recx = Reg()
                XS = 512.0 ** -0.5
                for h in range(4):
                    wt, wr = wload(w_xq, 16, h * 512, 512)
                    for s4 in range(4):
                        for t in range(2):
                            q = (s4 * 2 + t) % 2
                            for kc in range(16):
                                MM(psb[q][:, :], wt[:, kc, s4 * 128:(s4 + 1) * 128], hxT[:, kc, t * 512:(t + 1) * 512], kc == 0, kc == 15,
                                   [wr, r_hx], [r_ps[q]])
                            CP("act", qT[:, s4, t * 512:(t + 1) * 512], psb[q][:, :], [r_ps[q]], [r_qT])
                    for t in range(2):
                        cols = slice(t * 512, (t + 1) * 512)
                        for m in range(2):
                            for dc in range(4):
                                MM(psb[m][:, :], kT[:, h * 4 + dc, m * 128:(m + 1) * 128], qT[:, dc, cols], dc == 0, dc == 3,
                                   [r_kT, r_qT], [r_ps[m]])
                            ACT(Ex[m][:, :], psb[m][:, :], AF.Exp, [r_ps[m]], [r_Ex[m]], scale=XS)
                        for m in range(2):
                            MM(psb[2][:, :], ones_b[:, :], Ex[m][:, :], m == 0, m == 1, [CONST, r_Ex[m]], [r_ps[2]])
                        P.op("dve", lambda g: g.reciprocal(out=recx[:], in_=psb[2][:, :]), [r_ps[2]], [r_recx])
                        for dv in range(4):
                            q = 3 + dv % 2
                            for m in range(2):
                                MM(psb[q][:, :], vM[:, m, h * 512 + dv * 128:h * 512 + (dv + 1) * 128], Ex[m][:, :], m == 0, m == 1,
                                   [r_vM, r_Ex[m]], [r_ps[q]])
                            TT("dve", oT[:, dv, cols], psb[q][:, :], recx[:], ALU.mult, [r_ps[q], r_recx], [r_oT])
                    for qd in range(4):
                        wt, wr = wload(w_xo, 4, qd * 512, 512, r0=h * 512)
                        for c in range(8):
                            q = 5 + c % 2
                            for kc in range(4):
                                MM(psb[q][:, :], oT[:, kc, c * 128:(c + 1) * 128], wt[:, kc, :], kc == 0, kc == 3, [r_oT, wr], [r_ps[q]])
                            TT("dve", x_res[:, c, qd * 512:(qd + 1) * 512], psb[q][:, :], x_res[:, c, qd * 512:(qd + 1) * 512], ALU.add,
                               [r_ps[q], r_x[c]], [r_x[c]])
                tap("x2", x_res[:, :, :], r_x)
                P.barrier()
                stop_if("F")
            with ExitStack() as Gp:
                stt = alloc(Gp, "sttG", [128, 32], F32)
                r_stt = Reg()
                xnT = alloc(Gp, "xnT", [128, 16, NOWN], BF16)
                r_xn = Reg("xnT")
                Gt, r_G = GtH, r_GH
                bupT = alloc(Gp, "bupT", [128, 1024], F32)
                r_bup = Reg("bup")
                with ExitStack() as G1:
                    norm_to_T(G1, "ffn", xnT, r_xn, stt, r_stt)
                    wr32 = alloc(G1, "wr32", [128, 16, NEXP], F32)
                    wrh = alloc(G1, "wrh", [128, 16, NEXP], BF16)
                    wrl = alloc(G1, "wrl", [128, 16, NEXP], BF16)
                    wrd = alloc(G1, "wrd", [128, 16, NEXP], F32)
                    xgh = alloc(G1, "xgh", [128, 16, 128], BF16)
                    xgl = alloc(G1, "xgl", [128, 16, 128], BF16)
                    xgd = alloc(G1, "xgd", [128, 16, 128], F32)
                    brb = alloc(G1, "brb", [128, NEXP], F32)
                    xg = alloc(G1, "xg", [128, 16, 128], F32)
                    lg = alloc(G1, "lg", [128, NEXP], F32)
                    t8 = alloc(G1, "t8", [128, 8], F32)
                    msk = alloc(G1, "msk", [128, NEXP], F32)
                    ex = alloc(G1, "ex", [128, NEXP], F32)
                    sm = alloc(G1, "sm", [128, 4], F32)
                    braw = alloc(G1, "braw", [128, 128], F32)
                    r_w32, r_xg, r_lg, r_braw = Reg(), Reg(), Reg(), Reg()
                    P.dma("sp", wr32[:, :, :], w_router.rearrange("(kc p) n -> p kc n", p=128), W=[r_w32])
                    P.dma("sp", brb[:, :], b_router.broadcast_to([128, NEXP]), W=[r_w32])
                    r_wsp = Reg()
                    CP("dve", wrh[:], wr32[:], [r_w32], [r_wsp])
                    TT("dve", wrd[:], wr32[:], wrh[:], ALU.subtract, [r_w32, r_wsp], [r_wsp])
                    CP("dve", wrl[:], wrd[:], [r_wsp], [r_wsp])
                    for i in range(8):
                        P.dma("sp", braw[:, :], b_up[i * 128:(i + 1) * 128, :], W=[r_braw])
                        TR(psb[6][:, 0:128], braw[:, :], ident_f[:], [r_braw, CONST], [r_ps[6]])
                        CP("dve", bupT[:, i * 128:(i + 1) * 128], psb[6][:, 0:128], [r_ps[6]], [r_bup])
                    for c in range(8):
                        for qq in range(4):
                            for j in range(4):
                                fc = qq * 4 + j
                                TR(psb[qq][:, j * 128:(j + 1) * 128], x_res[:, c, fc * 128:(fc + 1) * 128], ident_f[:], [r_x[c], CONST], [r_ps[qq]])
                            for j in range(4):
                                fc = qq * 4 + j
                                TS("dve", xg[:, fc, :], psb[qq][:, j * 128:(j + 1) * 128], pcol("ffn", fc), None, ALU.mult, None,
                                   [r_ps[qq], CONST], [r_xg])
                        CP("dve", xgh[:], xg[:], [r_xg], [r_xg])
                        TT("dve", xgd[:], xg[:], xgh[:], ALU.subtract, [r_xg], [r_xg])
                        CP("dve", xgl[:], xgd[:], [r_xg], [r_xg])
                        for fc in range(16):
                            MM(psb[4][:, 0:NEXP], xgh[:, fc, :], wrh[:, fc, :], fc == 0, False, [r_xg, r_wsp], [r_ps[4]])
                            MM(psb[4][:, 0:NEXP], xgh[:, fc, :], wrl[:, fc, :], False, False, [r_xg, r_wsp], [r_ps[4]])
                            MM(psb[4][:, 0:NEXP], xgl[:, fc, :], wrh[:, fc, :], False, fc == 15, [r_xg, r_wsp], [r_ps[4]])
                        STT(lg[:], psb[4][:, 0:NEXP], stt[:, 8 + c:9 + c], brb[:], ALU.mult, ALU.add, [r_ps[4], r_stt, r_w32], [r_lg])
                        tap_lg = lg
                        P.op("dve", lambda g: g.max(out=t8[:], in_=lg[:]), [r_lg], [r_lg])
                        TS("dve", msk[:], lg[:], t8[:, 3:4], None, ALU.is_ge, None, [r_lg], [r_lg])
                        TS("dve", sm[:, 0:1], t8[:, 0:1], -1.0, None, ALU.mult, None, [r_lg], [r_lg])
                        ACT(ex[:], lg[:], AF.Exp, [r_lg], [r_lg], bias=sm[:, 0:1])
                        STT(ex[:], ex[:], 1.0, msk[:], ALU.mult, ALU.mult, [r_lg], [r_lg], accum_out=sm[:, 1:2])
                        P.op("dve", lambda g: g.reciprocal(out=sm[:, 2:3], in_=sm[:, 1:2]), [r_lg], [r_lg])
                        TS("dve", Gt[:, c, :], ex[:], sm[:, 2:3], None, ALU.mult, None, [r_lg], [r_G])
                    P.barrier()
                tap("logits", Gt[:, :, :], [r_G])
                stop_if("R")
                actT = alloc(Gp, "actT", [128, 16, NOWN], BF16)
                r_act = Reg("act")
                gb = alloc(Gp, "gb", [128, 512], F32)
                sgb = alloc(Gp, "sgb", [128, 512], F32)
                gs = alloc(Gp, "gs", [128, 2, 512], F32)
                lb = alloc(Gp, "lb", [128, 512], F32)
                r_gb, r_gs, r_lb = Reg(), Reg(), Reg()
                for e in range(NEXP if P.enabled else 0):
                    for n in range(4):
                        wg, rg = wload(w_up[e], 16, n * 512, 512)
                        wl, rl = wload(w_up[e], 16, 2048 + n * 512, 512)
                        for s4 in range(4):
                            ffc = n * 4 + s4
                            bg = bupT[:, e * 32 + ffc:e * 32 + ffc + 1]
                            bl = bupT[:, e * 32 + 16 + ffc:e * 32 + 16 + ffc + 1]
                            for t in range(2):
                                cols = slice(t * 512, (t + 1) * 512)
                                for kc in range(16):
                                    MM(psb[t][:, :], wg[:, kc, s4 * 128:(s4 + 1) * 128], xnT[:, kc, cols], kc == 0, kc == 15, [rg, r_xn], [r_ps[t]])
                                TS("dve", gb[:], psb[t][:, :], bg, 7.0, ALU.add, ALU.min, [r_ps[t], r_bup], [r_gb])
                                ACT(sgb[:], gb[:], AF.Sigmoid, [r_gb], [r_gb], scale=1.702)
                                TT("dve", gs[:, t, :], gb[:], sgb[:], ALU.mult, [r_gb], [r_gs])
                            for t in range(2):
                                cols = slice(t * 512, (t + 1) * 512)
                                for kc in range(16):
                                    MM(psb[2 + t][:, :], wl[:, kc, s4 * 128:(s4 + 1) * 128], xnT[:, kc, cols], kc == 0, kc == 15, [rl, r_xn], [r_ps[2 + t]])
                                TS("dve", lb[:], psb[2 + t][:, :], bl, 7.0, ALU.add, ALU.min, [r_ps[2 + t], r_bup], [r_lb])
                                TS("dve", lb[:], lb[:], -7.0, 1.0, ALU.max, ALU.add, [r_lb], [r_lb])
                                TT("dve", actT[:, ffc, cols], gs[:, t, :], lb[:], ALU.mult, [r_gs, r_lb], [r_act])
                    for qd in range(4):
                        wt, wr = wload(w_down[e], 16, qd * 512, 512)
                        for c in range(8):
                            q = 4 + c % 2
                            for kc in range(16):
                                MM(psb[q][:, :], actT[:, kc, c * 128:(c + 1) * 128], wt[:, kc, :], kc == 0, kc == 15, [r_act, wr], [r_ps[q]])
                            STT(x_res[:, c, qd * 512:(qd + 1) * 512], psb[q][:, :], Gt[:, c, e:e + 1], x_res[:, c, qd * 512:(qd + 1) * 512],
                                ALU.mult, ALU.add, [r_ps[q], r_G, r_x[c]], [r_x[c]])
                P.barrier()
            with ExitStack() as Hp:
                bd = alloc(Hp, "bd", [NEXP, D], F32)
                bdh = alloc(Hp, "bdh", [NEXP, D], BF16)
                bdl = alloc(Hp, "bdl", [NEXP, D], BF16)
                gT = alloc(Hp, "gT", [NEXP, 128], F32)
                gTh = alloc(Hp, "gTh", [NEXP, 128], BF16)
                gTl = alloc(Hp, "gTl", [NEXP, 128], BF16)
                gTd = alloc(Hp, "gTd", [NEXP, 128], F32)
                gbc = alloc(Hp, "gbc", [128, D], F32)
                stt = alloc(Hp, "sttH", [128, 32], F32)
                junk = alloc(Hp, "junkH", [128, D], BF16)
                yo = [alloc(Hp, "yo%d" % i, [128, D], F32) for i in range(2)]
                r_bd, r_gT, r_stt, r_j = Reg(), Reg(), Reg(), Reg()
                r_yo = [Reg(), Reg()]
                P.dma("sp", bd[:, :], b_down, W=[r_bd])
                P.dma("sp", gbc[:, :], final_norm.broadcast_to([128, D]), W=[r_bd])
                r_bds = Reg()
                CP("dve", bdh[:], bd[:], [r_bd], [r_bds])
                TT("dve", yo[0][0:NEXP, :], bd[:], bdh[:], ALU.subtract, [r_bd, r_bds], [r_yo[0]])
                CP("dve", bdl[:], yo[0][0:NEXP, :], [r_yo[0]], [r_bds])
                for c in range(8):
                    TR(psb[0][0:NEXP, 0:128], GtH[:, c, :], ident_f[:], [r_GH, CONST], [r_ps[0]])
                    CP("dve", gT[:, :], psb[0][0:NEXP, 0:128], [r_ps[0]], [r_gT])
                    CP("dve", gTh[:, :], gT[:, :], [r_gT], [r_gT])
                    TT("dve", gTd[:, :], gT[:, :], gTh[:, :], ALU.subtract, [r_gT], [r_gT])
                    CP("dve", gTl[:, :], gTd[:, :], [r_gT], [r_gT])
                    for qd in range(4):
                        q = 1 + qd % 2
                        cs = slice(qd * 512, (qd + 1) * 512)
                        MM(psb[q][:, :], gTh[:, :], bdh[:, cs], True, False, [r_gT, r_bds], [r_ps[q]])
                        MM(psb[q][:, :], gTh[:, :], bdl[:, cs], False, False, [r_gT, r_bds], [r_ps[q]])
                        MM(psb[q][:, :], gTl[:, :], bdh[:, cs], False, True, [r_gT, r_bds], [r_ps[q]])
                        TT("dve", x_res[:, c, qd * 512:(qd + 1) * 512], psb[q][:, :], x_res[:, c, qd * 512:(qd + 1) * 512], ALU.add,
                           [r_ps[q], r_x[c]], [r_x[c]])
                    ACT(junk[:], x_res[:, c, :], AF.Square, [r_x[c]], [r_j, r_stt], accum_out=stt[:, c:c + 1])
                    rstd_from(stt[:, c:c + 1], stt[:, 8 + c:9 + c], D, r_stt, r_stt, stt[:, 16 + c:17 + c])
                    s = c % 2
                    STT(yo[s][:], x_res[:, c, :], stt[:, 8 + c:9 + c], gbc[:], ALU.mult, ALU.mult, [r_x[c], r_stt, r_bd], [r_yo[s]])
                    P.dma("sp", y_d[c * 128:(c + 1) * 128, :], yo[s][:], R=[r_yo[s]])
    if P.enabled:
        P.final_wait("sp")
    P.es.close()
    return nc


def _pv(inp):
    pv = np.zeros((128, 128), np.float32)
    def put(name, vec):
        v = np.asarray(vec, np.float32).reshape(-1, 128)
        r = PV_ROWS[name]
        pv[r:r + v.shape[0]] = v
    put("attn", inp["attn_norm"][0]); put("xattn", inp["xattn_norm"][0]); put("memn", inp["mem_norm"][0])
    put("ffn", inp["ffn_norm"][0]); put("qa", inp["q_a_norm"][0]); put("kva", inp["kv_a_norm"][0])
    put("mixa", inp["mix_norm_attn"][0]); put("mixs", inp["mix_norm_ssm"][0]); put("ssmd", inp["ssm_d"][0])
    put("bglu", inp["b_glu"][0])
    return pv


def make_in_maps(inp, stop=None):
    f = lambda a: np.ascontiguousarray(np.asarray(a, np.float32))
    shared = dict(
        pv=_pv(inp), w_in=f(inp["w_in"][0]), w_q_b=f(inp["w_q_b"][0]), w_kv_b=f(inp["w_kv_b"][0]),
        lam_re=f(inp["ssm_lambda_re"][0]), lam_im=f(inp["ssm_lambda_im"][0]),
        log_dt=f(inp["ssm_log_dt"][0]).reshape(64, 1),
        b_re=f(inp["ssm_b_re"][0]), b_im=f(inp["ssm_b_im"][0]), c_re=f(inp["ssm_c_re"][0]), c_im=f(inp["ssm_c_im"][0]),
        w_glu=f(inp["w_glu"][0]), w_out=f(inp["w_out"][0]), w_xq=f(inp["w_xq"][0]), w_xk=f(inp["w_xk"][0]),
        w_xv=f(inp["w_xv"][0]), w_xo=f(inp["w_xo"][0]), w_router=f(inp["w_router"][0]),
        b_router=f(inp["b_router"][0]).reshape(1, NEXP), w_up=f(inp["w_up"][0]),
        b_up=f(inp["b_up"][0]).reshape(NEXP * 32, 128), w_down=f(inp["w_down"][0]), b_down=f(inp["b_down"][0]),
        final_norm=f(inp["final_norm"]).reshape(1, D),
    )
    if stop is not None:
        shared["w_up"] = shared["w_up"][0:1]
        shared["w_down"] = shared["w_down"][0:1]
    x = np.asarray(inp["x"], np.float32)
    mem = np.asarray(inp["mem"], np.float32)
    pos = np.asarray(inp["positions"], np.int32)
    maps = []
    for c in range(8):
        b, h = c // 2, c % 2
        m = dict(shared)
        m["x_own"] = np.ascontiguousarray(x[b, h * NOWN:(h + 1) * NOWN])
        m["x_pre"] = np.ascontiguousarray(x[b, 0:NOWN]) if h == 1 else np.zeros((NOWN, D), np.float32)
        m["pos"] = np.ascontiguousarray(np.concatenate([pos[b, 0:NOWN], pos[b, h * NOWN:(h + 1) * NOWN]]).reshape(1, NALL))
        m["pbias"] = np.full((128, 1), 0.0 if h == 1 else NEG, np.float32)
        m["mem"] = np.ascontiguousarray(mem[b])
        maps.append(m)
    return maps


def kernel(**inputs):
    nc = build()
    maps = make_in_maps(inputs)
    res = run_bass_kernel_spmd(nc, maps, core_ids=list(range(8)))
    out = np.zeros((4, SEQ, D), np.float32)
    for c in range(8):
        b, h = c // 2, c % 2
        out[b, h * NOWN:(h + 1) * NOWN] = res.results[c]["y"]
    return out
```

```python
import numpy as np
from contextlib import ExitStack
import concourse.bass as bass
import concourse.mybir as mybir
from concourse.bass_utils import run_bass_kernel_spmd

F32 = mybir.dt.float32
BF16 = mybir.dt.bfloat16
I32 = mybir.dt.int32
ALU = mybir.AluOpType
AF = mybir.ActivationFunctionType

D = 2048
SEQ = 2048
NOWN = 1024
NALL = 2048
MEM = 256
NEXP = 32
DFF = 2048
EPS = 1e-6
NEG = -30000.0
EPOCH = 20000


class Reg:
    __slots__ = ("w", "r", "dsem", "name")

    def __init__(self, name=""):
        self.w = []
        self.r = []
        self.dsem = None
        self.name = name


class Prog:
    def __init__(self, nc):
        self.nc = nc
        self.es = ExitStack()
        self.eng = {"pe": nc.tensor, "dve": nc.vector, "act": nc.scalar,
                    "pool": nc.gpsimd, "sp": nc.sync}
        self.cnt = {k: 0 for k in self.eng}
        self.epoch = {k: 0 for k in self.eng}
        self.sems = {}
        self.seen = {k: {} for k in self.eng}
        self.nsem = 0
        self.dma_events = []
        self.enabled = True
        for k in self.eng:
            self._newsem((k, 0))

    def _newsem(self, key):
        s = self.es.enter_context(self.nc.semaphore("s%d" % self.nsem))
        self.nsem += 1
        self.sems[key] = s
        return s

    def _wait(self, e, ev):
        key, val = ev
        if self.seen[e].get(key, 0) >= val:
            return
        self.eng[e].wait_ge(self.sems[key], val)
        self.seen[e][key] = val

    def _deps(self, e, R, W):
        evs = []
        for r in R:
            evs += r.w
        for w in W:
            for ev in w.w + w.r:
                if ev[0][0] == e:
                    continue
                evs.append(ev)
        for ev in evs:
            self._wait(e, ev)

    def op(self, e, fn, R=(), W=()):
        if not self.enabled:
            return None
        self._deps(e, R, W)
        inst = fn(self.eng[e])
        if self.cnt[e] >= EPOCH:
            self.epoch[e] += 1
            self.cnt[e] = 0
            self._newsem((e, self.epoch[e]))
        key = (e, self.epoch[e])
        inst.then_inc(self.sems[key], 1)
        self.cnt[e] += 1
        ev = (key, self.cnt[e])
        for w in W:
            w.w = [x for x in w.w if x[0][0] != e] + [ev]
            w.r = []
        for r in R:
            r.r = [x for x in r.r if x[0][0] != e] + [ev]
        return inst

    def dma(self, q, out, in_, R=(), W=(), sreg=None):
        if not self.enabled:
            return None
        self._deps(q, R, W)
        own = sreg if sreg is not None else (W[0] if len(W) else R[0])
        if own.dsem is None:
            key = ("d", self.nsem)
            self._newsem(key)
            own.dsem = [key, 0]
        inst = self.eng[q].dma_start(out=out, in_=in_)
        own.dsem[1] += 16
        inst.then_inc(self.sems[own.dsem[0]], 16)
        ev = (own.dsem[0], own.dsem[1])
        for w in W:
            w.w = [x for x in w.w if x[0] != ev[0]] + [ev]
            w.r = []
        for r in R:
            r.r = [x for x in r.r if x[0] != ev[0]] + [ev]
        self.dma_events.append(ev)
        return inst

    def barrier(self):
        if not self.enabled:
            return
        evs = [((k, self.epoch[k]), self.cnt[k]) for k in self.eng if self.cnt[k] > 0]
        last = {}
        for ev in self.dma_events:
            last[ev[0]] = max(last.get(ev[0], 0), ev[1])
        evs += list(last.items())
        for e in self.eng:
            for ev in evs:
                if ev[0][0] == e:
                    continue
                self._wait(e, ev)
        self.dma_events = []

    def final_wait(self, e="sp"):
        last = {}
        for ev in self.dma_events:
            last[ev[0]] = max(last.get(ev[0], 0), ev[1])
        for ev in last.items():
            self._wait(e, ev)
        for k in self.eng:
            if k != e and self.cnt[k] > 0:
                self._wait(e, ((k, self.epoch[k]), self.cnt[k]))


PV_ROWS = dict(attn=0, xattn=16, memn=32, ffn=48, qa=64, kva=67, mixa=69, mixs=77,
               ssmd=85, bglu=93)
TWO_PI = 6.283185307179586
C1 = 6.28125
C2 = TWO_PI - C1


def build(taps=(), stop=None):
    nc = bass.Bass("TRN2", target_bir_lowering=False)
    P = Prog(nc)
    NEXP_D = NEXP if stop is None else 1

    def stop_if(tag):
        if stop == tag:
            P.final_wait("sp")
            P.enabled = False

    def din(name, shape, dt=F32):
        return nc.dram_tensor(name, list(shape), dt, kind="ExternalInput").ap()

    x_own = din("x_own", [NOWN, D])
    x_pre = din("x_pre", [NOWN, D])
    pos_d = din("pos", [1, NALL], I32)
    pbias_d = din("pbias", [128, 1])
    mem_d = din("mem", [MEM, D])
    pv_d = din("pv", [128, 128])
    w_in = din("w_in", [D, 1728])
    w_q_b = din("w_q_b", [384, 1536])
    w_kv_b = din("w_kv_b", [256, 2048])
    lam_re = din("lam_re", [64, 64])
    lam_im = din("lam_im", [64, 64])
    log_dt = din("log_dt", [64, 1])
    b_re = din("b_re", [64, 64, 16])
    b_im = din("b_im", [64, 64, 16])
    c_re = din("c_re", [64, 16, 64])
    c_im = din("c_im", [64, 16, 64])
    w_glu = din("w_glu", [1024, 1024])
    w_out = din("w_out", [D, D])
    w_xq = din("w_xq", [D, D])
    w_xk = din("w_xk", [D, D])
    w_xv = din("w_xv", [D, D])
    w_xo = din("w_xo", [D, D])
    w_router = din("w_router", [D, NEXP])
    b_router = din("b_router", [1, NEXP])
    w_up = din("w_up", [NEXP_D, D, 2 * DFF])
    b_up = din("b_up", [NEXP * 32, 128])
    w_down = din("w_down", [NEXP_D, DFF, D])
    b_down = din("b_down", [NEXP, D])
    final_norm = din("final_norm", [1, D])
    ffn_g = din("ffn_g", [1, D])
    y_d = nc.dram_tensor("y", [NOWN, D], F32, kind="ExternalOutput").ap()
    tap_d = {}
    for (tn, tshape, tdt) in taps:
        tap_d[tn] = nc.dram_tensor("tap_" + tn, list(tshape), tdt, kind="ExternalOutput").ap()

    uid = [0]

    def alloc(es, name, shape, dt):
        uid[0] += 1
        return es.enter_context(nc.sbuf_tensor("%s_%d" % (name, uid[0]), list(shape), dt))

    def palloc(es, name, shape, dt):
        uid[0] += 1
        return es.enter_context(nc.psum_tensor("%s_%d" % (name, uid[0]), list(shape), dt))

    def TT(e, out, a, b, op, R, W):
        return P.op(e, lambda g: g.tensor_tensor(out=out, in0=a, in1=b, op=op), R, W)

    def TS(e, out, a, s1, s2, op0, op1, R, W):
        if op1 is None:
            return P.op(e, lambda g: g.tensor_scalar(out=out, in0=a, scalar1=s1, scalar2=None, op0=op0), R, W)
        return P.op(e, lambda g: g.tensor_scalar(out=out, in0=a, scalar1=s1, scalar2=s2, op0=op0, op1=op1), R, W)

    def STT(out, a, s, b, op0, op1, R, W, **kw):
        return P.op("dve", lambda g: g.scalar_tensor_tensor(out=out, in0=a, scalar=s, in1=b, op0=op0, op1=op1, **kw), R, W)

    def ACT(out, in_, func, R, W, **kw):
        return P.op("act", lambda g: g.activation(out=out, in_=in_, func=func, **kw), R, W)

    def MM(out, lhsT, rhs, start, stop, R, W):
        return P.op("pe", lambda g: g.matmul(out, lhsT, rhs, start=start, stop=stop), R, W)

    def TR(out, in_, ident, R, W):
        return P.op("pe", lambda g: g.transpose(out, in_, ident), R, W)

    def CP(e, out, in_, R, W):
        if e == "act":
            return P.op(e, lambda g: g.activation(out=out, in_=in_, func=AF.Copy), R, W)
        return P.op(e, lambda g: g.tensor_copy(out=out, in_=in_), R, W)

    def MS(e, ap, val, W):
        return P.op(e, lambda g: g.memset(ap, val), (), W)

    def tap(name, sb_ap, R):
        if name in tap_d:
            P.dma("sp", tap_d[name], sb_ap, R=R, sreg=Reg())

    G = ExitStack()
    with G:
        ident_f = alloc(G, "ident_f", [128, 128], F32)
        ident_b = alloc(G, "ident_b", [128, 128], BF16)
        ones_b = alloc(G, "ones_b", [128, 128], BF16)
        ones_f = alloc(G, "ones_f", [128, 128], F32)
        maskW = alloc(G, "maskW", [128, 896], BF16)
        pvT = alloc(G, "pvT", [128, 128], F32)
        pbias = alloc(G, "pbias_sb", [128, 1], F32)
        wslot = [alloc(G, "wslot%d" % i, [128, 16, 512], BF16) for i in range(3)]
        wreg = [Reg("w%d" % i) for i in range(3)]
        wctr = [0]
        CONST = Reg("const")

        def wload(dram2d, nk, c0, ncols, r0=0):
            s = wctr[0] % 3
            wctr[0] += 1
            src = dram2d[r0:r0 + nk * 128, c0:c0 + ncols].rearrange("(kc p) n -> p kc n", p=128)
            P.dma("pool", wslot[s][:, 0:nk, 0:ncols], src, W=[wreg[s]])
            return wslot[s], wreg[s]

        with ExitStack() as C0:
            it = alloc(C0, "iota_t", [128, 896], I32)
            pvr = alloc(C0, "pv_raw", [128, 128], F32)
            ps0 = palloc(C0, "ps_c0", [128, 512], F32)
            rt = Reg()
            P.op("pool", lambda g: g.iota(it[:, 0:128], pattern=[[1, 128]], base=0, channel_multiplier=-1), (), [rt])
            TS("dve", ident_f[:], it[:, 0:128], 0, None, ALU.is_equal, None, [rt], [CONST])
            TS("dve", ident_b[:], it[:, 0:128], 0, None, ALU.is_equal, None, [rt], [CONST])
            MS("dve", ones_b[:], 1.0, [CONST])
            MS("dve", ones_f[:], 1.0, [CONST])
            P.op("pool", lambda g: g.iota(it[:, :], pattern=[[1, 896]], base=-384, channel_multiplier=-1), [rt], [rt])
            TS("dve", maskW[:], it[:, :], 0, None, ALU.is_ge, None, [rt], [CONST])
            rp = Reg()
            P.dma("sp", pvr[:], pv_d, W=[rp])
            P.dma("sp", pbias[:], pbias_d, W=[CONST])
            rps = Reg()
            TR(ps0[:, 0:128], pvr[:], ident_f[:], [rp, CONST], [rps])
            CP("dve", pvT[:], ps0[:, 0:128], [rps], [CONST])
            P.barrier()

        def pcol(name, i):
            c = PV_ROWS[name] + i
            return pvT[:, c:c + 1]

        def neg_sincos(es, ang, shape, nsin, ncos, rin, rout):
            n_part = shape[0]
            kt = alloc(es, "sc_k", shape, I32)
            kf = alloc(es, "sc_kf", shape, F32)
            r1 = alloc(es, "sc_r1", shape, F32)
            r2 = alloc(es, "sc_r2", shape, F32)
            rr = Reg()
            TS("dve", r1[:], ang, 1.0 / TWO_PI, None, ALU.mult, None, [rin], [rr])
            CP("dve", kt[:], r1[:], [rr], [rr])
            CP("dve", kf[:], kt[:], [rr], [rr])
            STT(r1[:], kf[:], -C1, ang, ALU.mult, ALU.add, [rr, rin], [rr])
            STT(r2[:], kf[:], -C2, r1[:], ALU.mult, ALU.add, [rr], [rr])
            TS("dve", r1[:], r2[:], 0.0, TWO_PI, ALU.is_lt, ALU.mult, [rr], [rr])
            TT("dve", r2[:], r2[:], r1[:], ALU.add, [rr], [rr])
            TS("dve", r1[:], r2[:], TWO_PI, -TWO_PI, ALU.is_ge, ALU.mult, [rr], [rr])
            TT("dve", r2[:], r2[:], r1[:], ALU.add, [rr], [rr])
            ACT(nsin, r2[:], AF.Sin, [rr], [rout], bias=negpi[0:n_part, :])
            TS("dve", r1[:], r2[:], np.pi / 2, None, ALU.add, None, [rr], [rr])
            TS("dve", kf[:], r1[:], TWO_PI, -TWO_PI, ALU.is_ge, ALU.mult, [rr], [rr])
            TT("dve", r1[:], r1[:], kf[:], ALU.add, [rr], [rr])
            ACT(ncos, r1[:], AF.Sin, [rr], [rout], bias=negpi[0:n_part, :])

        negpi = alloc(G, "negpi", [128, 1], F32)
        MS("dve", negpi[:], -np.pi, [CONST])
        epsc = alloc(G, "epsc", [128, 1], F32)
        MS("dve", epsc[:], EPS, [CONST])

        def rstd_from(ssq_ap, out_ap, n, rin, rout, tmp_ap):
            ACT(tmp_ap, ssq_ap, AF.Sqrt, [rin], [rout], scale=1.0 / n, bias=epsc[0:ssq_ap.shape[0], :])
            P.op("dve", lambda g: g.reciprocal(out=out_ap, in_=tmp_ap), [rout], [rout])

        zeroc = alloc(G, "zeroc", [128, 1], F32)
        MS("dve", zeroc[:], 0.0, [CONST])
        mix_d = nc.dram_tensor("mix_scr", [D, NOWN], BF16, kind="Internal").ap()
        r_mixd = Reg("mixd")
        psb = [palloc(G, "psb%d" % i, [128, 512], F32) for i in range(7)]
        r_ps = [Reg("ps%d" % i) for i in range(7)]
        pbt = palloc(G, "pbt", [128, 1024], BF16)
        r_pbt = Reg("pbt")

        UT = ExitStack()
        with UT:
            uT = alloc(UT, "uT", [128, 8, NALL], BF16)
            r_uT = Reg("uT")
            ATT = ExitStack()
            with ATT:
                cqT = alloc(ATT, "cqT", [128, 3, NOWN], BF16)
                ckvT = alloc(ATT, "ckvT", [128, 2, NALL], BF16)
                kpeT = alloc(ATT, "kpeT", [64, NALL], BF16)
                cosT = alloc(ATT, "cosT", [64, NALL], F32)
                sinS = alloc(ATT, "sinS", [64, NALL], F32)
                r_cq, r_ckv, r_kpe, r_cs = Reg("cq"), Reg("ckv"), Reg("kpe"), Reg("cs")
                with ExitStack() as A0:
                    posi = alloc(A0, "posi", [64, NALL], I32)
                    ang = alloc(A0, "ang", [64, NALL], F32)
                    nsn = alloc(A0, "nsn", [64, NALL], F32)
                    idx = alloc(A0, "idx", [64, 1], I32)
                    invf = alloc(A0, "invf", [64, 1], F32)
                    ra = Reg()
                    P.dma("sp", posi[:], pos_d.broadcast_to([64, NALL]), W=[ra])
                    P.op("pool", lambda g: g.iota(idx[0:32, :], pattern=[[0, 1]], base=0, channel_multiplier=1), (), [ra])
                    P.op("pool", lambda g: g.iota(idx[32:64, :], pattern=[[0, 1]], base=0, channel_multiplier=1), (), [ra])
                    ACT(invf[:], idx[:], AF.Exp, [ra], [ra], scale=-float(np.log(10000.0)) / 32.0)
                    CP("dve", ang[:], posi[:], [ra], [ra])
                    TS("dve", ang[:], ang[:], invf[:, 0:1], None, ALU.mult, None, [ra], [ra])
                    neg_sincos(A0, ang[:], [64, NALL], nsn[:], cosT[:], ra, r_cs)
                    TS("dve", cosT[:], cosT[:], -1.0, None, ALU.mult, None, [r_cs], [r_cs])
                    CP("dve", sinS[0:32, :], nsn[0:32, :], [r_cs], [r_cs])
                    TS("dve", sinS[32:64, :], nsn[32:64, :], -1.0, None, ALU.mult, None, [r_cs], [r_cs])
                    P.barrier()
                tap("cosT", cosT[:, :], [r_cs])
                with ExitStack() as A:
                    xT = alloc(A, "xT", [128, 16, NOWN], BF16)
                    r_xT = [Reg("xT%d" % c) for c in range(8)]
                    xs = [alloc(A, "xs%d" % i, [128, D], F32) for i in range(2)]
                    xnb = [alloc(A, "xnb%d" % i, [128, D], BF16) for i in range(2)]
                    r_xs = [Reg(), Reg()]
                    r_xn = [Reg(), Reg()]
                    st = alloc(A, "statA", [128, 64], F32)
                    r_st = Reg()
                    csb = alloc(A, "csb", [128, 384], F32)
                    cjunk = alloc(A, "cjunk", [128, 384], F32)
                    cnb = alloc(A, "cnb", [128, 384], BF16)
                    r_csb, r_cnb = Reg(), Reg()
                    wrot = alloc(A, "wrotA", [128, 16, 64], BF16)
                    r_wrot = Reg()
                    ta = alloc(A, "ropeA", [64, 512], F32)
                    tb = alloc(A, "ropeB", [64, 512], F32)
                    r_ta = Reg()
                    win3 = w_in.rearrange("(kc p) n -> p kc n", p=128)
                    P.dma("pool", wrot[:, :, 0:32], win3[:, :, 672:704], W=[r_wrot])
                    P.dma("pool", wrot[:, :, 32:64], win3[:, :, 640:672], W=[r_wrot])

                    def norm_T(ncols, nchunk, stcol, gain_name, dstT, dcol0, r_dst, ps_src, r_psrc):
                        CP("act", csb[:, 0:ncols], ps_src, [r_psrc], [r_csb])
                        ACT(cjunk[:, 0:ncols], csb[:, 0:ncols], AF.Square, [r_csb], [r_st], accum_out=st[:, stcol:stcol + 1])
                        rstd_from(st[:, stcol:stcol + 1], st[:, stcol + 1:stcol + 2], ncols, r_st, r_st, st[:, stcol + 2:stcol + 3])
                        TS("dve", cnb[:, 0:ncols], csb[:, 0:ncols], st[:, stcol + 1:stcol + 2], None, ALU.mult, None,
                           [r_csb, r_st], [r_cnb])
                        for j in range(nchunk):
                            TR(pbt[:, j * 128:(j + 1) * 128], cnb[:, j * 128:(j + 1) * 128], ident_b[:], [r_cnb, CONST], [r_pbt])
                        for j in range(nchunk):
                            TS("dve", dstT[:, j, dcol0:dcol0 + 128], pbt[:, j * 128:(j + 1) * 128], pcol(gain_name, j), None,
                               ALU.mult, None, [r_pbt, CONST], [r_dst])

                    for hf in range(2):
                        for c in range(8):
                            s = c % 2
                            src = (x_pre if hf == 0 else x_own)[c * 128:(c + 1) * 128, :]
                            P.dma("sp", xs[s][:], src, W=[r_xs[s]])
                            sc = hf * 8 + c
                            ACT(xnb[s][:], xs[s][:], AF.Square, [r_xs[s]], [r_xn[s], r_st], accum_out=st[:, sc:sc + 1])
                            rstd_from(st[:, sc:sc + 1], st[:, 16 + sc:17 + sc], D, r_st, r_st, st[:, 32 + sc:33 + sc])
                            ACT(xnb[s][:], xs[s][:], AF.Copy, [r_xs[s], r_st], [r_xn[s]], scale=st[:, 16 + sc:17 + sc])
                            for q in range(2):
                                for j in range(8):
                                    fc = q * 8 + j
                                    TR(pbt[:, j * 128:(j + 1) * 128], xnb[s][:, fc * 128:(fc + 1) * 128], ident_b[:],
                                       [r_xn[s], CONST], [r_pbt])
                                for j in range(8):
                                    fc = q * 8 + j
                                    if j % 2 == 0:
                                        TS("dve", xT[:, fc, c * 128:(c + 1) * 128], pbt[:, j * 128:(j + 1) * 128],
                                           pcol("attn", fc), None, ALU.mult, None, [r_pbt, CONST], [r_xT[c]])
                                    else:
                                        ACT(xT[:, fc, c * 128:(c + 1) * 128], pbt[:, j * 128:(j + 1) * 128], AF.Copy,
                                            [r_pbt, CONST], [r_xT[c]], scale=pcol("attn", fc))
                        if hf == 1:
                            tap("xT", xT[:, :, :], r_xT)
                            wt, wr = wload(w_in, 16, 0, 384)
                            for c in range(8):
                                q = c % 2
                                for kc in range(16):
                                    MM(psb[q][:, 0:384], xT[:, kc, c * 128:(c + 1) * 128], wt[:, kc, 0:384], kc == 0, kc == 15,
                                       [r_xT[c], wr], [r_ps[q]])
                                norm_T(384, 3, 48, "qa", cqT, c * 128, r_cq, psb[q][:, 0:384], r_ps[q])
                        wt, wr = wload(w_in, 16, 384, 320)
                        for c in range(8):
                            q = c % 2
                            for kc in range(16):
                                MM(psb[q][:, 0:256], xT[:, kc, c * 128:(c + 1) * 128], wt[:, kc, 0:256], kc == 0, kc == 15,
                                   [r_xT[c], wr], [r_ps[q]])
                            norm_T(256, 2, 52, "kva", ckvT, hf * NOWN + c * 128, r_ckv, psb[q][:, 0:256], r_ps[q])
                        for t in range(2):
                            cols = slice(t * 512, (t + 1) * 512)
                            gcols = slice(hf * NOWN + t * 512, hf * NOWN + (t + 1) * 512)
                            for kc in range(16):
                                MM(psb[2][0:64, :], wt[:, kc, 256:320], xT[:, kc, cols], kc == 0, kc == 15,
                                   r_xT[4 * t:4 * t + 4] + [wr], [r_ps[2]])
                            for kc in range(16):
                                MM(psb[3][0:64, :], wrot[:, kc, :], xT[:, kc, cols], kc == 0, kc == 15,
                                   r_xT[4 * t:4 * t + 4] + [r_wrot], [r_ps[3]])
                            TT("dve", ta[:], psb[2][0:64, :], cosT[:, gcols], ALU.mult, [r_ps[2], r_cs], [r_ta])
                            TT("dve", tb[:], psb[3][0:64, :], sinS[:, gcols], ALU.mult, [r_ps[3], r_cs], [r_ta])
                            TT("dve", kpeT[:, gcols], ta[:], tb[:], ALU.add, [r_ta], [r_kpe])
                        for pc in range(2):
                            wt, wr = wload(w_in, 16, 704 + 512 * pc, 512)
                            for sc in range(4):
                                cc = pc * 4 + sc
                                for t in range(2):
                                    q = 4 + (sc * 2 + t) % 2
                                    cols = slice(t * 512, (t + 1) * 512)
                                    gcols = slice(hf * NOWN + t * 512, hf * NOWN + (t + 1) * 512)
                                    for kc in range(16):
                                        MM(psb[q][:, :], wt[:, kc, sc * 128:(sc + 1) * 128], xT[:, kc, cols], kc == 0, kc == 15,
                                           r_xT[4 * t:4 * t + 4] + [wr], [r_ps[q]])
                                    if t % 2 == 0:
                                        CP("dve", uT[:, cc, gcols], psb[q][:, :], [r_ps[q]], [r_uT])
                                    else:
                                        CP("act", uT[:, cc, gcols], psb[q][:, :], [r_ps[q]], [r_uT])
                    tap("cqT", cqT[:, :, :], [r_cq])
                    tap("ckvT", ckvT[:, :, :], [r_ckv])
                    tap("kpeT", kpeT[:, :], [r_kpe])
                    tap("uT", uT[:, :, :], [r_uT])
                    P.barrier()
                    stop_if("A")
                with ExitStack() as BC:
                    attnT = alloc(BC, "attnT", [128, 8, NOWN], BF16)
                    r_attn = Reg("attn")
                    qnT = alloc(BC, "qnT", [128, 4, NOWN], BF16)
                    qpeT = alloc(BC, "qpeT", [64, 4, NOWN], BF16)
                    knT = alloc(BC, "knT", [128, 4, NALL], BF16)
                    Vt = alloc(BC, "Vt", [128, 16, 512], BF16)
                    r_qn, r_qpe, r_kn, r_V = Reg("qn"), Reg("qpe"), Reg("kn"), Reg("V")
                    wqrot = alloc(BC, "wqrot", [128, 3, 8, 64], BF16)
                    r_wqrot = Reg()
                    ta = alloc(BC, "ropeA2", [64, 512], F32)
                    tb = alloc(BC, "ropeB2", [64, 512], F32)
                    r_ta = Reg()
                    Et = [alloc(BC, "Et%d" % i, [128, 512], BF16) for i in range(2)]
                    r_E = [Reg(), Reg()]
                    rec = alloc(BC, "rec", [128, 512], F32)
                    r_rec = Reg()
                    wq4 = w_q_b.rearrange("(kc p) (h c) -> p kc h c", p=128, c=192)
                    for kc in range(3):
                        P.dma("pool", wqrot[:, kc, :, 0:32], wq4[:, kc, :, 160:192], W=[r_wqrot])
                        P.dma("pool", wqrot[:, kc, :, 32:64], wq4[:, kc, :, 128:160], W=[r_wqrot])
                    SCALE = 192.0 ** -0.5
                    for hg in range(2):
                        for hp in range(2):
                            wt, wr = wload(w_q_b, 3, (hg * 2 + hp) * 384, 384)
                            for hh in range(2):
                                hl = hp * 2 + hh
                                h = hg * 4 + hl
                                for t in range(2):
                                    cols = slice(t * 512, (t + 1) * 512)
                                    for kc in range(3):
                                        MM(psb[0][:, :], wt[:, kc, hh * 192:hh * 192 + 128], cqT[:, kc, cols], kc == 0, kc == 2,
                                           [r_cq, wr], [r_ps[0]])
                                    CP("act", qnT[:, hl, cols], psb[0][:, :], [r_ps[0]], [r_qn])
                                    for kc in range(3):
                                        MM(psb[1][0:64, :], wt[:, kc, hh * 192 + 128:hh * 192 + 192], cqT[:, kc, cols], kc == 0, kc == 2,
                                           [r_cq, wr], [r_ps[1]])
                                    for kc in range(3):
                                        MM(psb[2][0:64, :], wqrot[:, kc, h, :], cqT[:, kc, cols], kc == 0, kc == 2,
                                           [r_cq, r_wqrot], [r_ps[2]])
                                    gcols = slice(NOWN + t * 512, NOWN + (t + 1) * 512)
                                    TT("dve", ta[:], psb[1][0:64, :], cosT[:, gcols], ALU.mult, [r_ps[1], r_cs], [r_ta])
                                    TT("dve", tb[:], psb[2][0:64, :], sinS[:, gcols], ALU.mult, [r_ps[2], r_cs], [r_ta])
                                    TT("dve", qpeT[:, hl, cols], ta[:], tb[:], ALU.add, [r_ta], [r_qpe])
                        for hp in range(2):
                            wt, wr = wload(w_kv_b, 2, (hg * 2 + hp) * 512, 512)
                            for hh in range(2):
                                hl = hp * 2 + hh
                                for t in range(4):
                                    cols = slice(t * 512, (t + 1) * 512)
                                    q = 3 + t % 2
                                    for kc in range(2):
                                        MM(psb[q][:, :], wt[:, kc, hh * 256:hh * 256 + 128], ckvT[:, kc, cols], kc == 0, kc == 1,
                                           [r_ckv, wr], [r_ps[q]])
                                    if t % 2 == 0:
                                        CP("act", knT[:, hl, cols], psb[q][:, :], [r_ps[q]], [r_kn])
                                    else:
                                        CP("dve", knT[:, hl, cols], psb[q][:, :], [r_ps[q]], [r_kn])
                                for c in range(16):
                                    q = 5 + c % 2
                                    for kc in range(2):
                                        MM(psb[q][:, 0:128], ckvT[:, kc, c * 128:(c + 1) * 128], wt[:, kc, hh * 256 + 128:hh * 256 + 256],
                                           kc == 0, kc == 1, [r_ckv, wr], [r_ps[q]])
                                    if c % 2 == 0:
                                        CP("dve", Vt[:, c, hl * 128:(hl + 1) * 128], psb[q][:, 0:128], [r_ps[q]], [r_V])
                                    else:
                                        CP("act", Vt[:, c, hl * 128:(hl + 1) * 128], psb[q][:, 0:128], [r_ps[q]], [r_V])
                        if hg == 0:
                            tap("qnT", qnT[:, :, :], [r_qn])
                            tap("qpeT", qpeT[:, :, :], [r_qpe])
                            tap("knT", knT[:, :, :], [r_kn])
                            tap("Vt", Vt[:, :, :], [r_V])
                            stop_if("B0")
                        for hl in range(4):
                            h = hg * 4 + hl
                            for j in range(2):
                                nkc = 8 + 4 * j + 4
                                po, pm = psb[2 + j], psb[4 + j]
                                r_po, r_pm = r_ps[2 + j], r_ps[4 + j]

                                def s_mm(kc):
                                    r = kc - (8 + 4 * j)
                                    q0 = max(r, 0) * 128
                                    sb = kc % 2
                                    qs = slice(j * 512 + q0, (j + 1) * 512)
                                    MM(psb[sb][:, q0:512], knT[:, hl, kc * 128:(kc + 1) * 128], qnT[:, hl, qs], True, False,
                                       [r_kn, r_qn], [r_ps[sb]])
                                    MM(psb[sb][:, q0:512], kpeT[:, kc * 128:(kc + 1) * 128], qpeT[:, hl, qs], False, True,
                                       [r_kpe, r_qpe], [r_ps[sb]])

                                s_mm(0)
                                for kc in range(nkc):
                                    if kc + 1 < nkc:
                                        s_mm(kc + 1)
                                    r = kc - (8 + 4 * j)
                                    q0 = max(r, 0) * 128
                                    sb = kc % 2
                                    bias = pbias[:, 0:1] if kc < 8 else zeroc[:, 0:1]
                                    ACT(Et[sb][:, q0:512], psb[sb][:, q0:512], AF.Exp, [r_ps[sb], CONST], [r_E[sb]],
                                        scale=SCALE, bias=bias)
                                    if r >= 0:
                                        m0 = 384 - 128 * r + q0
                                        TT("dve", Et[sb][:, q0:512], Et[sb][:, q0:512], maskW[:, m0:m0 + 512 - q0], ALU.mult,
                                           [r_E[sb], CONST], [r_E[sb]])
                                    MM(po[:, q0:512], Vt[:, kc, hl * 128:(hl + 1) * 128], Et[sb][:, q0:512], kc == 0, kc == nkc - 1,
                                       [r_V, r_E[sb]], [r_po])
                                    MM(pm[:, q0:512], ones_b[:, :], Et[sb][:, q0:512], kc == 0, kc == nkc - 1,
                                       [CONST, r_E[sb]], [r_pm])
                                P.op("dve", lambda g: g.reciprocal(out=rec[:], in_=pm[:, :]), [r_pm], [r_rec])
                                TT("dve", attnT[:, h, j * 512:(j + 1) * 512], po[:, :], rec[:], ALU.mult, [r_po, r_rec], [r_attn])
                    tap("attnT", attnT[:, :, :], [r_attn])
                    stop_if("C1")
                    sq = alloc(BC, "sqA", [128, 512], BF16)
                    r_sq = Reg()
                    rsb = alloc(BC, "rsbA", [128, 512], F32)
                    r_rsb = Reg()
                    mo = alloc(BC, "moA", [128, 8, 512], BF16)
                    r_mo = Reg()
                    for j in range(2):
                        cols = slice(j * 512, (j + 1) * 512)
                        for h in range(8):
                            ACT(sq[:], attnT[:, h, cols], AF.Square, [r_attn], [r_sq])
                            MM(psb[0][:, :], ones_b[:, :], sq[:], h == 0, h == 7, [CONST, r_sq], [r_ps[0]])
                        ACT(rsb[:], psb[0][:, :], AF.Sqrt, [r_ps[0], CONST], [r_rsb], scale=1.0 / 1024, bias=epsc[:, 0:1])
                        P.op("dve", lambda g: g.reciprocal(out=rsb[:], in_=rsb[:]), [r_rsb], [r_rsb])
                        for h in range(8):
                            STT(mo[:, h, :], attnT[:, h, cols], pcol("mixa", h), rsb[:], ALU.mult, ALU.mult,
                                [r_attn, r_rsb, CONST], [r_mo])
                        P.dma("sp", mix_d[0:1024, cols].rearrange("(h p) n -> p h n", p=128), mo[:, :, :], R=[r_mo], W=[r_mixd])
                    P.barrier()
                    stop_if("C")
            with ExitStack() as Dp:
                def tab(name, n=32):
                    return alloc(Dp, name, [128, n], F32)
                r_tb = Reg("ssmtab")
                yact = alloc(Dp, "yact", [128, 8, NOWN], BF16)
                r_ya = Reg("yact")
                Bpad = alloc(Dp, "Bpad", [128, 2, 32, 128], BF16)
                Cpad = alloc(Dp, "Cpad", [128, 2, 32, 128], BF16)
                EC = alloc(Dp, "EC", [128, 11, 32], F32)
                ES = alloc(Dp, "ES", [128, 11, 32], F32)
                LR, LI, LDT = tab("LR"), tab("LI"), tab("LDT")
                DT, MAG, TH = tab("DT"), tab("MAG"), tab("TH")
                CS, SN = tab("CS"), tab("SN")
                NR, NI, DEN, CR, CI, T1 = tab("NR"), tab("NI"), tab("DEN"), tab("CR"), tab("CI"), tab("T1")
                SETUP = ExitStack()
                SETUP.__enter__()
                lamw = alloc(SETUP, "lamw", [64, 3, 128], F32)
                P.dma("sp", lamw[:, 0, 0:64], lam_re, W=[r_tb])
                P.dma("sp", lamw[:, 0, 64:128], lam_re, W=[r_tb])
                P.dma("sp", lamw[:, 1, 0:64], lam_im, W=[r_tb])
                P.dma("sp", lamw[:, 1, 64:128], lam_im, W=[r_tb])
                ldc = alloc(SETUP, "ldc", [64, 1], F32)
                r_ldc = Reg()
                P.dma("sp", ldc[:, :], log_dt, W=[r_ldc])
                CP("dve", lamw[:, 2, :], ldc[:, 0:1].broadcast_to([64, 128]), [r_ldc], [r_tb])
                for i, dst in enumerate((LR, LI, LDT)):
                    TR(psb[0][:, 0:64], lamw[:, i, :], ident_f[0:64, 0:64], [r_tb, CONST], [r_ps[0]])
                    CP("dve", dst[0:64, :], psb[0][0:64, 0:64:2], [r_ps[0]], [r_tb])
                    CP("dve", dst[64:128, :], psb[0][64:128, 1:64:2], [r_ps[0]], [r_tb])
                ACT(DT[:], LDT[:], AF.Exp, [r_tb], [r_tb])
                TT("dve", TH[:], LR[:], DT[:], ALU.mult, [r_tb], [r_tb])
                ACT(MAG[:], TH[:], AF.Exp, [r_tb], [r_tb])
                TT("dve", TH[:], LI[:], DT[:], ALU.mult, [r_tb], [r_tb])
                neg_sincos(SETUP, TH[:], [128, 32], SN[:], CS[:], r_tb, r_tb)
                TS("dve", CS[:], CS[:], -1.0, None, ALU.mult, None, [r_tb], [r_tb])
                TS("dve", SN[:], SN[:], -1.0, None, ALU.mult, None, [r_tb], [r_tb])
                TT("dve", NR[:], MAG[:], CS[:], ALU.mult, [r_tb], [r_tb])
                TS("dve", NR[:], NR[:], -1.0, None, ALU.add, None, [r_tb], [r_tb])
                TT("dve", NI[:], MAG[:], SN[:], ALU.mult, [r_tb], [r_tb])
                TT("dve", DEN[:], LR[:], LR[:], ALU.mult, [r_tb], [r_tb])
                TT("dve", T1[:], LI[:], LI[:], ALU.mult, [r_tb], [r_tb])
                TT("dve", DEN[:], DEN[:], T1[:], ALU.add, [r_tb], [r_tb])
                P.op("dve", lambda g: g.reciprocal(out=DEN[:], in_=DEN[:]), [r_tb], [r_tb])
                TT("dve", CR[:], NR[:], LR[:], ALU.mult, [r_tb], [r_tb])
                TT("dve", T1[:], NI[:], LI[:], ALU.mult, [r_tb], [r_tb])
                TT("dve", CR[:], CR[:], T1[:], ALU.add, [r_tb], [r_tb])
                TT("dve", CR[:], CR[:], DEN[:], ALU.mult, [r_tb], [r_tb])
                TT("dve", CI[:], NI[:], LR[:], ALU.mult, [r_tb], [r_tb])
                TT("dve", T1[:], NR[:], LI[:], ALU.mult, [r_tb], [r_tb])
                TT("dve", CI[:], CI[:], T1[:], ALU.subtract, [r_tb], [r_tb])
                TT("dve", CI[:], CI[:], DEN[:], ALU.mult, [r_tb], [r_tb])
                CP("dve", EC[:, 0, :], CS[:], [r_tb], [r_tb])
                CP("dve", ES[:, 0, :], SN[:], [r_tb], [r_tb])
                for k in range(10):
                    TT("dve", T1[:], EC[:, k, :], EC[:, k, :], ALU.mult, [r_tb], [r_tb])
                    TT("dve", NR[:], ES[:, k, :], ES[:, k, :], ALU.mult, [r_tb], [r_tb])
                    TT("dve", EC[:, k + 1, :], T1[:], NR[:], ALU.subtract, [r_tb], [r_tb])
                    TT("dve", T1[:], EC[:, k, :], ES[:, k, :], ALU.mult, [r_tb], [r_tb])
                    TS("dve", ES[:, k + 1, :], T1[:], 2.0, None, ALU.mult, None, [r_tb], [r_tb])
                Ball = alloc(SETUP, "Ball", [128, 2, 32, 16], F32)
                for ri, bsrc in enumerate((b_re, b_im)):
                    b3 = bsrc.rearrange("(j g) p c -> (g p) j c", g=2)
                    for jb in range(8):
                        P.dma("sp", Ball[:, ri, jb * 4:(jb + 1) * 4, :], b3[:, jb * 4:(jb + 1) * 4, :], W=[r_tb])
                BB = alloc(SETUP, "BB", [128, 2, 32, 16], F32)
                T2 = alloc(SETUP, "T2", [128, 32, 16], F32)
                crb = CR[:, :].unsqueeze(2).broadcast_to([128, 32, 16])
                cib = CI[:, :].unsqueeze(2).broadcast_to([128, 32, 16])
                TT("dve", BB[:, 0, :, :], Ball[:, 0, :, :], crb, ALU.mult, [r_tb], [r_tb])
                TT("dve", T2[:], Ball[:, 1, :, :], cib, ALU.mult, [r_tb], [r_tb])
                TT("dve", BB[:, 0, :, :], BB[:, 0, :, :], T2[:], ALU.subtract, [r_tb], [r_tb])
                TT("dve", BB[:, 1, :, :], Ball[:, 1, :, :], crb, ALU.mult, [r_tb], [r_tb])
                TT("dve", T2[:], Ball[:, 0, :, :], cib, ALU.mult, [r_tb], [r_tb])
                TT("dve", BB[:, 1, :, :], BB[:, 1, :, :], T2[:], ALU.add, [r_tb], [r_tb])
                Zp = alloc(SETUP, "Zp", [128, 2, 4, 128], F32)
                MS("dve", Zp[:], 0.0, [r_tb])
                MS("dve", Cpad[:], 0.0, [r_tb])
                Wc = alloc(SETUP, "Wc", [32, 2, 32, 128], F32)
                MS("dve", Wc[:], 0.0, [r_tb])
                for ri, csrc in enumerate((c_re, c_im)):
                    c4 = csrc.rearrange("(j g) c p -> g c j p", g=2)
                    for g2 in range(2):
                        P.dma("sp", Wc[g2 * 16:(g2 + 1) * 16, ri, :, g2 * 64:(g2 + 1) * 64], c4[g2], W=[r_tb])
                r_bp = Reg("bpad")
                for j in range(32):
                    base = 32 * (j % 4)
                    for ri in range(2):
                        q = (2 * j + ri) % 2
                        CP("dve", Zp[0:64, ri, j % 4, base:base + 16], BB[0:64, ri, j, :], [r_tb], [r_tb])
                        CP("dve", Zp[64:128, ri, j % 4, base + 16:base + 32], BB[64:128, ri, j, :], [r_tb], [r_tb])
                        TR(psb[q][:, 0:128], Zp[:, ri, j % 4, :], ident_f[:], [r_tb, CONST], [r_ps[q]])
                        CP("act", Bpad[:, ri, j, :], psb[q][:, 0:128], [r_ps[q]], [r_bp])
                        TR(psb[2 + q][:, 0:32], Wc[:, ri, j, :], ident_f[0:32, 0:32], [r_tb, CONST], [r_ps[2 + q]])
                        if ri == 0:
                            CP("act", Cpad[:, 0, j, base:base + 32], psb[2 + q][:, 0:32], [r_ps[2 + q]], [r_bp])
                        else:
                            ACT(Cpad[:, 1, j, base:base + 32], psb[2 + q][:, 0:32], AF.Copy, [r_ps[2 + q]], [r_bp], scale=-1.0)
                P.barrier()
                stop_if("D0")
                SETUP.close()
                MAINS = ExitStack()
                MAINS.__enter__()
                Rc = alloc(MAINS, "Rc", [128, 1024], F32)
                Rs = alloc(MAINS, "Rs", [128, 1024], F32)
                r_R = Reg("R")
                TA = alloc(MAINS, "TA", [128, 1024], F32)
                TB = alloc(MAINS, "TB", [128, 1024], F32)
                r_T = Reg("T")
                WR = alloc(MAINS, "WR", [128, 1024], F32)
                WI = alloc(MAINS, "WI", [128, 1024], F32)
                r_W = Reg("W")
                VR = alloc(MAINS, "VR", [128, 1024], F32)
                VI = alloc(MAINS, "VI", [128, 1024], F32)
                r_V2 = Reg("V2")
                XR = alloc(MAINS, "XR", [128, 1024], BF16)
                XI = alloc(MAINS, "XI", [128, 1024], BF16)
                r_X = Reg("X")
                ini = alloc(MAINS, "ini", [128, 4], F32)
                r_ini = Reg("ini")
                yv = alloc(MAINS, "yv", [128, 1024], F32)
                yw = alloc(MAINS, "yw", [128, 1024], F32)
                r_yv = Reg("yv")
                for j in range(32):
                    cc = j // 4
                    MS("dve", Rc[:, 0:1], 1.0, [r_R])
                    MS("dve", Rs[:, 0:1], 0.0, [r_R])
                    for k in range(10):
                        n = 1 << k
                        ck, sk = EC[:, k, j:j + 1], ES[:, k, j:j + 1]
                        TS("dve", TA[:, 0:n], Rs[:, 0:n], sk, None, ALU.mult, None, [r_R, r_tb], [r_T])
                        STT(Rc[:, n:2 * n], Rc[:, 0:n], ck, TA[:, 0:n], ALU.mult, ALU.subtract, [r_R, r_T, r_tb], [r_R])
                        TS("dve", TA[:, 0:n], Rc[:, 0:n], sk, None, ALU.mult, None, [r_R, r_tb], [r_T])
                        STT(Rs[:, n:2 * n], Rs[:, 0:n], ck, TA[:, 0:n], ALU.mult, ALU.add, [r_R, r_T, r_tb], [r_R])
                    magb = MAG[:, j:j + 1].broadcast_to([128, 1024])
                    for hf in range(2):
                        for t in range(2):
                            cols = slice(hf * NOWN + t * 512, hf * NOWN + (t + 1) * 512)
                            lc = slice(t * 512, (t + 1) * 512)
                            MM(psb[0][:, :], Bpad[:, 0, j, :], uT[:, cc, cols], True, True, [r_bp, r_uT], [r_ps[0]])
                            MM(psb[1][:, :], Bpad[:, 1, j, :], uT[:, cc, cols], True, True, [r_bp, r_uT], [r_ps[1]])
                            TT("dve", TA[:, lc], psb[0][:, :], Rc[:, lc], ALU.mult, [r_ps[0], r_R], [r_T])
                            TT("dve", TB[:, lc], psb[1][:, :], Rs[:, lc], ALU.mult, [r_ps[1], r_R], [r_T])
                            TT("dve", WR[:, lc], TA[:, lc], TB[:, lc], ALU.add, [r_T], [r_W])
                            TT("dve", TA[:, lc], psb[1][:, :], Rc[:, lc], ALU.mult, [r_ps[1], r_R], [r_T])
                            TT("dve", TB[:, lc], psb[0][:, :], Rs[:, lc], ALU.mult, [r_ps[0], r_R], [r_T])
                            TT("dve", WI[:, lc], TA[:, lc], TB[:, lc], ALU.subtract, [r_T], [r_W])
                        if hf == 0:
                            i_r, i_i = 0.0, 0.0
                            rdeps = [r_W, r_tb]
                        else:
                            cL, sL = EC[:, 10, j:j + 1], ES[:, 10, j:j + 1]
                            TS("dve", ini[:, 2:3], VI[:, 1023:1024], sL, None, ALU.mult, None, [r_V2, r_tb], [r_ini])
                            STT(ini[:, 0:1], VR[:, 1023:1024], cL, ini[:, 2:3], ALU.mult, ALU.subtract, [r_V2, r_ini, r_tb], [r_ini])
                            TS("dve", ini[:, 3:4], VR[:, 1023:1024], sL, None, ALU.mult, None, [r_V2, r_tb], [r_ini])
                            STT(ini[:, 1:2], VI[:, 1023:1024], cL, ini[:, 3:4], ALU.mult, ALU.add, [r_V2, r_ini, r_tb], [r_ini])
                            i_r, i_i = ini[:, 0:1], ini[:, 1:2]
                            rdeps = [r_W, r_tb, r_ini]
                        P.op("dve", lambda g: g.tensor_tensor_scan(out=VR[:, :], data0=magb, data1=WR[:, :], initial=i_r,
                                                                   op0=ALU.mult, op1=ALU.add), rdeps, [r_V2])
                        P.op("dve", lambda g: g.tensor_tensor_scan(out=VI[:, :], data0=magb, data1=WI[:, :], initial=i_i,
                                                                   op0=ALU.mult, op1=ALU.add), rdeps, [r_V2])
                    TT("dve", TA[:, :], VR[:, :], Rc[:, :], ALU.mult, [r_V2, r_R], [r_T])
                    TT("dve", TB[:, :], VI[:, :], Rs[:, :], ALU.mult, [r_V2, r_R], [r_T])
                    TT("dve", XR[:, :], TA[:, :], TB[:, :], ALU.subtract, [r_T], [r_X])
                    TT("dve", TA[:, :], VR[:, :], Rs[:, :], ALU.mult, [r_V2, r_R], [r_T])
                    TT("dve", TB[:, :], VI[:, :], Rc[:, :], ALU.mult, [r_V2, r_R], [r_T])
                    TT("dve", XI[:, :], TA[:, :], TB[:, :], ALU.add, [r_T], [r_X])
                    for t in range(2):
                        lc = slice(t * 512, (t + 1) * 512)
                        MM(psb[4 + t][:, :], Cpad[:, 0, j, :], XR[:, lc], j % 4 == 0, False, [r_bp, r_X], [r_ps[4 + t]])
                        MM(psb[4 + t][:, :], Cpad[:, 1, j, :], XI[:, lc], False, j % 4 == 3, [r_bp, r_X], [r_ps[4 + t]])
                    if j % 4 == 3:
                        for t in range(2):
                            lc = slice(t * 512, (t + 1) * 512)
                            gc = slice(NOWN + t * 512, NOWN + (t + 1) * 512)
                            STT(yv[:, lc], uT[:, cc, gc], pcol("ssmd", cc), psb[4 + t][:, :], ALU.mult, ALU.add,
                                [r_uT, CONST, r_ps[4 + t]], [r_yv])
                        ACT(yw[:, :], yv[:, :], AF.Square, [r_yv], [r_yv])
                        TS("dve", yw[:, :], yw[:, :], 0.044715, 1.0, ALU.mult, ALU.add, [r_yv], [r_yv])
                        TT("dve", yw[:, :], yw[:, :], yv[:, :], ALU.mult, [r_yv], [r_yv])
                        ACT(yw[:, :], yw[:, :], AF.Sigmoid, [r_yv], [r_yv], scale=1.5957691216057308)
                        TT("dve", yact[:, cc, :], yw[:, :], yv[:, :], ALU.mult, [r_yv], [r_ya])
                P.barrier()
                stop_if("D1")
                MAINS.close()
                so = alloc(Dp, "so", [128, 8, 512], F32)
                r_so = Reg("so")
                sgt = alloc(Dp, "sgt", [128, 512], F32)
                sqb = alloc(Dp, "sqb", [128, 512], BF16)
                r_sg = Reg("sg")
                mo = alloc(Dp, "moS", [128, 8, 512], BF16)
                r_mo = Reg()
                wts = [wload(w_glu, 8, 0, 512), wload(w_glu, 8, 512, 512)]
                for t in range(2):
                    lc = slice(t * 512, (t + 1) * 512)
                    for co in range(8):
                        wt, wr = wts[co // 4]
                        q = co % 2
                        for kc in range(8):
                            MM(psb[q][:, :], wt[:, kc, (co % 4) * 128:(co % 4 + 1) * 128], yact[:, kc, lc], kc == 0, kc == 7,
                               [wr, r_ya], [r_ps[q]])
                        ACT(sgt[:, :], psb[q][:, :], AF.Sigmoid, [r_ps[q], CONST], [r_sg], bias=pcol("bglu", co))
                        TT("dve", so[:, co, :], sgt[:, :], yact[:, co, lc], ALU.mult, [r_sg, r_ya], [r_so])
                    tap("ssmT", so[:, :, :], [r_so]) if t == 0 else None
                    for co in range(8):
                        ACT(sqb[:, :], so[:, co, :], AF.Square, [r_so], [r_sg])
                        MM(psb[2][:, :], ones_b[:, :], sqb[:, :], co == 0, co == 7, [CONST, r_sg], [r_ps[2]])
                    ACT(sgt[:, :], psb[2][:, :], AF.Sqrt, [r_ps[2], CONST], [r_sg], scale=1.0 / 1024, bias=epsc[:, 0:1])
                    P.op("dve", lambda g: g.reciprocal(out=sgt[:, :], in_=sgt[:, :]), [r_sg], [r_sg])
                    for co in range(8):
                        STT(mo[:, co, :], so[:, co, :], pcol("mixs", co), sgt[:, :], ALU.mult, ALU.mult, [r_so, r_sg, CONST], [r_mo])
                    P.dma("sp", mix_d[1024:2048, lc].rearrange("(h p) n -> p h n", p=128), mo[:, :, :], R=[r_mo], W=[r_mixd])
                P.barrier()
                stop_if("D")
        XRs = ExitStack()
        with XRs:
            x_res = alloc(XRs, "x_res", [128, 8, D], F32)
            GtH = alloc(XRs, "GtH", [128, 8, NEXP], F32)
            r_GH = Reg("G")
            r_x = [Reg("x%d" % c) for c in range(8)]
            for c in range(8):
                P.dma("sp", x_res[:, c, :], x_own[c * 128:(c + 1) * 128, :], W=[r_x[c]])

            def proj_add(actT, r_act, wdram):
                for qd in range(4):
                    wt, wr = wload(wdram, 16, qd * 512, 512)
                    for c in range(8):
                        q = c % 2
                        for kc in range(16):
                            MM(psb[q][:, :], actT[:, kc, c * 128:(c + 1) * 128], wt[:, kc, :], kc == 0, kc == 15,
                               [r_act, wr], [r_ps[q]])
                        TT("dve", x_res[:, c, qd * 512:(qd + 1) * 512], psb[q][:, :], x_res[:, c, qd * 512:(qd + 1) * 512], ALU.add,
                           [r_ps[q], r_x[c]], [r_x[c]])

            def norm_to_T(es, gain_name, dstT, r_dst, stt, r_stt, also_f32=None):
                junk = alloc(es, "nT_junk", [128, D], BF16)
                xb = alloc(es, "nT_xb", [128, D], BF16)
                r_j, r_xb = Reg(), Reg()
                for c in range(8):
                    ACT(junk[:], x_res[:, c, :], AF.Square, [r_x[c]], [r_j, r_stt], accum_out=stt[:, c:c + 1])
                    rstd_from(stt[:, c:c + 1], stt[:, 8 + c:9 + c], D, r_stt, r_stt, stt[:, 16 + c:17 + c])
                    ACT(xb[:], x_res[:, c, :], AF.Copy, [r_x[c], r_stt], [r_xb], scale=stt[:, 8 + c:9 + c])
                    for qq in range(2):
                        for j in range(8):
                            fc = qq * 8 + j
                            TR(pbt[:, j * 128:(j + 1) * 128], xb[:, fc * 128:(fc + 1) * 128], ident_b[:], [r_xb, CONST], [r_pbt])
                        for j in range(8):
                            fc = qq * 8 + j
                            if j % 2 == 0:
                                TS("dve", dstT[:, fc, c * 128:(c + 1) * 128], pbt[:, j * 128:(j + 1) * 128], pcol(gain_name, fc), None,
                                   ALU.mult, None, [r_pbt, CONST], [r_dst])
                            else:
                                ACT(dstT[:, fc, c * 128:(c + 1) * 128], pbt[:, j * 128:(j + 1) * 128], AF.Copy, [r_pbt, CONST], [r_dst],
                                    scale=pcol(gain_name, fc))

            with ExitStack() as E:
                mixT = alloc(E, "mixT", [128, 16, NOWN], BF16)
                r_mix = Reg("mix")
                P.dma("sp", mixT[:, :, :], mix_d.rearrange("(kc p) n -> p kc n", p=128), R=[r_mixd], W=[r_mix])
                proj_add(mixT, r_mix, w_out)
                tap("x1", x_res[:, :, :], r_x)
                P.barrier()
                stop_if("E")
            with ExitStack() as Fp:
                stt = alloc(Fp, "sttF", [128, 32], F32)
                r_stt = Reg()
                hxT = alloc(Fp, "hxT", [128, 16, NOWN], BF16)
                r_hx = Reg("hx")
                with ExitStack() as F1:
                    norm_to_T(F1, "xattn", hxT, r_hx, stt, r_stt)
                    P.barrier()
                memT = alloc(Fp, "memT", [128, 16, MEM], BF16)
                r_mem = Reg("memT")
                with ExitStack() as F2:
                    ms = alloc(F2, "ms", [128, D], F32)
                    mjunk = alloc(F2, "mjunk", [128, D], BF16)
                    mb = alloc(F2, "mb", [128, D], BF16)
                    r_ms, r_mb = Reg(), Reg()
                    for c in range(2):
                        P.dma("sp", ms[:], mem_d[c * 128:(c + 1) * 128, :], W=[r_ms])
                        ACT(mjunk[:], ms[:], AF.Square, [r_ms], [r_mb, r_stt], accum_out=stt[:, 24 + c:25 + c])
                        rstd_from(stt[:, 24 + c:25 + c], stt[:, 26 + c:27 + c], D, r_stt, r_stt, stt[:, 28 + c:29 + c])
                        ACT(mb[:], ms[:], AF.Copy, [r_ms, r_stt], [r_mb], scale=stt[:, 26 + c:27 + c])
                        for qq in range(2):
                            for j in range(8):
                                fc = qq * 8 + j
                                TR(pbt[:, j * 128:(j + 1) * 128], mb[:, fc * 128:(fc + 1) * 128], ident_b[:], [r_mb, CONST], [r_pbt])
                            for j in range(8):
                                fc = qq * 8 + j
                                TS("dve", memT[:, fc, c * 128:(c + 1) * 128], pbt[:, j * 128:(j + 1) * 128], pcol("memn", fc), None,
                                   ALU.mult, None, [r_pbt, CONST], [r_mem])
                    P.barrier()
                kT = alloc(Fp, "kT", [128, 16, MEM], BF16)
                r_kT = Reg("kT")
                vM = alloc(Fp, "vM", [128, 2, D], BF16)
                r_vM = Reg("vM")
                qT = alloc(Fp, "qT", [128, 4, NOWN], BF16)
                r_qT = Reg("qT")
                oT = alloc(Fp, "oT", [128, 4, NOWN], BF16)
                r_oT = Reg("oT")
                for qd in range(4):
                    wt, wr = wload(w_xk, 16, qd * 512, 512)
                    for s4 in range(4):
                        q = s4 % 2
                        for kc in range(16):
                            MM(psb[q][:, 0:MEM], wt[:, kc, s4 * 128:(s4 + 1) * 128], memT[:, kc, :], kc == 0, kc == 15, [wr, r_mem], [r_ps[q]])
                        CP("act", kT[:, qd * 4 + s4, :], psb[q][:, 0:MEM], [r_ps[q]], [r_kT])
                for qd in range(4):
                    wt, wr = wload(w_xv, 16, qd * 512, 512)
                    for c in range(2):
                        q = c % 2
                        for kc in range(16):
                            MM(psb[q][:, :], memT[:, kc, c * 128:(c + 1) * 128], wt[:, kc, :], kc == 0, kc == 15, [wr, r_mem], [r_ps[q]])
                        CP("act", vM[:, c, qd * 512:(qd + 1) * 512], psb[q][:, :], [r_ps[q]], [r_vM])
                Ex = [alloc(Fp, "Ex%d" % i, [128, 512], BF16) for i in range(2)]
                r_Ex = [Reg(), Reg()]
                recx = alloc(Fp, "recx", [128, 512], F32)
                r_recx = Reg()
                XS = 512.0 ** -0.5
                for h in range(4):
                    wt, wr = wload(w_xq, 16, h * 512, 512)
                    for s4 in range(4):
                        for t in range(2):
                            q = (s4 * 2 + t) % 2
                            for kc in range(16):
                                MM(psb[q][:, :], wt[:, kc, s4 * 128:(s4 + 1) * 128], hxT[:, kc, t * 512:(t + 1) * 512], kc == 0, kc == 15,
                                   [wr, r_hx], [r_ps[q]])
                            CP("act", qT[:, s4, t * 512:(t + 1) * 512], psb[q][:, :], [r_ps[q]], [r_qT])
                    for t in range(2):
                        cols = slice(t * 512, (t + 1) * 512)
                        for m in range(2):
                            for dc in range(4):
                                MM(psb[m][:, :], kT[:, h * 4 + dc, m * 128:(m + 1) * 128], qT[:, dc, cols], dc == 0, dc == 3,
                                   [r_kT, r_qT], [r_ps[m]])
                            ACT(Ex[m][:, :], psb[m][:, :], AF.Exp, [r_ps[m]], [r_Ex[m]], scale=XS)
                        for m in range(2):
                            MM(psb[2][:, :], ones_b[:, :], Ex[m][:, :], m == 0, m == 1, [CONST, r_Ex[m]], [r_ps[2]])
                        P.op("dve", lambda g: g.reciprocal(out=recx[:], in_=psb[2][:, :]), [r_ps[2]], [r_recx])
                        for dv in range(4):
                            q = 3 + dv % 2
                            for m in range(2):
                                MM(psb[q][:, :], vM[:, m, h * 512 + dv * 128:h * 512 + (dv + 1) * 128], Ex[m][:, :], m == 0, m == 1,
                                   [r_vM, r_Ex[m]], [r_ps[q]])
                            TT("dve", oT[:, dv, cols], psb[q][:, :], recx[:], ALU.mult, [r_ps[q], r_recx], [r_oT])
                    for qd in range(4):
                        wt, wr = wload(w_xo, 4, qd * 512, 512, r0=h * 512)
                        for c in range(8):
                            q = 5 + c % 2
                            for kc in range(4):
                                MM(psb[q][:, :], oT[:, kc, c * 128:(c + 1) * 128], wt[:, kc, :], kc == 0, kc == 3, [r_oT, wr], [r_ps[q]])
                            TT("dve", x_res[:, c, qd * 512:(qd + 1) * 512], psb[q][:, :], x_res[:, c, qd * 512:(qd + 1) * 512], ALU.add,
                               [r_ps[q], r_x[c]], [r_x[c]])
                tap("x2", x_res[:, :, :], r_x)
                P.barrier()
                stop_if("F")
            CAP = 384
            NSC = CAP // 128
            with ExitStack() as Gp:
                stt = alloc(Gp, "sttG", [128, 32], F32)
                r_stt = Reg()
                xn_tm = alloc(Gp, "xn_tm", [128, 8, D], BF16)
                r_xn = Reg("xn_tm")
                Gt, r_G = GtH, r_GH
                bupT = alloc(Gp, "bupT", [128, 1024], F32)
                r_bup = Reg("bup")
                Mf = alloc(Gp, "Mf", [128, 8, NEXP], F32)
                posf = alloc(Gp, "posf", [128, 8, NEXP], F32)
                r_rt = Reg("route")
                iotaC = alloc(Gp, "iotaC", [128, CAP], F32)
                with ExitStack() as G1:
                    gbc = alloc(G1, "gbcF", [128, D], F32)
                    junk = alloc(G1, "junkG", [128, D], BF16)
                    wr32 = alloc(G1, "wr32", [128, 16, NEXP], F32)
                    wrh = alloc(G1, "wrh", [128, 16, NEXP], BF16)
                    wrl = alloc(G1, "wrl", [128, 16, NEXP], BF16)
                    wrd = alloc(G1, "wrd", [128, 16, NEXP], F32)
                    xgh = alloc(G1, "xgh", [128, 16, 128], BF16)
                    xgl = alloc(G1, "xgl", [128, 16, 128], BF16)
                    xgd = alloc(G1, "xgd", [128, 16, 128], F32)
                    brb = alloc(G1, "brb", [128, NEXP], F32)
                    xg = alloc(G1, "xg", [128, 16, 128], F32)
                    lg = alloc(G1, "lg", [128, NEXP], F32)
                    t8 = alloc(G1, "t8", [128, 8], F32)
                    ex = alloc(G1, "ex", [128, NEXP], F32)
                    sm = alloc(G1, "sm", [128, 4], F32)
                    braw = alloc(G1, "braw", [128, 128], F32)
                    Mb = alloc(G1, "Mb", [128, 8, NEXP], BF16)
                    triS = alloc(G1, "triS", [128, 128], BF16)
                    iti = alloc(G1, "iti", [128, CAP], I32)
                    r_w32, r_xg, r_lg, r_braw, r_j = Reg(), Reg(), Reg(), Reg(), Reg()
                    P.dma("sp", wr32[:, :, :], w_router.rearrange("(kc p) n -> p kc n", p=128), W=[r_w32])
                    P.dma("sp", brb[:, :], b_router.broadcast_to([128, NEXP]), W=[r_w32])
                    P.dma("sp", gbc[:, :], ffn_g.broadcast_to([128, D]), W=[r_w32])
                    r_wsp = Reg()
                    CP("dve", wrh[:], wr32[:], [r_w32], [r_wsp])
                    TT("dve", wrd[:], wr32[:], wrh[:], ALU.subtract, [r_w32, r_wsp], [r_wsp])
                    CP("dve", wrl[:], wrd[:], [r_wsp], [r_wsp])
                    r_it = Reg()
                    P.op("pool", lambda g: g.iota(iti[:, 0:128], pattern=[[1, 128]], base=0, channel_multiplier=-1), (), [r_it])
                    TS("dve", triS[:], iti[:, 0:128], 0, None, ALU.is_gt, None, [r_it], [r_rt])
                    P.op("pool", lambda g: g.iota(iti[:, :], pattern=[[1, CAP]], base=0, channel_multiplier=0), [r_it], [r_it])
                    CP("dve", iotaC[:], iti[:], [r_it], [r_rt])
                    for i in range(8):
                        P.dma("sp", braw[:, :], b_up[i * 128:(i + 1) * 128, :], W=[r_braw])
                        TR(psb[6][:, 0:128], braw[:, :], ident_f[:], [r_braw, CONST], [r_ps[6]])
                        CP("dve", bupT[:, i * 128:(i + 1) * 128], psb[6][:, 0:128], [r_ps[6]], [r_bup])
                    for c in range(8):
                        ACT(junk[:], x_res[:, c, :], AF.Square, [r_x[c]], [r_j, r_stt], accum_out=stt[:, c:c + 1])
                        rstd_from(stt[:, c:c + 1], stt[:, 8 + c:9 + c], D, r_stt, r_stt, stt[:, 16 + c:17 + c])
                        STT(xn_tm[:, c, :], x_res[:, c, :], stt[:, 8 + c:9 + c], gbc[:], ALU.mult, ALU.mult,
                            [r_x[c], r_stt, r_w32], [r_xn])
                        for qq in range(4):
                            for j in range(4):
                                fc = qq * 4 + j
                                TR(psb[qq][:, j * 128:(j + 1) * 128], x_res[:, c, fc * 128:(fc + 1) * 128], ident_f[:], [r_x[c], CONST], [r_ps[qq]])
                            for j in range(4):
                                fc = qq * 4 + j
                                TS("dve", xg[:, fc, :], psb[qq][:, j * 128:(j + 1) * 128], pcol("ffn", fc), None, ALU.mult, None,
                                   [r_ps[qq], CONST], [r_xg])
                        CP("dve", xgh[:], xg[:], [r_xg], [r_xg])
                        TT("dve", xgd[:], xg[:], xgh[:], ALU.subtract, [r_xg], [r_xg])
                        CP("dve", xgl[:], xgd[:], [r_xg], [r_xg])
                        for fc in range(16):
                            MM(psb[4][:, 0:NEXP], xgh[:, fc, :], wrh[:, fc, :], fc == 0, False, [r_xg, r_wsp], [r_ps[4]])
                            MM(psb[4][:, 0:NEXP], xgh[:, fc, :], wrl[:, fc, :], False, False, [r_xg, r_wsp], [r_ps[4]])
                            MM(psb[4][:, 0:NEXP], xgl[:, fc, :], wrh[:, fc, :], False, fc == 15, [r_xg, r_wsp], [r_ps[4]])
                        STT(lg[:], psb[4][:, 0:NEXP], stt[:, 8 + c:9 + c], brb[:], ALU.mult, ALU.add, [r_ps[4], r_stt, r_w32], [r_lg])
                        P.op("dve", lambda g: g.max(out=t8[:], in_=lg[:]), [r_lg], [r_lg])
                        TS("dve", Mf[:, c, :], lg[:], t8[:, 3:4], None, ALU.is_ge, None, [r_lg], [r_rt])
                        TS("dve", sm[:, 0:1], t8[:, 0:1], -1.0, None, ALU.mult, None, [r_lg], [r_lg])
                        ACT(ex[:], lg[:], AF.Exp, [r_lg], [r_lg], bias=sm[:, 0:1])
                        STT(ex[:], ex[:], 1.0, Mf[:, c, :], ALU.mult, ALU.mult, [r_lg, r_rt], [r_lg], accum_out=sm[:, 1:2])
                        P.op("dve", lambda g: g.reciprocal(out=sm[:, 2:3], in_=sm[:, 1:2]), [r_lg], [r_lg])
                        TS("dve", Gt[:, c, :], ex[:], sm[:, 2:3], None, ALU.mult, None, [r_lg], [r_G])
                        CP("dve", Mb[:, c, :], Mf[:, c, :], [r_rt], [r_rt])
                    for c in range(8):
                        for c2 in range(c):
                            MM(psb[5][:, 0:NEXP], ones_b[:, :], Mb[:, c2, :], c2 == 0, False, [CONST, r_rt], [r_ps[5]])
                        MM(psb[5][:, 0:NEXP], triS[:, :], Mb[:, c, :], c == 0, True, [r_rt], [r_ps[5]])
                        CP("dve", posf[:, c, :], psb[5][:, 0:NEXP], [r_ps[5]], [r_rt])
                    P.barrier()
                tap("logits", Gt[:, :, :], [r_G])
                stop_if("R")
                XO = alloc(Gp, "XO", [128, 16 * CAP], BF16)
                XeT = XO[:, :].rearrange("p (f s) -> p f s", s=CAP)
                oe = XO[:, :].rearrange("p (c n) -> p c n", n=D)
                r_XO = Reg("XO")
                actT = alloc(Gp, "actT", [128, 16, CAP], BF16)
                r_act = Reg("act")
                Sel = alloc(Gp, "Sel", [128, 8, CAP], BF16)
                r_Sel = Reg("Sel")
                SelT = alloc(Gp, "SelT", [128, NSC, NOWN], BF16)
                r_SelT = Reg("SelT")
                gb = alloc(Gp, "gb", [128, CAP], F32)
                sgb = alloc(Gp, "sgb", [128, CAP], F32)
                gs = alloc(Gp, "gs", [128, CAP], F32)
                lb = alloc(Gp, "lb", [128, CAP], F32)
                r_gb, r_gs, r_lb = Reg(), Reg(), Reg()
                for e in range(NEXP if P.enabled else 0):
                    for c in range(8):
                        TS("dve", Sel[:, c, :], iotaC[:, :], posf[:, c, e:e + 1], Mf[:, c, e:e + 1], ALU.is_equal, ALU.mult,
                           [r_rt], [r_Sel])
                    for fc in range(16):
                        q = fc % 2
                        for c in range(8):
                            MM(psb[q][:, 0:CAP], xn_tm[:, c, fc * 128:(fc + 1) * 128], Sel[:, c, :], c == 0, c == 7, [r_xn, r_Sel], [r_ps[q]])
                        if fc % 2 == 0:
                            CP("act", XeT[:, fc, :], psb[q][:, 0:CAP], [r_ps[q]], [r_XO])
                        else:
                            CP("dve", XeT[:, fc, :], psb[q][:, 0:CAP], [r_ps[q]], [r_XO])
                    for sc in range(NSC):
                        for c in range(8):
                            TR(pbt[:, c * 128:(c + 1) * 128], Sel[:, c, sc * 128:(sc + 1) * 128], ident_b[:], [r_Sel, CONST], [r_pbt])
                        CP("dve", SelT[:, sc, :], pbt[:, :], [r_pbt], [r_SelT])
                    for n in range(4):
                        wg, rg = wload(w_up[e], 16, n * 512, 512)
                        wl, rl = wload(w_up[e], 16, 2048 + n * 512, 512)
                        for s4 in range(4):
                            ffc = n * 4 + s4
                            bg = bupT[:, e * 32 + ffc:e * 32 + ffc + 1]
                            bl = bupT[:, e * 32 + 16 + ffc:e * 32 + 16 + ffc + 1]
                            qg, ql = 2 + s4 % 2, 4 + s4 % 2
                            for kc in range(16):
                                MM(psb[qg][:, 0:CAP], wg[:, kc, s4 * 128:(s4 + 1) * 128], XeT[:, kc, :], kc == 0, kc == 15, [rg, r_XO], [r_ps[qg]])
                            for kc in range(16):
                                MM(psb[ql][:, 0:CAP], wl[:, kc, s4 * 128:(s4 + 1) * 128], XeT[:, kc, :], kc == 0, kc == 15, [rl, r_XO], [r_ps[ql]])
                            TS("dve", gb[:], psb[qg][:, 0:CAP], bg, 7.0, ALU.add, ALU.min, [r_ps[qg], r_bup], [r_gb])
                            ACT(sgb[:], gb[:], AF.Sigmoid, [r_gb], [r_gb], scale=1.702)
                            TT("dve", gs[:], gb[:], sgb[:], ALU.mult, [r_gb], [r_gs])
                            TS("dve", lb[:], psb[ql][:, 0:CAP], bl, 7.0, ALU.add, ALU.min, [r_ps[ql], r_bup], [r_lb])
                            TS("dve", lb[:], lb[:], -7.0, 1.0, ALU.max, ALU.add, [r_lb], [r_lb])
                            TT("dve", actT[:, ffc, :], gs[:], lb[:], ALU.mult, [r_gs, r_lb], [r_act])
                    for qd in range(4):
                        wt, wr = wload(w_down[e], 16, qd * 512, 512)
                        for sc in range(NSC):
                            q = (qd * NSC + sc) % 2
                            for kc in range(16):
                                MM(psb[q][:, :], actT[:, kc, sc * 128:(sc + 1) * 128], wt[:, kc, :], kc == 0, kc == 15, [r_act, wr], [r_ps[q]])
                            CP("act", oe[:, sc, qd * 512:(qd + 1) * 512], psb[q][:, :], [r_ps[q]], [r_XO])
                    for c in range(8):
                        for qd in range(4):
                            q = 2 + (c * 4 + qd) % 4
                            for sc in range(NSC):
                                MM(psb[q][:, :], SelT[:, sc, c * 128:(c + 1) * 128], oe[:, sc, qd * 512:(qd + 1) * 512], sc == 0, sc == NSC - 1,
                                   [r_SelT, r_XO], [r_ps[q]])
                            STT(x_res[:, c, qd * 512:(qd + 1) * 512], psb[q][:, :], Gt[:, c, e:e + 1], x_res[:, c, qd * 512:(qd + 1) * 512],
                                ALU.mult, ALU.add, [r_ps[q], r_G, r_x[c]], [r_x[c]])
                P.barrier()
            with ExitStack() as Hp:
                bd = alloc(Hp, "bd", [NEXP, D], F32)
                bdh = alloc(Hp, "bdh", [NEXP, D], BF16)
                bdl = alloc(Hp, "bdl", [NEXP, D], BF16)
                gT = alloc(Hp, "gT", [NEXP, 128], F32)
                gTh = alloc(Hp, "gTh", [NEXP, 128], BF16)
                gTl = alloc(Hp, "gTl", [NEXP, 128], BF16)
                gTd = alloc(Hp, "gTd", [NEXP, 128], F32)
                gbc = alloc(Hp, "gbc", [128, D], F32)
                stt = alloc(Hp, "sttH", [128, 32], F32)
                junk = alloc(Hp, "junkH", [128, D], BF16)
                yo = [alloc(Hp, "yo%d" % i, [128, D], F32) for i in range(2)]
                r_bd, r_gT, r_stt, r_j = Reg(), Reg(), Reg(), Reg()
                r_yo = [Reg(), Reg()]
                P.dma("sp", bd[:, :], b_down, W=[r_bd])
                P.dma("sp", gbc[:, :], final_norm.broadcast_to([128, D]), W=[r_bd])
                r_bds = Reg()
                CP("dve", bdh[:], bd[:], [r_bd], [r_bds])
                TT("dve", yo[0][0:NEXP, :], bd[:], bdh[:], ALU.subtract, [r_bd, r_bds], [r_yo[0]])
                CP("dve", bdl[:], yo[0][0:NEXP, :], [r_yo[0]], [r_bds])
                for c in range(8):
                    TR(psb[0][0:NEXP, 0:128], GtH[:, c, :], ident_f[:], [r_GH, CONST], [r_ps[0]])
                    CP("dve", gT[:, :], psb[0][0:NEXP, 0:128], [r_ps[0]], [r_gT])
                    CP("dve", gTh[:, :], gT[:, :], [r_gT], [r_gT])
                    TT("dve", gTd[:, :], gT[:, :], gTh[:, :], ALU.subtract, [r_gT], [r_gT])
                    CP("dve", gTl[:, :], gTd[:, :], [r_gT], [r_gT])
                    for qd in range(4):
                        q = 1 + qd % 2
                        cs = slice(qd * 512, (qd + 1) * 512)
                        MM(psb[q][:, :], gTh[:, :], bdh[:, cs], True, False, [r_gT, r_bds], [r_ps[q]])
                        MM(psb[q][:, :], gTh[:, :], bdl[:, cs], False, False, [r_gT, r_bds], [r_ps[q]])
                        MM(psb[q][:, :], gTl[:, :], bdh[:, cs], False, True, [r_gT, r_bds], [r_ps[q]])
                        TT("dve", x_res[:, c, qd * 512:(qd + 1) * 512], psb[q][:, :], x_res[:, c, qd * 512:(qd + 1) * 512], ALU.add,
                           [r_ps[q], r_x[c]], [r_x[c]])
                    ACT(junk[:], x_res[:, c, :], AF.Square, [r_x[c]], [r_j, r_stt], accum_out=stt[:, c:c + 1])
                    rstd_from(stt[:, c:c + 1], stt[:, 8 + c:9 + c], D, r_stt, r_stt, stt[:, 16 + c:17 + c])
                    s = c % 2
                    STT(yo[s][:], x_res[:, c, :], stt[:, 8 + c:9 + c], gbc[:], ALU.mult, ALU.mult, [r_x[c], r_stt, r_bd], [r_yo[s]])
                    P.dma("sp", y_d[c * 128:(c + 1) * 128, :], yo[s][:], R=[r_yo[s]])
    if P.enabled:
        P.final_wait("sp")
    P.es.close()
    return nc


def _pv(inp):
    pv = np.zeros((128, 128), np.float32)
    def put(name, vec):
        v = np.asarray(vec, np.float32).reshape(-1, 128)
        r = PV_ROWS[name]
        pv[r:r + v.shape[0]] = v
    put("attn", inp["attn_norm"][0]); put("xattn", inp["xattn_norm"][0]); put("memn", inp["mem_norm"][0])
    put("ffn", inp["ffn_norm"][0]); put("qa", inp["q_a_norm"][0]); put("kva", inp["kv_a_norm"][0])
    put("mixa", inp["mix_norm_attn"][0]); put("mixs", inp["mix_norm_ssm"][0]); put("ssmd", inp["ssm_d"][0])
    put("bglu", inp["b_glu"][0])
    return pv


def make_in_maps(inp, stop=None):
    f = lambda a: np.ascontiguousarray(np.asarray(a, np.float32))
    shared = dict(
        pv=_pv(inp), w_in=f(inp["w_in"][0]), w_q_b=f(inp["w_q_b"][0]), w_kv_b=f(inp["w_kv_b"][0]),
        lam_re=f(inp["ssm_lambda_re"][0]), lam_im=f(inp["ssm_lambda_im"][0]),
        log_dt=f(inp["ssm_log_dt"][0]).reshape(64, 1),
        b_re=f(inp["ssm_b_re"][0]), b_im=f(inp["ssm_b_im"][0]), c_re=f(inp["ssm_c_re"][0]), c_im=f(inp["ssm_c_im"][0]),
        w_glu=f(inp["w_glu"][0]), w_out=f(inp["w_out"][0]), w_xq=f(inp["w_xq"][0]), w_xk=f(inp["w_xk"][0]),
        w_xv=f(inp["w_xv"][0]), w_xo=f(inp["w_xo"][0]), w_router=f(inp["w_router"][0]),
        b_router=f(inp["b_router"][0]).reshape(1, NEXP), w_up=f(inp["w_up"][0]),
        b_up=f(inp["b_up"][0]).reshape(NEXP * 32, 128), w_down=f(inp["w_down"][0]), b_down=f(inp["b_down"][0]),
        final_norm=f(inp["final_norm"]).reshape(1, D),
        ffn_g=f(inp["ffn_norm"][0]).reshape(1, D),
    )
    if stop is not None:
        shared["w_up"] = shared["w_up"][0:1]
        shared["w_down"] = shared["w_down"][0:1]
    x = np.asarray(inp["x"], np.float32)
    mem = np.asarray(inp["mem"], np.float32)
    pos = np.asarray(inp["positions"], np.int32)
    maps = []
    for c in range(8):
        b, h = c // 2, c % 2
        m = dict(shared)
        m["x_own"] = np.ascontiguousarray(x[b, h * NOWN:(h + 1) * NOWN])
        m["x_pre"] = np.ascontiguousarray(x[b, 0:NOWN]) if h == 1 else np.zeros((NOWN, D), np.float32)
        m["pos"] = np.ascontiguousarray(np.concatenate([pos[b, 0:NOWN], pos[b, h * NOWN:(h + 1) * NOWN]]).reshape(1, NALL))
        m["pbias"] = np.full((128, 1), 0.0 if h == 1 else NEG, np.float32)
        m["mem"] = np.ascontiguousarray(mem[b])
        maps.append(m)
    return maps


def kernel(**inputs):
    nc = build()
    maps = make_in_maps(inputs)
    res = run_bass_kernel_spmd(nc, maps, core_ids=list(range(8)))
    out = np.zeros((4, SEQ, D), np.float32)
    for c in range(8):
        b, h = c // 2, c % 2
        out[b, h * NOWN:(h + 1) * NOWN] = res.results[c]["y"]
    return out
```

```python
import numpy as np
from contextlib import ExitStack
import concourse.bass as bass
import concourse.mybir as mybir
from concourse.bass_utils import run_bass_kernel_spmd

F32 = mybir.dt.float32
BF16 = mybir.dt.bfloat16
I32 = mybir.dt.int32
ALU = mybir.AluOpType
AF = mybir.ActivationFunctionType

D = 2048
SEQ = 2048
NOWN = 1024
NALL = 2048
MEM = 256
NEXP = 32
DFF = 2048
EPS = 1e-6
NEG = -30000.0
EPOCH = 20000


class Reg:
    __slots__ = ("w", "r", "dsem", "name")

    def __init__(self, name=""):
        self.w = []
        self.r = []
        self.dsem = None
        self.name = name


class Prog:
    def __init__(self, nc):
        self.nc = nc
        self.es = ExitStack()
        self.eng = {"pe": nc.tensor, "dve": nc.vector, "act": nc.scalar,
                    "pool": nc.gpsimd, "sp": nc.sync}
        self.cnt = {k: 0 for k in self.eng}
        self.epoch = {k: 0 for k in self.eng}
        self.sems = {}
        self.seen = {k: {} for k in self.eng}
        self.nsem = 0
        self.dma_events = []
        self.enabled = True
        for k in self.eng:
            self._newsem((k, 0))

    def _newsem(self, key):
        s = self.es.enter_context(self.nc.semaphore("s%d" % self.nsem))
        self.nsem += 1
        self.sems[key] = s
        return s

    def _wait(self, e, ev):
        key, val = ev
        if self.seen[e].get(key, 0) >= val:
            return
        self.eng[e].wait_ge(self.sems[key], val)
        self.seen[e][key] = val

    def _deps(self, e, R, W):
        evs = []
        for r in R:
            evs += r.w
        for w in W:
            for ev in w.w + w.r:
                if ev[0][0] == e:
                    continue
                evs.append(ev)
        for ev in evs:
            self._wait(e, ev)

    def op(self, e, fn, R=(), W=()):
        if not self.enabled:
            return None
        self._deps(e, R, W)
        inst = fn(self.eng[e])
        if self.cnt[e] >= EPOCH:
            self.epoch[e] += 1
            self.cnt[e] = 0
            self._newsem((e, self.epoch[e]))
        key = (e, self.epoch[e])
        inst.then_inc(self.sems[key], 1)
        self.cnt[e] += 1
        ev = (key, self.cnt[e])
        for w in W:
            w.w = [x for x in w.w if x[0][0] != e] + [ev]
            w.r = []
        for r in R:
            r.r = [x for x in r.r if x[0][0] != e] + [ev]
        return inst

    def dma(self, q, out, in_, R=(), W=(), sreg=None):
        if not self.enabled:
            return None
        self._deps(q, R, W)
        own = sreg if sreg is not None else (W[0] if len(W) else R[0])
        if own.dsem is None:
            key = ("d", self.nsem)
            self._newsem(key)
            own.dsem = [key, 0]
        inst = self.eng[q].dma_start(out=out, in_=in_)
        own.dsem[1] += 16
        inst.then_inc(self.sems[own.dsem[0]], 16)
        ev = (own.dsem[0], own.dsem[1])
        for w in W:
            w.w = [x for x in w.w if x[0] != ev[0]] + [ev]
            w.r = []
        for r in R:
            r.r = [x for x in r.r if x[0] != ev[0]] + [ev]
        self.dma_events.append(ev)
        return inst

    def barrier(self):
        if not self.enabled:
            return
        evs = [((k, self.epoch[k]), self.cnt[k]) for k in self.eng if self.cnt[k] > 0]
        last = {}
        for ev in self.dma_events:
            last[ev[0]] = max(last.get(ev[0], 0), ev[1])
        evs += list(last.items())
        for e in self.eng:
            for ev in evs:
                if ev[0][0] == e:
                    continue
                self._wait(e, ev)
        self.dma_events = []

    def final_wait(self, e="sp"):
        last = {}
        for ev in self.dma_events:
            last[ev[0]] = max(last.get(ev[0], 0), ev[1])
        for ev in last.items():
            self._wait(e, ev)
        for k in self.eng:
            if k != e and self.cnt[k] > 0:
                self._wait(e, ((k, self.epoch[k]), self.cnt[k]))


PV_ROWS = dict(attn=0, xattn=16, memn=32, ffn=48, qa=64, kva=67, mixa=69, mixs=77,
               ssmd=85, bglu=93)
TWO_PI = 6.283185307179586
C1 = 6.28125
C2 = TWO_PI - C1


def build(taps=(), stop=None):
    nc = bass.Bass("TRN2", target_bir_lowering=False)
    P = Prog(nc)
    NEXP_D = NEXP if stop is None else 1

    def stop_if(tag):
        if stop == tag:
            P.final_wait("sp")
            P.enabled = False

    def din(name, shape, dt=F32):
        return nc.dram_tensor(name, list(shape), dt, kind="ExternalInput").ap()

    x_own = din("x_own", [NOWN, D])
    x_pre = din("x_pre", [NOWN, D])
    pos_d = din("pos", [1, NALL], I32)
    pbias_d = din("pbias", [128, 1])
    mem_d = din("mem", [MEM, D])
    pv_d = din("pv", [128, 128])
    w_in = din("w_in", [D, 1728])
    w_q_b = din("w_q_b", [384, 1536])
    w_kv_b = din("w_kv_b", [256, 2048])
    lam_re = din("lam_re", [64, 64])
    lam_im = din("lam_im", [64, 64])
    log_dt = din("log_dt", [64, 1])
    b_re = din("b_re", [64, 64, 16])
    b_im = din("b_im", [64, 64, 16])
    c_re = din("c_re", [64, 16, 64])
    c_im = din("c_im", [64, 16, 64])
    w_glu = din("w_glu", [1024, 1024])
    w_out = din("w_out", [D, D])
    w_xq = din("w_xq", [D, D])
    w_xk = din("w_xk", [D, D])
    w_xv = din("w_xv", [D, D])
    w_xo = din("w_xo", [D, D])
    w_router = din("w_router", [D, NEXP])
    b_router = din("b_router", [1, NEXP])
    w_up = din("w_up", [NEXP_D, D, 2 * DFF])
    b_up = din("b_up", [NEXP * 32, 128])
    w_down = din("w_down", [NEXP_D, DFF, D])
    b_down = din("b_down", [NEXP, D])
    final_norm = din("final_norm", [1, D])
    ffn_g = din("ffn_g", [1, D])
    y_d = nc.dram_tensor("y", [NOWN, D], F32, kind="ExternalOutput").ap()
    tap_d = {}
    for (tn, tshape, tdt) in taps:
        tap_d[tn] = nc.dram_tensor("tap_" + tn, list(tshape), tdt, kind="ExternalOutput").ap()

    uid = [0]

    def alloc(es, name, shape, dt):
        uid[0] += 1
        return es.enter_context(nc.sbuf_tensor("%s_%d" % (name, uid[0]), list(shape), dt))

    def palloc(es, name, shape, dt):
        uid[0] += 1
        return es.enter_context(nc.psum_tensor("%s_%d" % (name, uid[0]), list(shape), dt))

    def TT(e, out, a, b, op, R, W):
        return P.op(e, lambda g: g.tensor_tensor(out=out, in0=a, in1=b, op=op), R, W)

    def TS(e, out, a, s1, s2, op0, op1, R, W):
        if op1 is None:
            return P.op(e, lambda g: g.tensor_scalar(out=out, in0=a, scalar1=s1, scalar2=None, op0=op0), R, W)
        return P.op(e, lambda g: g.tensor_scalar(out=out, in0=a, scalar1=s1, scalar2=s2, op0=op0, op1=op1), R, W)

    def STT(out, a, s, b, op0, op1, R, W, **kw):
        return P.op("dve", lambda g: g.scalar_tensor_tensor(out=out, in0=a, scalar=s, in1=b, op0=op0, op1=op1, **kw), R, W)

    def ACT(out, in_, func, R, W, **kw):
        return P.op("act", lambda g: g.activation(out=out, in_=in_, func=func, **kw), R, W)

    def MM(out, lhsT, rhs, start, stop, R, W):
        return P.op("pe", lambda g: g.matmul(out, lhsT, rhs, start=start, stop=stop), R, W)

    def TR(out, in_, ident, R, W):
        return P.op("pe", lambda g: g.transpose(out, in_, ident), R, W)

    def CP(e, out, in_, R, W):
        if e == "act":
            return P.op(e, lambda g: g.activation(out=out, in_=in_, func=AF.Copy), R, W)
        return P.op(e, lambda g: g.tensor_copy(out=out, in_=in_), R, W)

    def MS(e, ap, val, W):
        return P.op(e, lambda g: g.memset(ap, val), (), W)

    def tap(name, sb_ap, R):
        if name in tap_d:
            P.dma("sp", tap_d[name], sb_ap, R=R, sreg=Reg())

    G = ExitStack()
    with G:
        ident_f = alloc(G, "ident_f", [128, 128], F32)
        ident_b = alloc(G, "ident_b", [128, 128], BF16)
        ones_b = alloc(G, "ones_b", [128, 128], BF16)
        ones_f = alloc(G, "ones_f", [128, 128], F32)
        maskW = alloc(G, "maskW", [128, 896], BF16)
        pvT = alloc(G, "pvT", [128, 128], F32)
        pbias = alloc(G, "pbias_sb", [128, 1], F32)
        wslot = [alloc(G, "wslot%d" % i, [128, 16, 512], BF16) for i in range(3)]
        wreg = [Reg("w%d" % i) for i in range(3)]
        wctr = [0]
        CONST = Reg("const")

        def wload(dram2d, nk, c0, ncols, r0=0):
            s = wctr[0] % 3
            wctr[0] += 1
            src = dram2d[r0:r0 + nk * 128, c0:c0 + ncols].rearrange("(kc p) n -> p kc n", p=128)
            P.dma("pool", wslot[s][:, 0:nk, 0:ncols], src, W=[wreg[s]])
            return wslot[s], wreg[s]

        with ExitStack() as C0:
            it = alloc(C0, "iota_t", [128, 896], I32)
            pvr = alloc(C0, "pv_raw", [128, 128], F32)
            ps0 = palloc(C0, "ps_c0", [128, 512], F32)
            rt = Reg()
            P.op("pool", lambda g: g.iota(it[:, 0:128], pattern=[[1, 128]], base=0, channel_multiplier=-1), (), [rt])
            TS("dve", ident_f[:], it[:, 0:128], 0, None, ALU.is_equal, None, [rt], [CONST])
            TS("dve", ident_b[:], it[:, 0:128], 0, None, ALU.is_equal, None, [rt], [CONST])
            MS("dve", ones_b[:], 1.0, [CONST])
            MS("dve", ones_f[:], 1.0, [CONST])
            P.op("pool", lambda g: g.iota(it[:, :], pattern=[[1, 896]], base=-384, channel_multiplier=-1), [rt], [rt])
            TS("dve", maskW[:], it[:, :], 0, None, ALU.is_ge, None, [rt], [CONST])
            rp = Reg()
            P.dma("sp", pvr[:], pv_d, W=[rp])
            P.dma("sp", pbias[:], pbias_d, W=[CONST])
            rps = Reg()
            TR(ps0[:, 0:128], pvr[:], ident_f[:], [rp, CONST], [rps])
            CP("dve", pvT[:], ps0[:, 0:128], [rps], [CONST])
            P.barrier()

        def pcol(name, i):
            c = PV_ROWS[name] + i
            return pvT[:, c:c + 1]

        def neg_sincos(es, ang, shape, nsin, ncos, rin, rout):
            n_part = shape[0]
            kt = alloc(es, "sc_k", shape, I32)
            kf = alloc(es, "sc_kf", shape, F32)
            r1 = alloc(es, "sc_r1", shape, F32)
            r2 = alloc(es, "sc_r2", shape, F32)
            rr = Reg()
            TS("dve", r1[:], ang, 1.0 / TWO_PI, None, ALU.mult, None, [rin], [rr])
            CP("dve", kt[:], r1[:], [rr], [rr])
            CP("dve", kf[:], kt[:], [rr], [rr])
            STT(r1[:], kf[:], -C1, ang, ALU.mult, ALU.add, [rr, rin], [rr])
            STT(r2[:], kf[:], -C2, r1[:], ALU.mult, ALU.add, [rr], [rr])
            TS("dve", r1[:], r2[:], 0.0, TWO_PI, ALU.is_lt, ALU.mult, [rr], [rr])
            TT("dve", r2[:], r2[:], r1[:], ALU.add, [rr], [rr])
            TS("dve", r1[:], r2[:], TWO_PI, -TWO_PI, ALU.is_ge, ALU.mult, [rr], [rr])
            TT("dve", r2[:], r2[:], r1[:], ALU.add, [rr], [rr])
            ACT(nsin, r2[:], AF.Sin, [rr], [rout], bias=negpi[0:n_part, :])
            TS("dve", r1[:], r2[:], np.pi / 2, None, ALU.add, None, [rr], [rr])
            TS("dve", kf[:], r1[:], TWO_PI, -TWO_PI, ALU.is_ge, ALU.mult, [rr], [rr])
            TT("dve", r1[:], r1[:], kf[:], ALU.add, [rr], [rr])
            ACT(ncos, r1[:], AF.Sin, [rr], [rout], bias=negpi[0:n_part, :])

        negpi = alloc(G, "negpi", [128, 1], F32)
        MS("dve", negpi[:], -np.pi, [CONST])
        epsc = alloc(G, "epsc", [128, 1], F32)
        MS("dve", epsc[:], EPS, [CONST])

        def rstd_from(ssq_ap, out_ap, n, rin, rout, tmp_ap):
            ACT(tmp_ap, ssq_ap, AF.Sqrt, [rin], [rout], scale=1.0 / n, bias=epsc[0:ssq_ap.shape[0], :])
            P.op("dve", lambda g: g.reciprocal(out=out_ap, in_=tmp_ap), [rout], [rout])

        zeroc = alloc(G, "zeroc", [128, 1], F32)
        MS("dve", zeroc[:], 0.0, [CONST])
        mix_d = nc.dram_tensor("mix_scr", [D, NOWN], BF16, kind="Internal").ap()
        r_mixd = Reg("mixd")
        psb = [palloc(G, "psb%d" % i, [128, 512], F32) for i in range(7)]
        r_ps = [Reg("ps%d" % i) for i in range(7)]
        pbt = palloc(G, "pbt", [128, 1024], BF16)
        r_pbt = Reg("pbt")

        UT = ExitStack()
        with UT:
            uT = alloc(UT, "uT", [128, 8, NALL], BF16)
            r_uT = Reg("uT")
            ATT = ExitStack()
            with ATT:
                cqT = alloc(ATT, "cqT", [128, 3, NOWN], BF16)
                ckvT = alloc(ATT, "ckvT", [128, 2, NALL], BF16)
                kpeT = alloc(ATT, "kpeT", [64, NALL], BF16)
                cosT = alloc(ATT, "cosT", [64, NALL], F32)
                sinS = alloc(ATT, "sinS", [64, NALL], F32)
                r_cq, r_ckv, r_kpe, r_cs = Reg("cq"), Reg("ckv"), Reg("kpe"), Reg("cs")
                with ExitStack() as A0:
                    posi = alloc(A0, "posi", [64, NALL], I32)
                    ang = alloc(A0, "ang", [64, NALL], F32)
                    nsn = alloc(A0, "nsn", [64, NALL], F32)
                    idx = alloc(A0, "idx", [64, 1], I32)
                    invf = alloc(A0, "invf", [64, 1], F32)
                    ra = Reg()
                    P.dma("sp", posi[:], pos_d.broadcast_to([64, NALL]), W=[ra])
                    P.op("pool", lambda g: g.iota(idx[0:32, :], pattern=[[0, 1]], base=0, channel_multiplier=1), (), [ra])
                    P.op("pool", lambda g: g.iota(idx[32:64, :], pattern=[[0, 1]], base=0, channel_multiplier=1), (), [ra])
                    ACT(invf[:], idx[:], AF.Exp, [ra], [ra], scale=-float(np.log(10000.0)) / 32.0)
                    CP("dve", ang[:], posi[:], [ra], [ra])
                    TS("dve", ang[:], ang[:], invf[:, 0:1], None, ALU.mult, None, [ra], [ra])
                    neg_sincos(A0, ang[:], [64, NALL], nsn[:], cosT[:], ra, r_cs)
                    TS("dve", cosT[:], cosT[:], -1.0, None, ALU.mult, None, [r_cs], [r_cs])
                    CP("dve", sinS[0:32, :], nsn[0:32, :], [r_cs], [r_cs])
                    TS("dve", sinS[32:64, :], nsn[32:64, :], -1.0, None, ALU.mult, None, [r_cs], [r_cs])
                    P.barrier()
                tap("cosT", cosT[:, :], [r_cs])
                with ExitStack() as A:
                    xT = alloc(A, "xT", [128, 16, NOWN], BF16)
                    r_xT = [Reg("xT%d" % c) for c in range(8)]
                    xs = [alloc(A, "xs%d" % i, [128, D], F32) for i in range(2)]
                    xnb = [alloc(A, "xnb%d" % i, [128, D], BF16) for i in range(2)]
                    r_xs = [Reg(), Reg()]
                    r_xn = [Reg(), Reg()]
                    st = alloc(A, "statA", [128, 64], F32)
                    r_st = Reg()
                    csb = alloc(A, "csb", [128, 384], F32)
                    cjunk = alloc(A, "cjunk", [128, 384], F32)
                    cnb = alloc(A, "cnb", [128, 384], BF16)
                    r_csb, r_cnb = Reg(), Reg()
                    wrot = alloc(A, "wrotA", [128, 16, 64], BF16)
                    r_wrot = Reg()
                    ta = alloc(A, "ropeA", [64, 512], F32)
                    tb = alloc(A, "ropeB", [64, 512], F32)
                    r_ta = Reg()
                    win3 = w_in.rearrange("(kc p) n -> p kc n", p=128)
                    P.dma("pool", wrot[:, :, 0:32], win3[:, :, 672:704], W=[r_wrot])
                    P.dma("pool", wrot[:, :, 32:64], win3[:, :, 640:672], W=[r_wrot])

                    def norm_T(ncols, nchunk, stcol, gain_name, dstT, dcol0, r_dst, ps_src, r_psrc):
                        CP("act", csb[:, 0:ncols], ps_src, [r_psrc], [r_csb])
                        ACT(cjunk[:, 0:ncols], csb[:, 0:ncols], AF.Square, [r_csb], [r_st], accum_out=st[:, stcol:stcol + 1])
                        rstd_from(st[:, stcol:stcol + 1], st[:, stcol + 1:stcol + 2], ncols, r_st, r_st, st[:, stcol + 2:stcol + 3])
                        TS("dve", cnb[:, 0:ncols], csb[:, 0:ncols], st[:, stcol + 1:stcol + 2], None, ALU.mult, None,
                           [r_csb, r_st], [r_cnb])
                        for j in range(nchunk):
                            TR(pbt[:, j * 128:(j + 1) * 128], cnb[:, j * 128:(j + 1) * 128], ident_b[:], [r_cnb, CONST], [r_pbt])
                        for j in range(nchunk):
                            TS("dve", dstT[:, j, dcol0:dcol0 + 128], pbt[:, j * 128:(j + 1) * 128], pcol(gain_name, j), None,
                               ALU.mult, None, [r_pbt, CONST], [r_dst])

                    for hf in range(2):
                        for c in range(8):
                            s = c % 2
                            src = (x_pre if hf == 0 else x_own)[c * 128:(c + 1) * 128, :]
                            P.dma("sp", xs[s][:], src, W=[r_xs[s]])
                            sc = hf * 8 + c
                            ACT(xnb[s][:], xs[s][:], AF.Square, [r_xs[s]], [r_xn[s], r_st], accum_out=st[:, sc:sc + 1])
                            rstd_from(st[:, sc:sc + 1], st[:, 16 + sc:17 + sc], D, r_st, r_st, st[:, 32 + sc:33 + sc])
                            ACT(xnb[s][:], xs[s][:], AF.Copy, [r_xs[s], r_st], [r_xn[s]], scale=st[:, 16 + sc:17 + sc])
                            for q in range(2):
                                for j in range(8):
                                    fc = q * 8 + j
                                    TR(pbt[:, j * 128:(j + 1) * 128], xnb[s][:, fc * 128:(fc + 1) * 128], ident_b[:],
                                       [r_xn[s], CONST], [r_pbt])
                                for j in range(8):
                                    fc = q * 8 + j
                                    if j % 2 == 0:
                                        TS("dve", xT[:, fc, c * 128:(c + 1) * 128], pbt[:, j * 128:(j + 1) * 128],
                                           pcol("attn", fc), None, ALU.mult, None, [r_pbt, CONST], [r_xT[c]])
                                    else:
                                        ACT(xT[:, fc, c * 128:(c + 1) * 128], pbt[:, j * 128:(j + 1) * 128], AF.Copy,
                                            [r_pbt, CONST], [r_xT[c]], scale=pcol("attn", fc))
                        if hf == 1:
                            tap("xT", xT[:, :, :], r_xT)
                            wt, wr = wload(w_in, 16, 0, 384)
                            for c in range(8):
                                q = c % 2
                                for kc in range(16):
                                    MM(psb[q][:, 0:384], xT[:, kc, c * 128:(c + 1) * 128], wt[:, kc, 0:384], kc == 0, kc == 15,
                                       [r_xT[c], wr], [r_ps[q]])
                                norm_T(384, 3, 48, "qa", cqT, c * 128, r_cq, psb[q][:, 0:384], r_ps[q])
                        wt, wr = wload(w_in, 16, 384, 320)
                        for c in range(8):
                            q = c % 2
                            for kc in range(16):
                                MM(psb[q][:, 0:256], xT[:, kc, c * 128:(c + 1) * 128], wt[:, kc, 0:256], kc == 0, kc == 15,
                                   [r_xT[c], wr], [r_ps[q]])
                            norm_T(256, 2, 52, "kva", ckvT, hf * NOWN + c * 128, r_ckv, psb[q][:, 0:256], r_ps[q])
                        for t in range(2):
                            cols = slice(t * 512, (t + 1) * 512)
                            gcols = slice(hf * NOWN + t * 512, hf * NOWN + (t + 1) * 512)
                            for kc in range(16):
                                MM(psb[2][0:64, :], wt[:, kc, 256:320], xT[:, kc, cols], kc == 0, kc == 15,
                                   r_xT[4 * t:4 * t + 4] + [wr], [r_ps[2]])
                            for kc in range(16):
                                MM(psb[3][0:64, :], wrot[:, kc, :], xT[:, kc, cols], kc == 0, kc == 15,
                                   r_xT[4 * t:4 * t + 4] + [r_wrot], [r_ps[3]])
                            TT("dve", ta[:], psb[2][0:64, :], cosT[:, gcols], ALU.mult, [r_ps[2], r_cs], [r_ta])
                            TT("dve", tb[:], psb[3][0:64, :], sinS[:, gcols], ALU.mult, [r_ps[3], r_cs], [r_ta])
                            TT("dve", kpeT[:, gcols], ta[:], tb[:], ALU.add, [r_ta], [r_kpe])
                        for pc in range(2):
                            wt, wr = wload(w_in, 16, 704 + 512 * pc, 512)
                            for sc in range(4):
                                cc = pc * 4 + sc
                                for t in range(2):
                                    q = 4 + (sc * 2 + t) % 2
                                    cols = slice(t * 512, (t + 1) * 512)
                                    gcols = slice(hf * NOWN + t * 512, hf * NOWN + (t + 1) * 512)
                                    for kc in range(16):
                                        MM(psb[q][:, :], wt[:, kc, sc * 128:(sc + 1) * 128], xT[:, kc, cols], kc == 0, kc == 15,
                                           r_xT[4 * t:4 * t + 4] + [wr], [r_ps[q]])
                                    if t % 2 == 0:
                                        CP("dve", uT[:, cc, gcols], psb[q][:, :], [r_ps[q]], [r_uT])
                                    else:
                                        CP("act", uT[:, cc, gcols], psb[q][:, :], [r_ps[q]], [r_uT])
                    tap("cqT", cqT[:, :, :], [r_cq])
                    tap("ckvT", ckvT[:, :, :], [r_ckv])
                    tap("kpeT", kpeT[:, :], [r_kpe])
                    tap("uT", uT[:, :, :], [r_uT])
                    P.barrier()
                    stop_if("A")
                with ExitStack() as BC:
                    attnT = alloc(BC, "attnT", [128, 8, NOWN], BF16)
                    r_attn = Reg("attn")
                    qnT = alloc(BC, "qnT", [128, 4, NOWN], BF16)
                    qpeT = alloc(BC, "qpeT", [64, 4, NOWN], BF16)
                    knT = alloc(BC, "knT", [128, 4, NALL], BF16)
                    Vt = alloc(BC, "Vt", [128, 16, 512], BF16)
                    r_qn, r_qpe, r_kn, r_V = Reg("qn"), Reg("qpe"), Reg("kn"), Reg("V")
                    wqrot = alloc(BC, "wqrot", [128, 3, 8, 64], BF16)
                    r_wqrot = Reg()
                    ta = alloc(BC, "ropeA2", [64, 512], F32)
                    tb = alloc(BC, "ropeB2", [64, 512], F32)
                    r_ta = Reg()
                    Et = [alloc(BC, "Et%d" % i, [128, 512], BF16) for i in range(2)]
                    r_E = [Reg(), Reg()]
                    rec = alloc(BC, "rec", [128, 512], F32)
                    r_rec = Reg()
                    wq4 = w_q_b.rearrange("(kc p) (h c) -> p kc h c", p=128, c=192)
                    for kc in range(3):
                        P.dma("pool", wqrot[:, kc, :, 0:32], wq4[:, kc, :, 160:192], W=[r_wqrot])
                        P.dma("pool", wqrot[:, kc, :, 32:64], wq4[:, kc, :, 128:160], W=[r_wqrot])
                    SCALE = 192.0 ** -0.5
                    for hg in range(2):
                        for hp in range(2):
                            wt, wr = wload(w_q_b, 3, (hg * 2 + hp) * 384, 384)
                            for hh in range(2):
                                hl = hp * 2 + hh
                                h = hg * 4 + hl
                                for t in range(2):
                                    cols = slice(t * 512, (t + 1) * 512)
                                    for kc in range(3):
                                        MM(psb[0][:, :], wt[:, kc, hh * 192:hh * 192 + 128], cqT[:, kc, cols], kc == 0, kc == 2,
                                           [r_cq, wr], [r_ps[0]])
                                    CP("act", qnT[:, hl, cols], psb[0][:, :], [r_ps[0]], [r_qn])
                                    for kc in range(3):
                                        MM(psb[1][0:64, :], wt[:, kc, hh * 192 + 128:hh * 192 + 192], cqT[:, kc, cols], kc == 0, kc == 2,
                                           [r_cq, wr], [r_ps[1]])
                                    for kc in range(3):
                                        MM(psb[2][0:64, :], wqrot[:, kc, h, :], cqT[:, kc, cols], kc == 0, kc == 2,
                                           [r_cq, r_wqrot], [r_ps[2]])
                                    gcols = slice(NOWN + t * 512, NOWN + (t + 1) * 512)
                                    TT("dve", ta[:], psb[1][0:64, :], cosT[:, gcols], ALU.mult, [r_ps[1], r_cs], [r_ta])
                                    TT("dve", tb[:], psb[2][0:64, :], sinS[:, gcols], ALU.mult, [r_ps[2], r_cs], [r_ta])
                                    TT("dve", qpeT[:, hl, cols], ta[:], tb[:], ALU.add, [r_ta], [r_qpe])
                        for hp in range(2):
                            wt, wr = wload(w_kv_b, 2, (hg * 2 + hp) * 512, 512)
                            for hh in range(2):
                                hl = hp * 2 + hh
                                for t in range(4):
                                    cols = slice(t * 512, (t + 1) * 512)
                                    q = 3 + t % 2
                                    for kc in range(2):
                                        MM(psb[q][:, :], wt[:, kc, hh * 256:hh * 256 + 128], ckvT[:, kc, cols], kc == 0, kc == 1,
                                           [r_ckv, wr], [r_ps[q]])
                                    if t % 2 == 0:
                                        CP("act", knT[:, hl, cols], psb[q][:, :], [r_ps[q]], [r_kn])
                                    else:
                                        CP("dve", knT[:, hl, cols], psb[q][:, :], [r_ps[q]], [r_kn])
                                for c in range(16):
                                    q = 5 + c % 2
                                    for kc in range(2):
                                        MM(psb[q][:, 0:128], ckvT[:, kc, c * 128:(c + 1) * 128], wt[:, kc, hh * 256 + 128:hh * 256 + 256],
                                           kc == 0, kc == 1, [r_ckv, wr], [r_ps[q]])
                                    if c % 2 == 0:
                                        CP("dve", Vt[:, c, hl * 128:(hl + 1) * 128], psb[q][:, 0:128], [r_ps[q]], [r_V])
                                    else:
                                        CP("act", Vt[:, c, hl * 128:(hl + 1) * 128], psb[q][:, 0:128], [r_ps[q]], [r_V])
                        if hg == 0:
                            tap("qnT", qnT[:, :, :], [r_qn])
                            tap("qpeT", qpeT[:, :, :], [r_qpe])
                            tap("knT", knT[:, :, :], [r_kn])
                            tap("Vt", Vt[:, :, :], [r_V])
                            stop_if("B0")
                        for hl in range(4):
                            h = hg * 4 + hl
                            for j in range(2):
                                nkc = 8 + 4 * j + 4
                                po, pm = psb[2 + j], psb[4 + j]
                                r_po, r_pm = r_ps[2 + j], r_ps[4 + j]

                                def s_mm(kc):
                                    r = kc - (8 + 4 * j)
                                    q0 = max(r, 0) * 128
                                    sb = kc % 2
                                    qs = slice(j * 512 + q0, (j + 1) * 512)
                                    MM(psb[sb][:, q0:512], knT[:, hl, kc * 128:(kc + 1) * 128], qnT[:, hl, qs], True, False,
                                       [r_kn, r_qn], [r_ps[sb]])
                                    MM(psb[sb][:, q0:512], kpeT[:, kc * 128:(kc + 1) * 128], qpeT[:, hl, qs], False, True,
                                       [r_kpe, r_qpe], [r_ps[sb]])

                                s_mm(0)
                                for kc in range(nkc):
                                    if kc + 1 < nkc:
                                        s_mm(kc + 1)
                                    r = kc - (8 + 4 * j)
                                    q0 = max(r, 0) * 128
                                    sb = kc % 2
                                    bias = pbias[:, 0:1] if kc < 8 else zeroc[:, 0:1]
                                    ACT(Et[sb][:, q0:512], psb[sb][:, q0:512], AF.Exp, [r_ps[sb], CONST], [r_E[sb]],
                                        scale=SCALE, bias=bias)
                                    if r >= 0:
                                        m0 = 384 - 128 * r + q0
                                        TT("dve", Et[sb][:, q0:512], Et[sb][:, q0:512], maskW[:, m0:m0 + 512 - q0], ALU.mult,
                                           [r_E[sb], CONST], [r_E[sb]])
                                    MM(po[:, q0:512], Vt[:, kc, hl * 128:(hl + 1) * 128], Et[sb][:, q0:512], kc == 0, kc == nkc - 1,
                                       [r_V, r_E[sb]], [r_po])
                                    MM(pm[:, q0:512], ones_b[:, :], Et[sb][:, q0:512], kc == 0, kc == nkc - 1,
                                       [CONST, r_E[sb]], [r_pm])
                                P.op("dve", lambda g: g.reciprocal(out=rec[:], in_=pm[:, :]), [r_pm], [r_rec])
                                TT("dve", attnT[:, h, j * 512:(j + 1) * 512], po[:, :], rec[:], ALU.mult, [r_po, r_rec], [r_attn])
                    tap("attnT", attnT[:, :, :], [r_attn])
                    stop_if("C1")
                    sq = alloc(BC, "sqA", [128, 512], BF16)
                    r_sq = Reg()
                    rsb = alloc(BC, "rsbA", [128, 512], F32)
                    r_rsb = Reg()
                    mo = alloc(BC, "moA", [128, 8, 512], BF16)
                    r_mo = Reg()
                    for j in range(2):
                        cols = slice(j * 512, (j + 1) * 512)
                        for h in range(8):
                            ACT(sq[:], attnT[:, h, cols], AF.Square, [r_attn], [r_sq])
                            MM(psb[0][:, :], ones_b[:, :], sq[:], h == 0, h == 7, [CONST, r_sq], [r_ps[0]])
                        ACT(rsb[:], psb[0][:, :], AF.Sqrt, [r_ps[0], CONST], [r_rsb], scale=1.0 / 1024, bias=epsc[:, 0:1])
                        P.op("dve", lambda g: g.reciprocal(out=rsb[:], in_=rsb[:]), [r_rsb], [r_rsb])
                        for h in range(8):
                            STT(mo[:, h, :], attnT[:, h, cols], pcol("mixa", h), rsb[:], ALU.mult, ALU.mult,
                                [r_attn, r_rsb, CONST], [r_mo])
                        P.dma("sp", mix_d[0:1024, cols].rearrange("(h p) n -> p h n", p=128), mo[:, :, :], R=[r_mo], W=[r_mixd])
                    P.barrier()
                    stop_if("C")
            with ExitStack() as Dp:
                def tab(name, n=32):
                    return alloc(Dp, name, [128, n], F32)
                r_tb = Reg("ssmtab")
                yact = alloc(Dp, "yact", [128, 8, NOWN], BF16)
                r_ya = Reg("yact")
                Bpad = alloc(Dp, "Bpad", [128, 2, 32, 128], BF16)
                Cpad = alloc(Dp, "Cpad", [128, 2, 32, 128], BF16)
                EC = alloc(Dp, "EC", [128, 11, 32], F32)
                ES = alloc(Dp, "ES", [128, 11, 32], F32)
                LR, LI, LDT = tab("LR"), tab("LI"), tab("LDT")
                DT, MAG, TH = tab("DT"), tab("MAG"), tab("TH")
                CS, SN = tab("CS"), tab("SN")
                NR, NI, DEN, CR, CI, T1 = tab("NR"), tab("NI"), tab("DEN"), tab("CR"), tab("CI"), tab("T1")
                SETUP = ExitStack()
                SETUP.__enter__()
                lamw = alloc(SETUP, "lamw", [64, 3, 128], F32)
                P.dma("sp", lamw[:, 0, 0:64], lam_re, W=[r_tb])
                P.dma("sp", lamw[:, 0, 64:128], lam_re, W=[r_tb])
                P.dma("sp", lamw[:, 1, 0:64], lam_im, W=[r_tb])
                P.dma("sp", lamw[:, 1, 64:128], lam_im, W=[r_tb])
                ldc = alloc(SETUP, "ldc", [64, 1], F32)
                r_ldc = Reg()
                P.dma("sp", ldc[:, :], log_dt, W=[r_ldc])
                CP("dve", lamw[:, 2, :], ldc[:, 0:1].broadcast_to([64, 128]), [r_ldc], [r_tb])
                for i, dst in enumerate((LR, LI, LDT)):
                    TR(psb[0][:, 0:64], lamw[:, i, :], ident_f[0:64, 0:64], [r_tb, CONST], [r_ps[0]])
                    CP("dve", dst[0:64, :], psb[0][0:64, 0:64:2], [r_ps[0]], [r_tb])
                    CP("dve", dst[64:128, :], psb[0][64:128, 1:64:2], [r_ps[0]], [r_tb])
                ACT(DT[:], LDT[:], AF.Exp, [r_tb], [r_tb])
                TT("dve", TH[:], LR[:], DT[:], ALU.mult, [r_tb], [r_tb])
                ACT(MAG[:], TH[:], AF.Exp, [r_tb], [r_tb])
                TT("dve", TH[:], LI[:], DT[:], ALU.mult, [r_tb], [r_tb])
                neg_sincos(SETUP, TH[:], [128, 32], SN[:], CS[:], r_tb, r_tb)
                TS("dve", CS[:], CS[:], -1.0, None, ALU.mult, None, [r_tb], [r_tb])
                TS("dve", SN[:], SN[:], -1.0, None, ALU.mult, None, [r_tb], [r_tb])
                TT("dve", NR[:], MAG[:], CS[:], ALU.mult, [r_tb], [r_tb])
                TS("dve", NR[:], NR[:], -1.0, None, ALU.add, None, [r_tb], [r_tb])
                TT("dve", NI[:], MAG[:], SN[:], ALU.mult, [r_tb], [r_tb])
                TT("dve", DEN[:], LR[:], LR[:], ALU.mult, [r_tb], [r_tb])
                TT("dve", T1[:], LI[:], LI[:], ALU.mult, [r_tb], [r_tb])
                TT("dve", DEN[:], DEN[:], T1[:], ALU.add, [r_tb], [r_tb])
                P.op("dve", lambda g: g.reciprocal(out=DEN[:], in_=DEN[:]), [r_tb], [r_tb])
                TT("dve", CR[:], NR[:], LR[:], ALU.mult, [r_tb], [r_tb])
                TT("dve", T1[:], NI[:], LI[:], ALU.mult, [r_tb], [r_tb])
                TT("dve", CR[:], CR[:], T1[:], ALU.add, [r_tb], [r_tb])
                TT("dve", CR[:], CR[:], DEN[:], ALU.mult, [r_tb], [r_tb])
                TT("dve", CI[:], NI[:], LR[:], ALU.mult, [r_tb], [r_tb])
                TT("dve", T1[:], NR[:], LI[:], ALU.mult, [r_tb], [r_tb])
                TT("dve", CI[:], CI[:], T1[:], ALU.subtract, [r_tb], [r_tb])
                TT("dve", CI[:], CI[:], DEN[:], ALU.mult, [r_tb], [r_tb])
                CP("dve", EC[:, 0, :], CS[:], [r_tb], [r_tb])
                CP("dve", ES[:, 0, :], SN[:], [r_tb], [r_tb])
                for k in range(10):
                    TT("dve", T1[:], EC[:, k, :], EC[:, k, :], ALU.mult, [r_tb], [r_tb])
                    TT("dve", NR[:], ES[:, k, :], ES[:, k, :], ALU.mult, [r_tb], [r_tb])
                    TT("dve", EC[:, k + 1, :], T1[:], NR[:], ALU.subtract, [r_tb], [r_tb])
                    TT("dve", T1[:], EC[:, k, :], ES[:, k, :], ALU.mult, [r_tb], [r_tb])
                    TS("dve", ES[:, k + 1, :], T1[:], 2.0, None, ALU.mult, None, [r_tb], [r_tb])
                Ball = alloc(SETUP, "Ball", [128, 2, 32, 16], F32)
                for ri, bsrc in enumerate((b_re, b_im)):
                    b3 = bsrc.rearrange("(j g) p c -> (g p) j c", g=2)
                    for jb in range(8):
                        P.dma("sp", Ball[:, ri, jb * 4:(jb + 1) * 4, :], b3[:, jb * 4:(jb + 1) * 4, :], W=[r_tb])
                BB = alloc(SETUP, "BB", [128, 2, 32, 16], F32)
                T2 = alloc(SETUP, "T2", [128, 32, 16], F32)
                crb = CR[:, :].unsqueeze(2).broadcast_to([128, 32, 16])
                cib = CI[:, :].unsqueeze(2).broadcast_to([128, 32, 16])
                TT("dve", BB[:, 0, :, :], Ball[:, 0, :, :], crb, ALU.mult, [r_tb], [r_tb])
                TT("dve", T2[:], Ball[:, 1, :, :], cib, ALU.mult, [r_tb], [r_tb])
                TT("dve", BB[:, 0, :, :], BB[:, 0, :, :], T2[:], ALU.subtract, [r_tb], [r_tb])
                TT("dve", BB[:, 1, :, :], Ball[:, 1, :, :], crb, ALU.mult, [r_tb], [r_tb])
                TT("dve", T2[:], Ball[:, 0, :, :], cib, ALU.mult, [r_tb], [r_tb])
                TT("dve", BB[:, 1, :, :], BB[:, 1, :, :], T2[:], ALU.add, [r_tb], [r_tb])
                Zp = alloc(SETUP, "Zp", [128, 2, 4, 128], F32)
                MS("dve", Zp[:], 0.0, [r_tb])
                MS("dve", Cpad[:], 0.0, [r_tb])
                Wc = alloc(SETUP, "Wc", [32, 2, 32, 128], F32)
                MS("dve", Wc[:], 0.0, [r_tb])
                for ri, csrc in enumerate((c_re, c_im)):
                    c4 = csrc.rearrange("(j g) c p -> g c j p", g=2)
                    for g2 in range(2):
                        P.dma("sp", Wc[g2 * 16:(g2 + 1) * 16, ri, :, g2 * 64:(g2 + 1) * 64], c4[g2], W=[r_tb])
                r_bp = Reg("bpad")
                for j in range(32):
                    base = 32 * (j % 4)
                    for ri in range(2):
                        q = (2 * j + ri) % 2
                        CP("dve", Zp[0:64, ri, j % 4, base:base + 16], BB[0:64, ri, j, :], [r_tb], [r_tb])
                        CP("dve", Zp[64:128, ri, j % 4, base + 16:base + 32], BB[64:128, ri, j, :], [r_tb], [r_tb])
                        TR(psb[q][:, 0:128], Zp[:, ri, j % 4, :], ident_f[:], [r_tb, CONST], [r_ps[q]])
                        CP("act", Bpad[:, ri, j, :], psb[q][:, 0:128], [r_ps[q]], [r_bp])
                        TR(psb[2 + q][:, 0:32], Wc[:, ri, j, :], ident_f[0:32, 0:32], [r_tb, CONST], [r_ps[2 + q]])
                        if ri == 0:
                            CP("act", Cpad[:, 0, j, base:base + 32], psb[2 + q][:, 0:32], [r_ps[2 + q]], [r_bp])
                        else:
                            ACT(Cpad[:, 1, j, base:base + 32], psb[2 + q][:, 0:32], AF.Copy, [r_ps[2 + q]], [r_bp], scale=-1.0)
                P.barrier()
                stop_if("D0")
                SETUP.close()
                MAINS = ExitStack()
                MAINS.__enter__()
                Rc = alloc(MAINS, "Rc", [128, 1024], F32)
                Rs = alloc(MAINS, "Rs", [128, 1024], F32)
                r_R = Reg("R")
                TA = alloc(MAINS, "TA", [128, 1024], F32)
                TB = alloc(MAINS, "TB", [128, 1024], F32)
                r_T = Reg("T")
                WR = alloc(MAINS, "WR", [128, 1024], F32)
                WI = alloc(MAINS, "WI", [128, 1024], F32)
                r_W = Reg("W")
                VR = alloc(MAINS, "VR", [128, 1024], F32)
                VI = alloc(MAINS, "VI", [128, 1024], F32)
                r_V2 = Reg("V2")
                XR = alloc(MAINS, "XR", [128, 1024], BF16)
                XI = alloc(MAINS, "XI", [128, 1024], BF16)
                r_X = Reg("X")
                ini = alloc(MAINS, "ini", [128, 4], F32)
                r_ini = Reg("ini")
                yv = alloc(MAINS, "yv", [128, 1024], F32)
                yw = alloc(MAINS, "yw", [128, 1024], F32)
                r_yv = Reg("yv")
                for j in range(32):
                    cc = j // 4
                    MS("dve", Rc[:, 0:1], 1.0, [r_R])
                    MS("dve", Rs[:, 0:1], 0.0, [r_R])
                    for k in range(10):
                        n = 1 << k
                        ck, sk = EC[:, k, j:j + 1], ES[:, k, j:j + 1]
                        TS("dve", TA[:, 0:n], Rs[:, 0:n], sk, None, ALU.mult, None, [r_R, r_tb], [r_T])
                        STT(Rc[:, n:2 * n], Rc[:, 0:n], ck, TA[:, 0:n], ALU.mult, ALU.subtract, [r_R, r_T, r_tb], [r_R])
                        TS("dve", TA[:, 0:n], Rc[:, 0:n], sk, None, ALU.mult, None, [r_R, r_tb], [r_T])
                        STT(Rs[:, n:2 * n], Rs[:, 0:n], ck, TA[:, 0:n], ALU.mult, ALU.add, [r_R, r_T, r_tb], [r_R])
                    magb = MAG[:, j:j + 1].broadcast_to([128, 1024])
                    for hf in range(2):
                        for t in range(2):
                            cols = slice(hf * NOWN + t * 512, hf * NOWN + (t + 1) * 512)
                            lc = slice(t * 512, (t + 1) * 512)
                            MM(psb[0][:, :], Bpad[:, 0, j, :], uT[:, cc, cols], True, True, [r_bp, r_uT], [r_ps[0]])
                            MM(psb[1][:, :], Bpad[:, 1, j, :], uT[:, cc, cols], True, True, [r_bp, r_uT], [r_ps[1]])
                            TT("dve", TA[:, lc], psb[0][:, :], Rc[:, lc], ALU.mult, [r_ps[0], r_R], [r_T])
                            TT("dve", TB[:, lc], psb[1][:, :], Rs[:, lc], ALU.mult, [r_ps[1], r_R], [r_T])
                            TT("dve", WR[:, lc], TA[:, lc], TB[:, lc], ALU.add, [r_T], [r_W])
                            TT("dve", TA[:, lc], psb[1][:, :], Rc[:, lc], ALU.mult, [r_ps[1], r_R], [r_T])
                            TT("dve", TB[:, lc], psb[0][:, :], Rs[:, lc], ALU.mult, [r_ps[0], r_R], [r_T])
                            TT("dve", WI[:, lc], TA[:, lc], TB[:, lc], ALU.subtract, [r_T], [r_W])
                        if hf == 0:
                            i_r, i_i = 0.0, 0.0
                            rdeps = [r_W, r_tb]
                        else:
                            cL, sL = EC[:, 10, j:j + 1], ES[:, 10, j:j + 1]
                            TS("dve", ini[:, 2:3], VI[:, 1023:1024], sL, None, ALU.mult, None, [r_V2, r_tb], [r_ini])
                            STT(ini[:, 0:1], VR[:, 1023:1024], cL, ini[:, 2:3], ALU.mult, ALU.subtract, [r_V2, r_ini, r_tb], [r_ini])
                            TS("dve", ini[:, 3:4], VR[:, 1023:1024], sL, None, ALU.mult, None, [r_V2, r_tb], [r_ini])
                            STT(ini[:, 1:2], VI[:, 1023:1024], cL, ini[:, 3:4], ALU.mult, ALU.add, [r_V2, r_ini, r_tb], [r_ini])
                            i_r, i_i = ini[:, 0:1], ini[:, 1:2]
                            rdeps = [r_W, r_tb, r_ini]
                        P.op("dve", lambda g: g.tensor_tensor_scan(out=VR[:, :], data0=magb, data1=WR[:, :], initial=i_r,
                                                                   op0=ALU.mult, op1=ALU.add), rdeps, [r_V2])
                        P.op("dve", lambda g: g.tensor_tensor_scan(out=VI[:, :], data0=magb, data1=WI[:, :], initial=i_i,
                                                                   op0=ALU.mult, op1=ALU.add), rdeps, [r_V2])
                    TT("dve", TA[:, :], VR[:, :], Rc[:, :], ALU.mult, [r_V2, r_R], [r_T])
                    TT("dve", TB[:, :], VI[:, :], Rs[:, :], ALU.mult, [r_V2, r_R], [r_T])
                    TT("dve", XR[:, :], TA[:, :], TB[:, :], ALU.subtract, [r_T], [r_X])
                    TT("dve", TA[:, :], VR[:, :], Rs[:, :], ALU.mult, [r_V2, r_R], [r_T])
                    TT("dve", TB[:, :], VI[:, :], Rc[:, :], ALU.mult, [r_V2, r_R], [r_T])
                    TT("dve", XI[:, :], TA[:, :], TB[:, :], ALU.add, [r_T], [r_X])
                    for t in range(2):
                        lc = slice(t * 512, (t + 1) * 512)
                        MM(psb[4 + t][:, :], Cpad[:, 0, j, :], XR[:, lc], j % 4 == 0, False, [r_bp, r_X], [r_ps[4 + t]])
                        MM(psb[4 + t][:, :], Cpad[:, 1, j, :], XI[:, lc], False, j % 4 == 3, [r_bp, r_X], [r_ps[4 + t]])
                    if j % 4 == 3:
                        for t in range(2):
                            lc = slice(t * 512, (t + 1) * 512)
                            gc = slice(NOWN + t * 512, NOWN + (t + 1) * 512)
                            STT(yv[:, lc], uT[:, cc, gc], pcol("ssmd", cc), psb[4 + t][:, :], ALU.mult, ALU.add,
                                [r_uT, CONST, r_ps[4 + t]], [r_yv])
                        ACT(yw[:, :], yv[:, :], AF.Square, [r_yv], [r_yv])
                        TS("dve", yw[:, :], yw[:, :], 0.044715, 1.0, ALU.mult, ALU.add, [r_yv], [r_yv])
                        TT("dve", yw[:, :], yw[:, :], yv[:, :], ALU.mult, [r_yv], [r_yv])
                        ACT(yw[:, :], yw[:, :], AF.Sigmoid, [r_yv], [r_yv], scale=1.5957691216057308)
                        TT("dve", yact[:, cc, :], yw[:, :], yv[:, :], ALU.mult, [r_yv], [r_ya])
                P.barrier()
                stop_if("D1")
                MAINS.close()
                so = alloc(Dp, "so", [128, 8, 512], F32)
                r_so = Reg("so")
                sgt = alloc(Dp, "sgt", [128, 512], F32)
                sqb = alloc(Dp, "sqb", [128, 512], BF16)
                r_sg = Reg("sg")
                mo = alloc(Dp, "moS", [128, 8, 512], BF16)
                r_mo = Reg()
                wts = [wload(w_glu, 8, 0, 512), wload(w_glu, 8, 512, 512)]
                for t in range(2):
                    lc = slice(t * 512, (t + 1) * 512)
                    for co in range(8):
                        wt, wr = wts[co // 4]
                        q = co % 2
                        for kc in range(8):
                            MM(psb[q][:, :], wt[:, kc, (co % 4) * 128:(co % 4 + 1) * 128], yact[:, kc, lc], kc == 0, kc == 7,
                               [wr, r_ya], [r_ps[q]])
                        ACT(sgt[:, :], psb[q][:, :], AF.Sigmoid, [r_ps[q], CONST], [r_sg], bias=pcol("bglu", co))
                        TT("dve", so[:, co, :], sgt[:, :], yact[:, co, lc], ALU.mult, [r_sg, r_ya], [r_so])
                    tap("ssmT", so[:, :, :], [r_so]) if t == 0 else None
                    for co in range(8):
                        ACT(sqb[:, :], so[:, co, :], AF.Square, [r_so], [r_sg])
                        MM(psb[2][:, :], ones_b[:, :], sqb[:, :], co == 0, co == 7, [CONST, r_sg], [r_ps[2]])
                    ACT(sgt[:, :], psb[2][:, :], AF.Sqrt, [r_ps[2], CONST], [r_sg], scale=1.0 / 1024, bias=epsc[:, 0:1])
                    P.op("dve", lambda g: g.reciprocal(out=sgt[:, :], in_=sgt[:, :]), [r_sg], [r_sg])
                    for co in range(8):
                        STT(mo[:, co, :], so[:, co, :], pcol("mixs", co), sgt[:, :], ALU.mult, ALU.mult, [r_so, r_sg, CONST], [r_mo])
                    P.dma("sp", mix_d[1024:2048, lc].rearrange("(h p) n -> p h n", p=128), mo[:, :, :], R=[r_mo], W=[r_mixd])
                P.barrier()
                stop_if("D")
        XRs = ExitStack()
        with XRs:
            x_res = alloc(XRs, "x_res", [128, 8, D], F32)
            GtH = alloc(XRs, "GtH", [128, 8, NEXP], F32)
            r_GH = Reg("G")
            r_x = [Reg("x%d" % c) for c in range(8)]
            for c in range(8):
                P.dma("sp", x_res[:, c, :], x_own[c * 128:(c + 1) * 128, :], W=[r_x[c]])

            def proj_add(actT, r_act, wdram):
                for qd in range(4):
                    wt, wr = wload(wdram, 16, qd * 512, 512)
                    for c in range(8):
                        q = c % 2
                        for kc in range(16):
                            MM(psb[q][:, :], actT[:, kc, c * 128:(c + 1) * 128], wt[:, kc, :], kc == 0, kc == 15,
                               [r_act, wr], [r_ps[q]])
                        TT("dve", x_res[:, c, qd * 512:(qd + 1) * 512], psb[q][:, :], x_res[:, c, qd * 512:(qd + 1) * 512], ALU.add,
                           [r_ps[q], r_x[c]], [r_x[c]])

            def norm_to_T(es, gain_name, dstT, r_dst, stt, r_stt, also_f32=None):
                junk = alloc(es, "nT_junk", [128, D], BF16)
                xb = alloc(es, "nT_xb", [128, D], BF16)
                r_j, r_xb = Reg(), Reg()
                for c in range(8):
                    ACT(junk[:], x_res[:, c, :], AF.Square, [r_x[c]], [r_j, r_stt], accum_out=stt[:, c:c + 1])
                    rstd_from(stt[:, c:c + 1], stt[:, 8 + c:9 + c], D, r_stt, r_stt, stt[:, 16 + c:17 + c])
                    ACT(xb[:], x_res[:, c, :], AF.Copy, [r_x[c], r_stt], [r_xb], scale=stt[:, 8 + c:9 + c])
                    for qq in range(2):
                        for j in range(8):
                            fc = qq * 8 + j
                            TR(pbt[:, j * 128:(j + 1) * 128], xb[:, fc * 128:(fc + 1) * 128], ident_b[:], [r_xb, CONST], [r_pbt])
                        for j in range(8):
                            fc = qq * 8 + j
                            if j % 2 == 0:
                                TS("dve", dstT[:, fc, c * 128:(c + 1) * 128], pbt[:, j * 128:(j + 1) * 128], pcol(gain_name, fc), None,
                                   ALU.mult, None, [r_pbt, CONST], [r_dst])
                            else:
                                ACT(dstT[:, fc, c * 128:(c + 1) * 128], pbt[:, j * 128:(j + 1) * 128], AF.Copy, [r_pbt, CONST], [r_dst],
                                    scale=pcol(gain_name, fc))

            with ExitStack() as E:
                mixT = alloc(E, "mixT", [128, 16, NOWN], BF16)
                r_mix = Reg("mix")
                P.dma("sp", mixT[:, :, :], mix_d.rearrange("(kc p) n -> p kc n", p=128), R=[r_mixd], W=[r_mix])
                proj_add(mixT, r_mix, w_out)
                tap("x1", x_res[:, :, :], r_x)
                P.barrier()
                stop_if("E")
            with ExitStack() as Fp:
                stt = alloc(Fp, "sttF", [128, 32], F32)
                r_stt = Reg()
                hxT = alloc(Fp, "hxT", [128, 16, NOWN], BF16)
                r_hx = Reg("hx")
                with ExitStack() as F1:
                    norm_to_T(F1, "xattn", hxT, r_hx, stt, r_stt)
                    P.barrier()
                memT = alloc(Fp, "memT", [128, 16, MEM], BF16)
                r_mem = Reg("memT")
                with ExitStack() as F2:
                    ms = alloc(F2, "ms", [128, D], F32)
                    mjunk = alloc(F2, "mjunk", [128, D], BF16)
                    mb = alloc(F2, "mb", [128, D], BF16)
                    r_ms, r_mb = Reg(), Reg()
                    for c in range(2):
                        P.dma("sp", ms[:], mem_d[c * 128:(c + 1) * 128, :], W=[r_ms])
                        ACT(mjunk[:], ms[:], AF.Square, [r_ms], [r_mb, r_stt], accum_out=stt[:, 24 + c:25 + c])
                        rstd_from(stt[:, 24 + c:25 + c], stt[:, 26 + c:27 + c], D, r_stt, r_stt, stt[:, 28 + c:29 + c])
                        ACT(mb[:], ms[:], AF.Copy, [r_ms, r_stt], [r_mb], scale=stt[:, 26 + c:27 + c])
                        for qq in range(2):
                            for j in range(8):
                                fc = qq * 8 + j
                                TR(pbt[:, j * 128:(j + 1) * 128], mb[:, fc * 128:(fc + 1) * 128], ident_b[:], [r_mb, CONST], [r_pbt])
                            for j in range(8):
                                fc = qq * 8 + j
                                TS("dve", memT[:, fc, c * 128:(c + 1) * 128], pbt[:, j * 128:(j + 1) * 128], pcol("memn", fc), None,
                                   ALU.mult, None, [r_pbt, CONST], [r_mem])
                    P.barrier()
                kT = alloc(Fp, "kT", [128, 16, MEM], BF16)
                r_kT = Reg("kT")
                vM = alloc(Fp, "vM", [128, 2, D], BF16)
                r_vM = Reg("vM")
                qT = alloc(Fp, "qT", [128, 4, NOWN], BF16)
                r_qT = Reg("qT")
                oT = alloc(Fp, "oT", [128, 4, NOWN], BF16)
                r_oT = Reg("oT")
                for qd in range(4):
                    wt, wr = wload(w_xk, 16, qd * 512, 512)
                    for s4 in range(4):
                        q = s4 % 2
                        for kc in range(16):
                            MM(psb[q][:, 0:MEM], wt[:, kc, s4 * 128:(s4 + 1) * 128], memT[:, kc, :], kc == 0, kc == 15, [wr, r_mem], [r_ps[q]])
                        CP("act", kT[:, qd * 4 + s4, :], psb[q][:, 0:MEM], [r_ps[q]], [r_kT])
                for qd in range(4):
                    wt, wr = wload(w_xv, 16, qd * 512, 512)
                    for c in range(2):
                        q = c % 2
                        for kc in range(16):
                            MM(psb[q][:, :], memT[:, kc, c * 128:(c + 1) * 128], wt[:, kc, :], kc == 0, kc == 15, [wr, r_mem], [r_ps[q]])
                        CP("act", vM[:, c, qd * 512:(qd + 1) * 512], psb[q][:, :], [r_ps[q]], [r_vM])
                Ex = [alloc(Fp, "Ex%d" % i, [128, 512], BF16) for i in range(2)]
                r_Ex = [Reg(), Reg()]
                recx = alloc(Fp, "recx", [128, 512], F32)
                r_recx = Reg()
                XS = 512.0 ** -0.5
                for h in range(4):
                    wt, wr = wload(w_xq, 16, h * 512, 512)
                    for s4 in range(4):
                        for t in range(2):
                            q = (s4 * 2 + t) % 2
                            for kc in range(16):
                                MM(psb[q][:, :], wt[:, kc, s4 * 128:(s4 + 1) * 128], hxT[:, kc, t * 512:(t + 1) * 512], kc == 0, kc == 15,
                                   [wr, r_hx], [r_ps[q]])
                            CP("act", qT[:, s4, t * 512:(t + 1) * 512], psb[q][:, :], [r_ps[q]], [r_qT])
                    for t in range(2):
                        cols = slice(t * 512, (t + 1) * 512)
                        for m in range(2):
                            for dc in range(4):
                                MM(psb[m][:, :], kT[:, h * 4 + dc, m * 128:(m + 1) * 128], qT[:, dc, cols], dc == 0, dc == 3,
                                   [r_kT, r_qT], [r_ps[m]])
                            ACT(Ex[m][:, :], psb[m][:, :], AF.Exp, [r_ps[m]], [r_Ex[m]], scale=XS)
                        for m in range(2):
                            MM(psb[2][:, :], ones_b[:, :], Ex[m][:, :], m == 0, m == 1, [CONST, r_Ex[m]], [r_ps[2]])
                        P.op("dve", lambda g: g.reciprocal(out=recx[:], in_=psb[2][:, :]), [r_ps[2]], [r_recx])
                        for dv in range(4):
                            q = 3 + dv % 2
                            for m in range(2):
                                MM(psb[q][:, :], vM[:, m, h * 512 + dv * 128:h * 512 + (dv + 1) * 128], Ex[m][:, :], m == 0, m == 1,
                                   [r_vM, r_Ex[m]], [r_ps[q]])
                            TT("dve", oT[:, dv, cols], psb[q][:, :], recx[:], ALU.mult, [r_ps[q], r_recx], [r_oT])
                    for qd in range(4):
                        wt, wr = wload(w_xo, 4, qd * 512, 512, r0=h * 512)
                        for c in range(8):
                            q = 5 + c % 2
                            for kc in range(4):
                                MM(psb[q][:, :], oT[:, kc, c * 128:(c + 1) * 128], wt[:, kc, :], kc == 0, kc == 3, [r_oT, wr], [r_ps[q]])
                            TT("dve", x_res[:, c, qd * 512:(qd + 1) * 512], psb[q][:, :], x_res[:, c, qd * 512:(qd + 1) * 512], ALU.add,
                               [r_ps[q], r_x[c]], [r_x[c]])
                tap("x2", x_res[:, :, :], r_x)
                P.barrier()
                stop_if("F")
            CAP = 384
            NSC = CAP // 128
            with ExitStack() as Gp:
                stt = alloc(Gp, "sttG", [128, 32], F32)
                r_stt = Reg()
                xn_tm = alloc(Gp, "xn_tm", [128, 8, D], BF16)
                r_xn = Reg("xn_tm")
                Gt, r_G = GtH, r_GH
                bupT = alloc(Gp, "bupT", [128, 1024], F32)
                r_bup = Reg("bup")
                Mf = alloc(Gp, "Mf", [128, 8, NEXP], F32)
                posf = alloc(Gp, "posf", [128, 8, NEXP], F32)
                r_rt = Reg("route")
                iotaC = alloc(Gp, "iotaC", [128, CAP], F32)
                with ExitStack() as G1:
                    gbc = alloc(G1, "gbcF", [128, D], F32)
                    junk = alloc(G1, "junkG", [128, D], BF16)
                    wr32 = alloc(G1, "wr32", [128, 16, NEXP], F32)
                    wrh = alloc(G1, "wrh", [128, 16, NEXP], BF16)
                    wrl = alloc(G1, "wrl", [128, 16, NEXP], BF16)
                    wrd = alloc(G1, "wrd", [128, 16, NEXP], F32)
                    xgh = alloc(G1, "xgh", [128, 16, 128], BF16)
                    xgl = alloc(G1, "xgl", [128, 16, 128], BF16)
                    xgd = alloc(G1, "xgd", [128, 16, 128], F32)
                    brb = alloc(G1, "brb", [128, NEXP], F32)
                    xg = alloc(G1, "xg", [128, 16, 128], F32)
                    lg = alloc(G1, "lg", [128, NEXP], F32)
                    t8 = alloc(G1, "t8", [128, 8], F32)
                    ex = alloc(G1, "ex", [128, NEXP], F32)
                    sm = alloc(G1, "sm", [128, 4], F32)
                    braw = alloc(G1, "braw", [128, 128], F32)
                    Mb = alloc(G1, "Mb", [128, 8, NEXP], BF16)
                    triS = alloc(G1, "triS", [128, 128], BF16)
                    iti = alloc(G1, "iti", [128, CAP], I32)
                    r_w32, r_xg, r_lg, r_braw, r_j = Reg(), Reg(), Reg(), Reg(), Reg()
                    P.dma("sp", wr32[:, :, :], w_router.rearrange("(kc p) n -> p kc n", p=128), W=[r_w32])
                    P.dma("sp", brb[:, :], b_router.broadcast_to([128, NEXP]), W=[r_w32])
                    P.dma("sp", gbc[:, :], ffn_g.broadcast_to([128, D]), W=[r_w32])
                    r_wsp = Reg()
                    CP("dve", wrh[:], wr32[:], [r_w32], [r_wsp])
                    TT("dve", wrd[:], wr32[:], wrh[:], ALU.subtract, [r_w32, r_wsp], [r_wsp])
                    CP("dve", wrl[:], wrd[:], [r_wsp], [r_wsp])
                    r_it = Reg()
                    P.op("pool", lambda g: g.iota(iti[:, 0:128], pattern=[[1, 128]], base=0, channel_multiplier=-1), (), [r_it])
                    TS("dve", triS[:], iti[:, 0:128], 0, None, ALU.is_gt, None, [r_it], [r_rt])
                    P.op("pool", lambda g: g.iota(iti[:, :], pattern=[[1, CAP]], base=0, channel_multiplier=0), [r_it], [r_it])
                    CP("dve", iotaC[:], iti[:], [r_it], [r_rt])
                    for i in range(8):
                        P.dma("sp", braw[:, :], b_up[i * 128:(i + 1) * 128, :], W=[r_braw])
                        TR(psb[6][:, 0:128], braw[:, :], ident_f[:], [r_braw, CONST], [r_ps[6]])
                        CP("dve", bupT[:, i * 128:(i + 1) * 128], psb[6][:, 0:128], [r_ps[6]], [r_bup])
                    for c in range(8):
                        ACT(junk[:], x_res[:, c, :], AF.Square, [r_x[c]], [r_j, r_stt], accum_out=stt[:, c:c + 1])
                        rstd_from(stt[:, c:c + 1], stt[:, 8 + c:9 + c], D, r_stt, r_stt, stt[:, 16 + c:17 + c])
                        STT(xn_tm[:, c, :], x_res[:, c, :], stt[:, 8 + c:9 + c], gbc[:], ALU.mult, ALU.mult,
                            [r_x[c], r_stt, r_w32], [r_xn])
                        for qq in range(4):
                            for j in range(4):
                                fc = qq * 4 + j
                                TR(psb[qq][:, j * 128:(j + 1) * 128], x_res[:, c, fc * 128:(fc + 1) * 128], ident_f[:], [r_x[c], CONST], [r_ps[qq]])
                            for j in range(4):
                                fc = qq * 4 + j
                                TS("dve", xg[:, fc, :], psb[qq][:, j * 128:(j + 1) * 128], pcol("ffn", fc), None, ALU.mult, None,
                                   [r_ps[qq], CONST], [r_xg])
                        CP("dve", xgh[:], xg[:], [r_xg], [r_xg])
                        TT("dve", xgd[:], xg[:], xgh[:], ALU.subtract, [r_xg], [r_xg])
                        CP("dve", xgl[:], xgd[:], [r_xg], [r_xg])
                        for fc in range(16):
                            MM(psb[4][:, 0:NEXP], xgh[:, fc, :], wrh[:, fc, :], fc == 0, False, [r_xg, r_wsp], [r_ps[4]])
                            MM(psb[4][:, 0:NEXP], xgh[:, fc, :], wrl[:, fc, :], False, False, [r_xg, r_wsp], [r_ps[4]])
                            MM(psb[4][:, 0:NEXP], xgl[:, fc, :], wrh[:, fc, :], False, fc == 15, [r_xg, r_wsp], [r_ps[4]])
                        STT(lg[:], psb[4][:, 0:NEXP], stt[:, 8 + c:9 + c], brb[:], ALU.mult, ALU.add, [r_ps[4], r_stt, r_w32], [r_lg])
                        P.op("dve", lambda g: g.max(out=t8[:], in_=lg[:]), [r_lg], [r_lg])
                        TS("dve", Mf[:, c, :], lg[:], t8[:, 3:4], None, ALU.is_ge, None, [r_lg], [r_rt])
                        TS("dve", sm[:, 0:1], t8[:, 0:1], -1.0, None, ALU.mult, None, [r_lg], [r_lg])
                        ACT(ex[:], lg[:], AF.Exp, [r_lg], [r_lg], bias=sm[:, 0:1])
                        STT(ex[:], ex[:], 1.0, Mf[:, c, :], ALU.mult, ALU.mult, [r_lg, r_rt], [r_lg], accum_out=sm[:, 1:2])
                        P.op("dve", lambda g: g.reciprocal(out=sm[:, 2:3], in_=sm[:, 1:2]), [r_lg], [r_lg])
                        TS("dve", Gt[:, c, :], ex[:], sm[:, 2:3], None, ALU.mult, None, [r_lg], [r_G])
                        CP("dve", Mb[:, c, :], Mf[:, c, :], [r_rt], [r_rt])
                    for c in range(8):
                        for c2 in range(c):
                            MM(psb[5][:, 0:NEXP], ones_b[:, :], Mb[:, c2, :], c2 == 0, False, [CONST, r_rt], [r_ps[5]])
                        MM(psb[5][:, 0:NEXP], triS[:, :], Mb[:, c, :], c == 0, True, [r_rt], [r_ps[5]])
                        CP("dve", posf[:, c, :], psb[5][:, 0:NEXP], [r_ps[5]], [r_rt])
                    P.barrier()
                tap("logits", Gt[:, :, :], [r_G])
                stop_if("R")
                XO = alloc(Gp, "XO", [128, 16 * CAP], BF16)
                XeT = XO[:, :].rearrange("p (f s) -> p f s", s=CAP)
                oe = XO[:, :].rearrange("p (c n) -> p c n", n=D)
                r_XO = Reg("XO")
                actT = alloc(Gp, "actT", [128, 16, CAP], BF16)
                r_act = Reg("act")
                Sel = alloc(Gp, "Sel", [128, 8, CAP], BF16)
                r_Sel = Reg("Sel")
                SelT = alloc(Gp, "SelT", [128, NSC, NOWN], BF16)
                r_SelT = Reg("SelT")
                gb = alloc(Gp, "gb", [128, CAP], F32)
                sgb = alloc(Gp, "sgb", [128, CAP], F32)
                gs = alloc(Gp, "gs", [128, CAP], F32)
                lb = alloc(Gp, "lb", [128, CAP], F32)
                r_gb, r_gs, r_lb = Reg(), Reg(), Reg()
                hreg = [Reg("wh%d" % i) for i in range(6)]
                hctr = [0]

                def wload_h(dram2d, c0):
                    s = hctr[0] % 6
                    hctr[0] += 1
                    dst = wslot[s // 2][:, :, (s % 2) * 256:(s % 2 + 1) * 256]
                    src = dram2d[:, c0:c0 + 256].rearrange("(kc p) n -> p kc n", p=128)
                    P.dma("pool", dst, src, W=[hreg[s]])
                    return dst, hreg[s]

                for e in range(NEXP if P.enabled else 0):
                    for c in range(8):
                        TS("dve", Sel[:, c, :], iotaC[:, :], posf[:, c, e:e + 1], Mf[:, c, e:e + 1], ALU.is_equal, ALU.mult,
                           [r_rt], [r_Sel])
                    for fc in range(16):
                        q = fc % 2
                        for c in range(8):
                            MM(psb[q][:, 0:CAP], xn_tm[:, c, fc * 128:(fc + 1) * 128], Sel[:, c, :], c == 0, c == 7, [r_xn, r_Sel], [r_ps[q]])
                        if fc % 2 == 0:
                            CP("act", XeT[:, fc, :], psb[q][:, 0:CAP], [r_ps[q]], [r_XO])
                        else:
                            CP("dve", XeT[:, fc, :], psb[q][:, 0:CAP], [r_ps[q]], [r_XO])
                    for sc in range(NSC):
                        for c in range(8):
                            TR(pbt[:, c * 128:(c + 1) * 128], Sel[:, c, sc * 128:(sc + 1) * 128], ident_b[:], [r_Sel, CONST], [r_pbt])
                        CP("dve", SelT[:, sc, :], pbt[:, :], [r_pbt], [r_SelT])
                    for n in range(8):
                        wg, rg = wload_h(w_up[e], n * 256)
                        wl, rl = wload_h(w_up[e], 2048 + n * 256)
                        for s2 in range(2):
                            ffc = n * 2 + s2
                            bg = bupT[:, e * 32 + ffc:e * 32 + ffc + 1]
                            bl = bupT[:, e * 32 + 16 + ffc:e * 32 + 16 + ffc + 1]
                            qg, ql = 2 + ffc % 2, 4 + ffc % 2
                            for kc in range(16):
                                MM(psb[qg][:, 0:CAP], wg[:, kc, s2 * 128:(s2 + 1) * 128], XeT[:, kc, :], kc == 0, kc == 15, [rg, r_XO], [r_ps[qg]])
                            for kc in range(16):
                                MM(psb[ql][:, 0:CAP], wl[:, kc, s2 * 128:(s2 + 1) * 128], XeT[:, kc, :], kc == 0, kc == 15, [rl, r_XO], [r_ps[ql]])
                            TS("dve", gb[:], psb[qg][:, 0:CAP], bg, 7.0, ALU.add, ALU.min, [r_ps[qg], r_bup], [r_gb])
                            ACT(sgb[:], gb[:], AF.Sigmoid, [r_gb], [r_gb], scale=1.702)
                            TT("dve", gs[:], gb[:], sgb[:], ALU.mult, [r_gb], [r_gs])
                            TS("dve", lb[:], psb[ql][:, 0:CAP], bl, 7.0, ALU.add, ALU.min, [r_ps[ql], r_bup], [r_lb])
                            TS("dve", lb[:], lb[:], -7.0, 1.0, ALU.max, ALU.add, [r_lb], [r_lb])
                            TT("dve", actT[:, ffc, :], gs[:], lb[:], ALU.mult, [r_gs, r_lb], [r_act])
                    for od in range(8):
                        wt, wr = wload_h(w_down[e], od * 256)
                        for sc in range(NSC):
                            q = (od * NSC + sc) % 2
                            for kc in range(16):
                                MM(psb[q][:, 0:256], actT[:, kc, sc * 128:(sc + 1) * 128], wt[:, kc, :], kc == 0, kc == 15, [r_act, wr], [r_ps[q]])
                            CP("act", oe[:, sc, od * 256:(od + 1) * 256], psb[q][:, 0:256], [r_ps[q]], [r_XO])
                    for c in range(8):
                        for qd in range(4):
                            q = 2 + (c * 4 + qd) % 4
                            for sc in range(NSC):
                                MM(psb[q][:, :], SelT[:, sc, c * 128:(c + 1) * 128], oe[:, sc, qd * 512:(qd + 1) * 512], sc == 0, sc == NSC - 1,
                                   [r_SelT, r_XO], [r_ps[q]])
                            STT(x_res[:, c, qd * 512:(qd + 1) * 512], psb[q][:, :], Gt[:, c, e:e + 1], x_res[:, c, qd * 512:(qd + 1) * 512],
                                ALU.mult, ALU.add, [r_ps[q], r_G, r_x[c]], [r_x[c]])
                P.barrier()
            with ExitStack() as Hp:
                bd = alloc(Hp, "bd", [NEXP, D], F32)
                bdh = alloc(Hp, "bdh", [NEXP, D], BF16)
                bdl = alloc(Hp, "bdl", [NEXP, D], BF16)
                gT = alloc(Hp, "gT", [NEXP, 128], F32)
                gTh = alloc(Hp, "gTh", [NEXP, 128], BF16)
                gTl = alloc(Hp, "gTl", [NEXP, 128], BF16)
                gTd = alloc(Hp, "gTd", [NEXP, 128], F32)
                gbc = alloc(Hp, "gbc", [128, D], F32)
                stt = alloc(Hp, "sttH", [128, 32], F32)
                junk = alloc(Hp, "junkH", [128, D], BF16)
                yo = [alloc(Hp, "yo%d" % i, [128, D], F32) for i in range(2)]
                r_bd, r_gT, r_stt, r_j = Reg(), Reg(), Reg(), Reg()
                r_yo = [Reg(), Reg()]
                P.dma("sp", bd[:, :], b_down, W=[r_bd])
                P.dma("sp", gbc[:, :], final_norm.broadcast_to([128, D]), W=[r_bd])
                r_bds = Reg()
                CP("dve", bdh[:], bd[:], [r_bd], [r_bds])
                TT("dve", yo[0][0:NEXP, :], bd[:], bdh[:], ALU.subtract, [r_bd, r_bds], [r_yo[0]])
                CP("dve", bdl[:], yo[0][0:NEXP, :], [r_yo[0]], [r_bds])
                for c in range(8):
                    TR(psb[0][0:NEXP, 0:128], GtH[:, c, :], ident_f[:], [r_GH, CONST], [r_ps[0]])
                    CP("dve", gT[:, :], psb[0][0:NEXP, 0:128], [r_ps[0]], [r_gT])
                    CP("dve", gTh[:, :], gT[:, :], [r_gT], [r_gT])
                    TT("dve", gTd[:, :], gT[:, :], gTh[:, :], ALU.subtract, [r_gT], [r_gT])
                    CP("dve", gTl[:, :], gTd[:, :], [r_gT], [r_gT])
                    for qd in range(4):
                        q = 1 + qd % 2
                        cs = slice(qd * 512, (qd + 1) * 512)
                        MM(psb[q][:, :], gTh[:, :], bdh[:, cs], True, False, [r_gT, r_bds], [r_ps[q]])
                        MM(psb[q][:, :], gTh[:, :], bdl[:, cs], False, False, [r_gT, r_bds], [r_ps[q]])
                        MM(psb[q][:, :], gTl[:, :], bdh[:, cs], False, True, [r_gT, r_bds], [r_ps[q]])
                        TT("dve", x_res[:, c, qd * 512:(qd + 1) * 512], psb[q][:, :], x_res[:, c, qd * 512:(qd + 1) * 512], ALU.add,
                           [r_ps[q], r_x[c]], [r_x[c]])
                    ACT(junk[:], x_res[:, c, :], AF.Square, [r_x[c]], [r_j, r_stt], accum_out=stt[:, c:c + 1])
                    rstd_from(stt[:, c:c + 1], stt[:, 8 + c:9 + c], D, r_stt, r_stt, stt[:, 16 + c:17 + c])
                    s = c % 2
                    STT(yo[s][:], x_res[:, c, :], stt[:, 8 + c:9 + c], gbc[:], ALU.mult, ALU.mult, [r_x[c], r_stt, r_bd], [r_yo[s]])
                    P.dma("sp", y_d[c * 128:(c + 1) * 128, :], yo[s][:], R=[r_yo[s]])
    if P.enabled:
        P.final_wait("sp")
    P.es.close()
    return nc


def _pv(inp):
    pv = np.zeros((128, 128), np.float32)
    def put(name, vec):
        v = np.asarray(vec, np.float32).reshape(-1, 128)
        r = PV_ROWS[name]
        pv[r:r + v.shape[0]] = v
    put("attn", inp["attn_norm"][0]); put("xattn", inp["xattn_norm"][0]); put("memn", inp["mem_norm"][0])
    put("ffn", inp["ffn_norm"][0]); put("qa", inp["q_a_norm"][0]); put("kva", inp["kv_a_norm"][0])
    put("mixa", inp["mix_norm_attn"][0]); put("mixs", inp["mix_norm_ssm"][0]); put("ssmd", inp["ssm_d"][0])
    put("bglu", inp["b_glu"][0])
    return pv


def make_in_maps(inp, stop=None):
    f = lambda a: np.ascontiguousarray(np.asarray(a, np.float32))
    shared = dict(
        pv=_pv(inp), w_in=f(inp["w_in"][0]), w_q_b=f(inp["w_q_b"][0]), w_kv_b=f(inp["w_kv_b"][0]),
        lam_re=f(inp["ssm_lambda_re"][0]), lam_im=f(inp["ssm_lambda_im"][0]),
        log_dt=f(inp["ssm_log_dt"][0]).reshape(64, 1),
        b_re=f(inp["ssm_b_re"][0]), b_im=f(inp["ssm_b_im"][0]), c_re=f(inp["ssm_c_re"][0]), c_im=f(inp["ssm_c_im"][0]),
        w_glu=f(inp["w_glu"][0]), w_out=f(inp["w_out"][0]), w_xq=f(inp["w_xq"][0]), w_xk=f(inp["w_xk"][0]),
        w_xv=f(inp["w_xv"][0]), w_xo=f(inp["w_xo"][0]), w_router=f(inp["w_router"][0]),
        b_router=f(inp["b_router"][0]).reshape(1, NEXP), w_up=f(inp["w_up"][0]),
        b_up=f(inp["b_up"][0]).reshape(NEXP * 32, 128), w_down=f(inp["w_down"][0]), b_down=f(inp["b_down"][0]),
        final_norm=f(inp["final_norm"]).reshape(1, D),
        ffn_g=f(inp["ffn_norm"][0]).reshape(1, D),
    )
    if stop is not None:
        shared["w_up"] = shared["w_up"][0:1]
        shared["w_down"] = shared["w_down"][0:1]
    x = np.asarray(inp["x"], np.float32)
    mem = np.asarray(inp["mem"], np.float32)
    pos = np.asarray(inp["positions"], np.int32)
    maps = []
    for c in range(8):
        b, h = c // 2, c % 2
        m = dict(shared)
        m["x_own"] = np.ascontiguousarray(x[b, h * NOWN:(h + 1) * NOWN])
        m["x_pre"] = np.ascontiguousarray(x[b, 0:NOWN]) if h == 1 else np.zeros((NOWN, D), np.float32)
        m["pos"] = np.ascontiguousarray(np.concatenate([pos[b, 0:NOWN], pos[b, h * NOWN:(h + 1) * NOWN]]).reshape(1, NALL))
        m["pbias"] = np.full((128, 1), 0.0 if h == 1 else NEG, np.float32)
        m["mem"] = np.ascontiguousarray(mem[b])
        maps.append(m)
    return maps


def kernel(**inputs):
    nc = build()
    maps = make_in_maps(inputs)
    res = run_bass_kernel_spmd(nc, maps, core_ids=list(range(8)))
    out = np.zeros((4, SEQ, D), np.float32)
    for c in range(8):
        b, h = c // 2, c % 2
        out[b, h * NOWN:(h + 1) * NOWN] = res.results[c]["y"]
    return out
```
